# Optimizing a Trainium2 kernel written in Bass

```python
import jax, jax.numpy as jnp
from jax import lax
import numpy as np

D_MODEL = 2048
BATCH = 2
SEQ = 4096
DEPTH = 1

LRU_WIDTH = D_MODEL // 2
LRU_HEADS = 8
LRU_BLOCK = LRU_WIDTH // LRU_HEADS
LRU_CONV = 4
LRU_C = 8.0
N_HEADS = 8
N_KV_HEADS = 2
GQA = N_HEADS // N_KV_HEADS
HEAD_DIM = (D_MODEL - LRU_WIDTH) // N_HEADS
ATTN_WIDTH = N_HEADS * HEAD_DIM
KV_WIDTH = N_KV_HEADS * HEAD_DIM
MIX_WIDTH = LRU_WIDTH + ATTN_WIDTH
N_BRANCH = 3
CMP_BLOCK = 32
CMP_STRIDE = 16
SEL_BLOCK = 64
N_SEL = 16
N_LOCAL_SEL = 2
WINDOW = 512
Q_BLOCK = 128
IN_WIDTH = 2 * LRU_WIDTH + ATTN_WIDTH + 6 * KV_WIDTH + N_BRANCH * N_HEADS
D_FF = 3 * D_MODEL
FFN_CONV = 3
EPS = 1e-6
NEG_INF = -1e30
FORCE_SCORE = 1e4

kernel_name = 'hymba_rglru_nsa_convffn_block'


def rms_norm(x, g):
    xf = x.astype(jnp.float32)
    y = xf * lax.rsqrt(jnp.mean(xf * xf, axis=-1, keepdims=True) + EPS)
    return (y * g.astype(jnp.float32)).astype(x.dtype)


def causal_dwconv(x, w, b):
    k, c = w.shape
    y = lax.conv_general_dilated(x, w[:, None, :].astype(x.dtype), window_strides=(1,),
                                 padding=[(k - 1, 0)], dimension_numbers=('NWC', 'WIO', 'NWC'),
                                 feature_group_count=c)
    return y + b.astype(x.dtype)


def rg_lru(x, w_a, b_a, w_x, b_x, lam):
    bsz, s, c = x.shape
    xf = x.astype(jnp.float32)
    xh = xf.reshape(bsz, s, LRU_HEADS, LRU_BLOCK)
    r = jax.nn.sigmoid(jnp.einsum('bshi,hij->bshj', xh, w_a.astype(jnp.float32)).reshape(bsz, s, c) + b_a.astype(jnp.float32))
    i = jax.nn.sigmoid(jnp.einsum('bshi,hij->bshj', xh, w_x.astype(jnp.float32)).reshape(bsz, s, c) + b_x.astype(jnp.float32))
    log_a = -LRU_C * r * jax.nn.softplus(-lam.astype(jnp.float32))
    a = jnp.exp(log_a)
    u = jnp.sqrt(-jnp.expm1(2.0 * log_a)) * (i * xf)

    def combine(left, right):
        a_l, b_l = left
        a_r, b_r = right
        return a_l * a_r, a_r * b_l + b_r

    _, h = lax.associative_scan(combine, (a, u), axis=1)
    return h.astype(x.dtype)


def compress_kv(kv, pe, w1, b1, w2):
    bsz, s, hk, d = kv.shape
    n_c = (s - CMP_BLOCK) // CMP_STRIDE + 1
    idx = jnp.arange(n_c)[:, None] * CMP_STRIDE + jnp.arange(CMP_BLOCK)[None, :]
    blocks = kv[:, idx] + pe[:, None, :]
    flat = blocks.transpose(0, 1, 3, 2, 4).reshape(bsz, n_c, hk, CMP_BLOCK * d)
    return jax.nn.gelu(flat @ w1 + b1) @ w2


def nsa_attention(q, k_cmp, v_cmp, k_sel, v_sel, k_win, v_win, gate_logits):
    bsz, s = q.shape[0], q.shape[1]
    n_c = k_cmp.shape[1]
    n_s = s // SEL_BLOCK
    k_top = min(N_SEL, n_s)
    cmp_start = jnp.arange(n_c) * CMP_STRIDE
    cmp_end = cmp_start + CMP_BLOCK - 1
    sel_start = jnp.arange(n_s) * SEL_BLOCK
    overlap = jnp.clip(jnp.minimum(cmp_start[:, None] + CMP_BLOCK, sel_start[None, :] + SEL_BLOCK)
                       - jnp.maximum(cmp_start[:, None], sel_start[None, :]), 0).astype(jnp.float32) / CMP_BLOCK
    kb = k_sel.reshape(bsz, n_s, SEL_BLOCK, N_KV_HEADS, HEAD_DIM).transpose(0, 3, 1, 2, 4)
    vb = v_sel.reshape(bsz, n_s, SEL_BLOCK, N_KV_HEADS, HEAD_DIM).transpose(0, 3, 1, 2, 4)
    pad = ((0, 0), (WINDOW, 0), (0, 0), (0, 0))
    k_win_p = jnp.pad(k_win, pad)
    v_win_p = jnp.pad(v_win, pad)
    b_idx = jnp.arange(bsz)[:, None, None, None]
    h_idx = jnp.arange(N_KV_HEADS)[None, :, None, None]
    blk = jnp.arange(n_s)

    def block(c):
        qs = c * Q_BLOCK
        q_b = lax.dynamic_slice_in_dim(q, qs, Q_BLOCK, axis=1)
        g_b = lax.dynamic_slice_in_dim(gate_logits, qs, Q_BLOCK, axis=1)
        t = qs + jnp.arange(Q_BLOCK)
        valid_c = cmp_end[None, :] <= t[:, None]
        s_c = jnp.einsum('bqhgd,bnhd->bhgqn', q_b, k_cmp).astype(jnp.float32)
        p_c = jax.nn.softmax(jnp.where(valid_c, s_c, NEG_INF), axis=-1) * valid_c
        o_c = jnp.einsum('bhgqn,bnhd->bqhgd', p_c.astype(v_cmp.dtype), v_cmp)
        imp = jnp.einsum('bhgqn,ns->bhqs', p_c, overlap)
        cur = t // SEL_BLOCK
        causal_s = blk[None, :] <= cur[:, None]
        forced = (blk[None, :] == 0) | (causal_s & (blk[None, :] > cur[:, None] - N_LOCAL_SEL))
        score = jnp.where(forced, FORCE_SCORE, jnp.where(causal_s, imp, -1.0))
        _, sel = lax.top_k(score, k_top)
        k_g = kb[b_idx, h_idx, sel]
        v_g = vb[b_idx, h_idx, sel]
        kpos = sel[..., None] * SEL_BLOCK + jnp.arange(SEL_BLOCK)
        valid_s = (kpos <= t[None, None, :, None, None]).reshape(bsz, N_KV_HEADS, 1, Q_BLOCK, k_top * SEL_BLOCK)
        s_s = jnp.einsum('bqhgd,bhqjld->bhgqjl', q_b, k_g).reshape(bsz, N_KV_HEADS, GQA, Q_BLOCK, k_top * SEL_BLOCK)
        p_s = jax.nn.softmax(jnp.where(valid_s, s_s.astype(jnp.float32), NEG_INF), axis=-1)
        p_s = p_s.reshape(bsz, N_KV_HEADS, GQA, Q_BLOCK, k_top, SEL_BLOCK)
        o_s = jnp.einsum('bhgqjl,bhqjld->bqhgd', p_s.astype(v_g.dtype), v_g)
        k_w = lax.dynamic_slice_in_dim(k_win_p, qs, WINDOW + Q_BLOCK, axis=1)
        v_w = lax.dynamic_slice_in_dim(v_win_p, qs, WINDOW + Q_BLOCK, axis=1)
        kpos_w = qs - WINDOW + jnp.arange(WINDOW + Q_BLOCK)
        diff = t[:, None] - kpos_w[None, :]
        valid_w = (diff >= 0) & (diff < WINDOW) & (kpos_w[None, :] >= 0)
        s_w = jnp.einsum('bqhgd,bkhd->bhgqk', q_b, k_w).astype(jnp.float32)
        p_w = jax.nn.softmax(jnp.where(valid_w, s_w, NEG_INF), axis=-1)
        o_w = jnp.einsum('bhgqk,bkhd->bqhgd', p_w.astype(v_w.dtype), v_w)
        g = jax.nn.sigmoid(g_b.astype(jnp.float32))
        o = g[..., 0:1] * o_c + g[..., 1:2] * o_s + g[..., 2:3] * o_w
        return o.reshape(bsz, Q_BLOCK, ATTN_WIDTH).astype(q.dtype)

    out = lax.map(block, jnp.arange(s // Q_BLOCK))
    return out.transpose(1, 0, 2, 3).reshape(bsz, s, ATTN_WIDTH)


def setup_inputs(seed: int = 0) -> dict:
    key = jax.random.key(seed)
    ks = jax.random.split(key, 32)
    f32 = jnp.float32

    def nrm(k, shape, scale):
        return jax.random.normal(k, shape, f32) * scale

    def gain(k, n):
        return 1.0 + nrm(k, (DEPTH, n), 0.02)

    a0 = jax.random.uniform(ks[9], (DEPTH, LRU_WIDTH), f32, minval=0.9, maxval=0.999)
    return {
        'x': nrm(ks[0], (BATCH, SEQ, D_MODEL), 1.0),
        'g_mix': gain(ks[1], D_MODEL),
        'w_in': nrm(ks[2], (DEPTH, D_MODEL, IN_WIDTH), D_MODEL ** -0.5),
        'lru_conv_w': nrm(ks[3], (DEPTH, LRU_CONV, LRU_WIDTH), LRU_CONV ** -0.5),
        'lru_conv_b': nrm(ks[4], (DEPTH, LRU_WIDTH), 0.02),
        'lru_wa': nrm(ks[5], (DEPTH, LRU_HEADS, LRU_BLOCK, LRU_BLOCK), LRU_BLOCK ** -0.5),
        'lru_ba': nrm(ks[6], (DEPTH, LRU_WIDTH), 0.02),
        'lru_wx': nrm(ks[7], (DEPTH, LRU_HEADS, LRU_BLOCK, LRU_BLOCK), LRU_BLOCK ** -0.5),
        'lru_bx': nrm(ks[8], (DEPTH, LRU_WIDTH), 0.02),
        'lru_lambda': jnp.log(a0) - jnp.log1p(-a0),
        'cmp_pe_k': nrm(ks[10], (DEPTH, CMP_BLOCK, HEAD_DIM), 0.02),
        'cmp_w1_k': nrm(ks[11], (DEPTH, CMP_BLOCK * HEAD_DIM, HEAD_DIM), (CMP_BLOCK * HEAD_DIM) ** -0.5),
        'cmp_b1_k': nrm(ks[12], (DEPTH, HEAD_DIM), 0.02),
        'cmp_w2_k': nrm(ks[13], (DEPTH, HEAD_DIM, HEAD_DIM), HEAD_DIM ** -0.5),
        'cmp_pe_v': nrm(ks[14], (DEPTH, CMP_BLOCK, HEAD_DIM), 0.02),
        'cmp_w1_v': nrm(ks[15], (DEPTH, CMP_BLOCK * HEAD_DIM, HEAD_DIM), (CMP_BLOCK * HEAD_DIM) ** -0.5),
        'cmp_b1_v': nrm(ks[16], (DEPTH, HEAD_DIM), 0.02),
        'cmp_w2_v': nrm(ks[17], (DEPTH, HEAD_DIM, HEAD_DIM), HEAD_DIM ** -0.5),
        'g_lru_out': gain(ks[18], LRU_WIDTH),
        'g_attn_out': gain(ks[19], ATTN_WIDTH),
        'w_out': nrm(ks[20], (DEPTH, MIX_WIDTH, D_MODEL), MIX_WIDTH ** -0.5),
        'g_ffn': gain(ks[21], D_MODEL),
        'w_up': nrm(ks[22], (DEPTH, D_MODEL, 2 * D_FF), D_MODEL ** -0.5),
        'ffn_conv_w': nrm(ks[23], (DEPTH, FFN_CONV, D_FF), FFN_CONV ** -0.5),
        'ffn_conv_b': nrm(ks[24], (DEPTH, D_FF), 0.02),
        'w_down': nrm(ks[25], (DEPTH, D_FF, D_MODEL), D_FF ** -0.5),
        'g_final': 1.0 + nrm(ks[26], (D_MODEL,), 0.02),
    }


def reference(x, g_mix, w_in, lru_conv_w, lru_conv_b, lru_wa, lru_ba, lru_wx, lru_bx, lru_lambda,
              cmp_pe_k, cmp_w1_k, cmp_b1_k, cmp_w2_k, cmp_pe_v, cmp_w1_v, cmp_b1_v, cmp_w2_v,
              g_lru_out, g_attn_out, w_out, g_ffn, w_up, ffn_conv_w, ffn_conv_b, w_down, g_final):
    bsz, s, _ = x.shape
    cuts = np.cumsum([LRU_WIDTH, LRU_WIDTH, ATTN_WIDTH] + [KV_WIDTH] * 6).tolist()
    h = x
    for l in range(DEPTH):
        xn = rms_norm(h, g_mix[l])
        proj = xn @ w_in[l]
        lru_x, lru_gate, q, kc, vc, ksel, vsel, kw, vw, gate_logits = jnp.split(proj, cuts, axis=-1)
        u = causal_dwconv(lru_x, lru_conv_w[l], lru_conv_b[l])
        y_lru = rg_lru(u, lru_wa[l], lru_ba[l], lru_wx[l], lru_bx[l], lru_lambda[l]) * jax.nn.gelu(lru_gate)
        q = (q * (HEAD_DIM ** -0.5)).reshape(bsz, s, N_KV_HEADS, GQA, HEAD_DIM)
        kc = kc.reshape(bsz, s, N_KV_HEADS, HEAD_DIM)
        vc = vc.reshape(bsz, s, N_KV_HEADS, HEAD_DIM)
        k_cmp = compress_kv(kc, cmp_pe_k[l], cmp_w1_k[l], cmp_b1_k[l], cmp_w2_k[l])
        v_cmp = compress_kv(vc, cmp_pe_v[l], cmp_w1_v[l], cmp_b1_v[l], cmp_w2_v[l])
        y_attn = nsa_attention(q, k_cmp, v_cmp,
                               ksel.reshape(bsz, s, N_KV_HEADS, HEAD_DIM),
                               vsel.reshape(bsz, s, N_KV_HEADS, HEAD_DIM),
                               kw.reshape(bsz, s, N_KV_HEADS, HEAD_DIM),
                               vw.reshape(bsz, s, N_KV_HEADS, HEAD_DIM),
                               gate_logits.reshape(bsz, s, N_KV_HEADS, GQA, N_BRANCH))
        mixed = jnp.concatenate([rms_norm(y_lru, g_lru_out[l]), rms_norm(y_attn, g_attn_out[l])], axis=-1)
        h = h + mixed @ w_out[l]
        xn = rms_norm(h, g_ffn[l])
        u_ff, v_ff = jnp.split(xn @ w_up[l], 2, axis=-1)
        u_ff = causal_dwconv(u_ff, ffn_conv_w[l], ffn_conv_b[l])
        h = h + (jax.nn.gelu(u_ff) * v_ff) @ w_down[l]
    return rms_norm(h, g_final)
```

```python
import numpy as np
import ml_dtypes
import concourse.bass as bass
import concourse.mybir as mybir
from concourse.bass_utils import run_bass_kernel_spmd

F32 = mybir.dt.float32
BF16 = mybir.dt.bfloat16
AF = mybir.ActivationFunctionType
ALU = mybir.AluOpType

D = 2048
KC = 16
S = 4096
NOWN = 1026
EPS = 1e-6
NDMA = 12
USE_GELU_TANH_LUT = True


class Tk:
    __slots__ = ("name", "w", "r")

    def __init__(self, name):
        self.name = name
        self.w = None
        self.r = []


class Prog:
    def __init__(self, nc):
        self.nc = nc
        self.engs = ("pe", "act", "dve", "pool", "sp")
        self.sem = {k: nc.alloc_semaphore("sem_" + k) for k in self.engs}
        self.cnt = {k: 0 for k in self.engs}
        self.streams = {k: [] for k in self.engs}
        self.waited = {k: {} for k in self.engs}
        self.dq = {q: [[nc.alloc_semaphore("dq_%s_%d" % (q, i)), 0] for i in range(NDMA)] for q in ("sp", "pool")}
        self.dq_rr = {"sp": 0, "pool": 0}
        self.out_events = []

    def _wait(self, eng, evs):
        best = {}
        for ev in evs:
            if ev is None:
                continue
            key, sem, val = ev
            if key == eng and eng == "pe":
                continue
            if self.waited[eng].get(key, 0) >= val:
                continue
            if key not in best or best[key][1] < val:
                best[key] = (sem, val)
        for key, (sem, val) in best.items():
            self.waited[eng][key] = val
            self.streams[eng].append(lambda e, sem=sem, val=val: e.wait_ge(sem, val))

    def _deps(self, r, w):
        evs = []
        for t in r:
            evs.append(t.w)
        for t in w:
            evs.append(t.w)
            evs.extend(t.r)
        return evs

    def op(self, eng, fn, r=(), w=()):
        self._wait(eng, self._deps(r, w))
        self.cnt[eng] += 1
        sem = self.sem[eng]
        self.streams[eng].append(lambda e, fn=fn, sem=sem: fn(e).then_inc(sem, 1))
        ev = (eng, sem, self.cnt[eng])
        for t in r:
            t.r.append(ev)
        for t in w:
            t.w = ev
            t.r = []
        return ev

    def dma(self, q, out, in_, r=(), w=(), is_output=False):
        slot = self.dq[q][self.dq_rr[q] % NDMA]
        self.dq_rr[q] += 1
        sem = slot[0]
        key = "dma_" + str(id(slot))
        evs = self._deps(r, w)
        if slot[1] > 0:
            evs.append((key, sem, slot[1]))
        self._wait(q, evs)
        slot[1] += 16
        val = slot[1]
        self.streams[q].append(lambda e, out=out, in_=in_, sem=sem: e.dma_start(out=out, in_=in_).then_inc(sem, 16))
        ev = (key, sem, val)
        for t in r:
            t.r.append(ev)
        for t in w:
            t.w = ev
            t.r = []
        if is_output:
            self.out_events.append(ev)
        return ev

    def barrier(self):
        evs = [(k, self.sem[k], self.cnt[k]) for k in self.engs if self.cnt[k] > 0]
        for q in ("sp", "pool"):
            for slot in self.dq[q]:
                if slot[1] > 0:
                    evs.append(("dma_" + str(id(slot)), slot[0], slot[1]))
        for e in self.engs:
            self._wait(e, [ev for ev in evs if ev[0] != e])

    def finish(self):
        self.barrier()
        self._wait("sp", self.out_events)
        nc = self.nc
        st = self.streams
        with nc.Block() as block:
            @block.tensor
            def _(e):
                for f in st["pe"]:
                    f(e)

            @block.scalar
            def _(e):
                for f in st["act"]:
                    f(e)

            @block.vector
            def _(e):
                for f in st["dve"]:
                    f(e)

            @block.gpsimd
            def _(e):
                for f in st["pool"]:
                    f(e)

            @block.sync
            def _(e):
                for f in st["sp"]:
                    f(e)

    def act(self, out, in_, func, r=(), w=(), **kw):
        return self.op("act", lambda e: e.activation(out=out, in_=in_, func=func, **kw), r, w)

    def mm(self, out, lhsT, rhs, start, stop, r=(), w=()):
        return self.op("pe", lambda e: e.matmul(out, lhsT, rhs, start=start, stop=stop), r, w)

    def tr(self, out, in_, ident, r=(), w=()):
        return self.op("pe", lambda e: e.transpose(out, in_, ident), r, w)

    def tt(self, eng, out, in0, in1, op, r=(), w=()):
        return self.op(eng, lambda e: e.tensor_tensor(out=out, in0=in0, in1=in1, op=op), r, w)

    def ts(self, eng, out, in0, s1, s2, op0, op1=None, r=(), w=()):
        if op1 is None:
            return self.op(eng, lambda e: e.tensor_scalar(out=out, in0=in0, scalar1=s1, scalar2=None, op0=op0), r, w)
        return self.op(eng, lambda e: e.tensor_scalar(out=out, in0=in0, scalar1=s1, scalar2=s2, op0=op0, op1=op1), r, w)

    def stt(self, out, in0, scalar, in1, op0, op1, r=(), w=()):
        return self.op("dve", lambda e: e.scalar_tensor_tensor(out=out, in0=in0, scalar=scalar, in1=in1, op0=op0, op1=op1), r, w)

    def cp(self, eng, out, in_, r=(), w=()):
        if eng == "act":
            return self.act(out, in_, AF.Copy, r, w)
        return self.op(eng, lambda e: e.tensor_copy(out=out, in_=in_), r, w)


class Arena:
    def __init__(self, nc, base, limit):
        self.nc = nc
        self.off = base
        self.limit = limit
        self.n = 0

    def alloc(self, name, shape, dt):
        esz = 2 if dt == BF16 else 4
        nb = esz
        for s in shape[1:]:
            nb *= s
        nb = (nb + 31) // 32 * 32
        t = self.nc.alloc_sbuf_tensor_at("%s_%d" % (name, self.n), list(shape), dt, offset=self.off)
        self.n += 1
        self.off += nb
        assert self.off <= self.limit, ("SBUF overflow", name, self.off, self.limit)
        return t

    def mark(self):
        return self.off

    def release(self, m):
        self.off = m


def build_program():
    nc = bass.Bass("TRN2", target_bir_lowering=False)
    P = Prog(nc)

    def din(name, shape, dt=F32):
        return nc.dram_tensor(name, list(shape), dt, kind="ExternalInput").ap()

    x_loc = din("x_loc", [S, D])
    stflag_d = din("stflag", [128, 8])
    validT_d = din("validT", [128, 32])
    validn_d = din("validn", [128, 2])
    f0_d = din("f0", [128, 64])
    wA1_d = din("wA1", [128, KC, 1024])
    wB3_d = din("wB3", [128, KC, 1024])
    wA2_d = din("wA2", [128, KC, 1024])
    wB1_d = din("wB1", [128, KC, 536])
    wB2_d = din("wB2", [128, KC, 1024])
    gmixT_d = din("gmixT", [128, KC])
    lcw_d = din("lcw", [128, 8, 4])
    lvec_d = din("lvec", [128, 4, 8])
    wa_d = din("wa", [128, 8, 128])
    wx_d = din("wx", [128, 8, 128])
    w1k_d = din("w1k", [128, 32, 128])
    w1v_d = din("w1v", [128, 32, 128])
    pe_d = din("peT", [128, 2, 32])
    cmpb_d = din("cmpb", [128, 2])
    w2k_d = din("w2k", [128, 128])
    w2v_d = din("w2v", [128, 128])
    ident_d = din("ident", [128, 128], BF16)
    ebig_d = din("ebig", [64, S], BF16)
    ovl_d = din("ovl", [128, 2, 64])
    cwide_d = din("cwide", [128, 3, 128])
    cwide_h_d = din("cwide_h", [2, 3, 128])
    wol_d = din("wol", [128, 8, D])
    woa_d = din("woa", [128, 8, D])
    gout_d = din("gout", [128, 2, 8])
    gffn_d = din("gffn_bc", [128, D])
    gfin_d = din("gfin_bc", [128, D])
    wup_d = din("wup", [24, 128, KC, 512])
    wdn_d = din("wdn", [24, 128, 2, D])
    fcw_d = din("fcw", [128, 48, 3])
    fcb_d = din("fcb", [128, 48])
    y_out = nc.dram_tensor("y", [1024, D], F32, kind="ExternalOutput").ap()

    base = (int(nc.sbuf_base) + 63) // 64 * 64
    A = Arena(nc, base, int(nc.sbuf_base) + int(nc.sbuf_bytes_remaining) - 64)
    banks = [nc.alloc_psum_tensor("bank%d" % i, [128, 512], F32) for i in range(8)]
    bk = [Tk("bank%d" % i) for i in range(8)]

    def bf(bank_ap):
        return bank_ap.bitcast(BF16)

    ident = A.alloc("ident", [128, 128], BF16)
    ident_t = Tk("ident")
    P.dma("sp", ident[:, :], ident_d[:, :], w=[ident_t])
    gmixT = A.alloc("gmixT", [128, KC], F32)
    gmixT_t = Tk("gmixT")
    P.dma("sp", gmixT[:, :], gmixT_d[:, :], w=[gmixT_t])
    ones_bf = A.alloc("ones", [128, 2], BF16)
    ones_t = Tk("ones")
    P.op("dve", lambda e: e.memset(ones_bf[:, :], 1.0), w=[ones_t])

    epsb = A.alloc("epsb", [128, 1], F32)
    epsb_t = Tk("epsb")
    P.op("dve", lambda e: e.memset(epsb[:, :], EPS), w=[epsb_t])
    oneb = A.alloc("oneb", [128, 1], F32)
    oneb_t = Tk("oneb")
    P.op("dve", lambda e: e.memset(oneb[:, :], 1.0), w=[oneb_t])
    y_base = A.mark()
    ylruT = A.alloc("ylruT", [128, 8, NOWN], BF16)
    ylru_t = [Tk("ylru%d" % c) for c in range(8)]
    yattT = A.alloc("yattT", [128, 8, NOWN], BF16)
    yatt_t = Tk("yatt")
    y_end = A.mark()

    m_mixer = A.mark()

    def proj_pass(w_dram, ncols, sts, groups, nrot=6):
        m0 = A.mark()
        W = A.alloc("W", [128, KC, ncols], BF16)
        W_t = Tk("W")
        xt = [A.alloc("xt", [128, D], F32) for _ in range(2)]
        xt_t = [Tk("xt0"), Tk("xt1")]
        for k in range(KC):
            P.dma("sp", xt[k % 2][:, 0:ncols], w_dram[:, k, :], w=[xt_t[k % 2]])
            P.ts("dve" if k % 2 == 0 else "pool", W[:, k, :], xt[k % 2][:, 0:ncols], gmixT[:, k:k + 1], None, ALU.mult,
                 r=[xt_t[k % 2], gmixT_t], w=[W_t])
        xn = A.alloc("xn", [128, D], BF16)
        xn_t = Tk("xn")
        xsq = xn
        xsq_t = xn_t
        ss = [A.alloc("ss", [128, 4], F32) for _ in range(2)]
        ss_t = [Tk("ss0"), Tk("ss1")]
        xnT = [A.alloc("xnT", [128, KC, 512], BF16) for _ in range(2)]
        xnT_t = [Tk("xnT0"), Tk("xnT1")]
        ctx = dict(W=W, W_t=W_t)
        for g in groups:
            if "setup" in g:
                g["setup"](ctx)
        rot = [2]
        for si, st in enumerate(sts):
            xb = si % 2
            for tt in range(4):
                tk = st * 4 + tt
                b = tk % 2
                P.dma("sp", xt[b][:, :], x_loc[tk * 128:(tk + 1) * 128, :], w=[xt_t[b]])
                P.act(xsq[:, :], xt[b][:, :], AF.Square, r=[xt_t[b]], w=[xsq_t, ss_t[b]], accum_out=ss[b][:, 0:1])
                P.act(ss[b][:, 1:2], ss[b][:, 0:1], AF.Ln, r=[ss_t[b]], w=[ss_t[b]], scale=1.0 / D, bias=epsb[:, 0:1])
                P.act(ss[b][:, 2:3], ss[b][:, 1:2], AF.Exp, r=[ss_t[b]], w=[ss_t[b]], scale=-0.5)
                P.act(xn[:, :], xt[b][:, :], AF.Copy, r=[xt_t[b], ss_t[b]], w=[xn_t], scale=ss[b][:, 2:3])
                for half in range(2):
                    pb = banks[half]
                    for j in range(8):
                        jj = half * 8 + j
                        P.tr(bf(pb[:, :])[:, j * 128:(j + 1) * 128], xn[:, jj * 128:(jj + 1) * 128], ident[:, :],
                             r=[xn_t, ident_t], w=[bk[half]])
                    P.cp("dve" if half == 0 else "act",
                         xnT[xb][:, half * 8:(half + 1) * 8, tt * 128:(tt + 1) * 128],
                         bf(pb[:, :]).rearrange("p (j t) -> p j t", j=8),
                         r=[bk[half]], w=[xnT_t[xb]])
            for g in groups:
                if st not in g.get("sts", sts):
                    continue
                if g["kind"] == "fm":
                    for j in range(g["ncols"] // 128):
                        bi = rot[0]
                        rot[0] = 2 + (rot[0] - 1) % nrot
                        for k in range(KC):
                            P.mm(banks[bi][:, :], W[:, k, g["col0"] + j * 128: g["col0"] + (j + 1) * 128], xnT[xb][:, k, :],
                                 k == 0, k == KC - 1, r=[W_t, xnT_t[xb]], w=[bk[bi]])
                        g["evac"](st, j, banks[bi], bk[bi])
                else:
                    nco = g["ncols"]
                    for tt in range(4):
                        bi = rot[0]
                        rot[0] = 2 + (rot[0] - 1) % nrot
                        for k in range(KC):
                            P.mm(banks[bi][:, 0:nco], xnT[xb][:, k, tt * 128:(tt + 1) * 128], W[:, k, g["col0"]: g["col0"] + nco],
                                 k == 0, k == KC - 1, r=[W_t, xnT_t[xb]], w=[bk[bi]])
                        g["evac"](st, tt, banks[bi], bk[bi])
        P.barrier()
        A.release(m0)


    def run_lru():
        m0 = A.mark()
        h_own = A.alloc("h_own", [128, 8, NOWN], F32)
        h_own_t = [Tk("h_own%d" % c) for c in range(8)]
        m_l = A.mark()
        lcw = A.alloc("lcw", [128, 8, 4], F32)
        lvec = A.alloc("lvec", [128, 4, 8], F32)
        c12 = A.alloc("c12", [128, 2, 8], F32)
        lsm_t = Tk("lsm")
        P.dma("sp", lcw[:, :, :], lcw_d[:, :, :], w=[lsm_t])
        P.dma("sp", lvec[:, :, :], lvec_d[:, :, :], w=[lsm_t])
        P.act(c12[:, 0, :], lvec[:, 3, :], AF.Exp, r=[lsm_t], w=[lsm_t], scale=-1.0)
        P.act(c12[:, 0, :], c12[:, 0, :], AF.Ln, r=[lsm_t], w=[lsm_t], bias=oneb[:, 0:1])
        P.ts("dve", c12[:, 1, :], c12[:, 0, :], -16.0, None, ALU.mult, r=[lsm_t], w=[lsm_t])
        P.ts("dve", c12[:, 0, :], c12[:, 0, :], -8.0, None, ALU.mult, r=[lsm_t], w=[lsm_t])
        wa = A.alloc("wa", [128, 8, 128], F32)
        wx = A.alloc("wx", [128, 8, 128], F32)
        wax_t = Tk("wax")
        P.dma("sp", wa[:, :, :], wa_d[:, :, :], w=[wax_t])
        P.dma("sp", wx[:, :, :], wx_d[:, :, :], w=[wax_t])
        stf = A.alloc("stf", [128, 8], F32)
        stf_t = Tk("stf")
        P.dma("sp", stf[:, :], stflag_d[:, :], w=[stf_t])
        hin = A.alloc("hin", [128, 8], F32)
        hin_t = [Tk("hin%d" % c) for c in range(8)]
        xbuf = A.alloc("xbuf", [128, 8, 515], F32)
        xbuf_t = [Tk("xbuf%d" % c) for c in range(8)]
        hst = A.alloc("hst", [128, 8], F32)
        hst_t = [Tk("hst%d" % c) for c in range(8)]
        for c in range(8):
            P.op("pool", lambda e, c=c: e.memset(xbuf[:, c, 0:3], 0.0), w=[xbuf_t[c]])
        NB = 2
        tmp = {}
        tmp_t = {}
        for nm in ("u", "r", "i", "a", "s", "v", "h"):
            tmp[nm] = [A.alloc("l" + nm, [128, 512], F32) for _ in range(NB)]
            tmp_t[nm] = [Tk("l%s%d" % (nm, b)) for b in range(NB)]
        gps = [6, 7]

        def evac(st, c, ps, ps_t):
            b = c % NB
            xb = xbuf[:, c, :]
            if st > 0:
                P.cp("dve", xbuf[:, c, 0:3], xbuf[:, c, 512:515], r=[xbuf_t[c]], w=[xbuf_t[c]])
            P.cp("act", xbuf[:, c, 3:515], ps[:, :], r=[ps_t], w=[xbuf_t[c]])
            u = tmp["u"][b]
            ut = tmp_t["u"][b]
            P.ts("dve", u[:, :], xbuf[:, c, 3:515], lcw[:, c, 3:4], lvec[:, 0, c:c + 1], ALU.mult, ALU.add,
                 r=[xbuf_t[c], lsm_t], w=[ut])
            for j in range(3):
                P.stt(u[:, :], xbuf[:, c, j:j + 512], lcw[:, c, j:j + 1], u[:, :], ALU.mult, ALU.add,
                      r=[xbuf_t[c], lsm_t, ut], w=[ut])
            P.mm(banks[gps[0]][:, :], wa[:, c, :], u[:, :], True, True, r=[wax_t, ut], w=[bk[gps[0]]])
            P.mm(banks[gps[1]][:, :], wx[:, c, :], u[:, :], True, True, r=[wax_t, ut], w=[bk[gps[1]]])
            rr, ii, aa, sq, vv = tmp["r"][b], tmp["i"][b], tmp["a"][b], tmp["s"][b], tmp["v"][b]
            P.act(rr[:, :], banks[gps[0]][:, :], AF.Sigmoid, r=[bk[gps[0]], lsm_t], w=[tmp_t["r"][b]], bias=lvec[:, 1, c:c + 1])
            P.act(ii[:, :], banks[gps[1]][:, :], AF.Sigmoid, r=[bk[gps[1]], lsm_t], w=[tmp_t["i"][b]], bias=lvec[:, 2, c:c + 1])
            P.act(aa[:, :], rr[:, :], AF.Exp, r=[tmp_t["r"][b], lsm_t], w=[tmp_t["a"][b]], scale=c12[:, 0, c:c + 1])
            P.act(sq[:, :], rr[:, :], AF.Exp, r=[tmp_t["r"][b], lsm_t], w=[tmp_t["s"][b]], scale=c12[:, 1, c:c + 1])
            P.ts("dve", sq[:, :], sq[:, :], -1.0, 1.0, ALU.mult, ALU.add, r=[tmp_t["s"][b]], w=[tmp_t["s"][b]])
            P.ts("dve", sq[:, :], sq[:, :], 1e-18, None, ALU.max, r=[tmp_t["s"][b]], w=[tmp_t["s"][b]])
            P.act(sq[:, :], sq[:, :], AF.Ln, r=[tmp_t["s"][b]], w=[tmp_t["s"][b]])
            P.act(sq[:, :], sq[:, :], AF.Exp, r=[tmp_t["s"][b]], w=[tmp_t["s"][b]], scale=0.5)
            P.tt("dve", vv[:, :], ii[:, :], u[:, :], ALU.mult, r=[tmp_t["i"][b], ut], w=[tmp_t["v"][b]])
            P.tt("dve", vv[:, :], vv[:, :], sq[:, :], ALU.mult, r=[tmp_t["s"][b], tmp_t["v"][b]], w=[tmp_t["v"][b]])
            if st >= 6:
                hout = h_own[:, c, 2 + (st - 6) * 512: 2 + (st - 5) * 512]
                ht = h_own_t[c]
            else:
                hout = tmp["h"][b][:, :]
                ht = tmp_t["h"][b]
            if st == 0:
                init = 0.0
            else:
                P.tt("dve", hin[:, c:c + 1], hst[:, c:c + 1], stf[:, st - 1:st], ALU.mult, r=[hst_t[c], stf_t], w=[hin_t[c]])
                init = hin[:, c:c + 1]
            P.op("dve", lambda e, hout=hout, aa=aa, vv=vv, init=init: e.tensor_tensor_scan(
                out=hout, data0=aa[:, :], data1=vv[:, :], initial=init, op0=ALU.mult, op1=ALU.add),
                r=[tmp_t["a"][b], tmp_t["v"][b], hin_t[c]], w=[ht])
            P.cp("dve", hst[:, c:c + 1], hout[:, 511:512], r=[ht], w=[hst_t[c]])
            if st == 5:
                P.cp("dve", h_own[:, c, 0:2], hout[:, 510:512], r=[ht], w=[h_own_t[c]])

        proj_pass(wA1_d, 1024, list(range(8)), [dict(kind="fm", col0=0, ncols=1024, evac=evac)], nrot=4)
        A.release(m_l)

        gt = [A.alloc("gt", [128, 512], F32) for _ in range(2)]
        gt_t = [Tk("gt0"), Tk("gt1")]
        gt2 = [A.alloc("gt2", [128, 512], F32) for _ in range(2)]
        gt2_t = [Tk("gt20"), Tk("gt21")]

        def evac_gate(st, c, ps, ps_t):
            b = c % 2
            if st == 5:
                lo, hi, o0 = 510, 512, 0
            else:
                lo, hi, o0 = 0, 512, 2 + (st - 6) * 512
            n = hi - lo
            gelu(gt[b][:, 0:n], ps[:, lo:hi], gt2[b][:, 0:n], [ps_t], gt_t[b], gt2_t[b])
            P.tt("dve", ylruT[:, c, o0:o0 + n], gt[b][:, 0:n], h_own[:, c, o0:o0 + n], ALU.mult,
                 r=[gt_t[b], h_own_t[c]], w=[ylru_t[c]])

        proj_pass(wB3_d, 1024, [5, 6, 7], [dict(kind="fm", col0=0, ncols=1024, evac=evac_gate)])
        A.release(m0)

    def gelu(out, in_, scratch, in_t, out_t, scratch_t):
        if USE_GELU_TANH_LUT:
            P.act(out, in_, AF.Gelu_apprx_tanh, r=in_t, w=[out_t])
            return
        P.act(scratch, in_, AF.Square, r=in_t, w=[scratch_t])
        P.ts("dve", scratch, scratch, 0.044715, 1.0, ALU.mult, ALU.add, r=[scratch_t], w=[scratch_t])
        P.tt("dve", scratch, scratch, in_, ALU.mult, r=[scratch_t] + list(in_t), w=[scratch_t])
        P.act(scratch, scratch, AF.Sigmoid, r=[scratch_t], w=[scratch_t], scale=1.5957691216057308)
        P.tt("dve", out, scratch, in_, ALU.mult, r=[scratch_t] + list(in_t), w=[out_t])


    run_lru()

    kselT = A.alloc("kselT", [128, 2, S], BF16)
    ksel_t = Tk("kselT")
    vsel = A.alloc("vsel", [128, 32, 2, 129], BF16)
    vsel_t = Tk("vsel")
    kcmpT = A.alloc("kcmpT", [128, 2, 256], BF16)
    kcmp_t = Tk("kcmpT")
    Rc = A.alloc("Rc", [128, 2, 2, 193], BF16)
    Rc_t = Tk("Rc")
    validT = A.alloc("validT", [128, 32], F32)
    validT_t = Tk("validT")
    P.dma("sp", validT[:, :], validT_d[:, :], w=[validT_t])
    P.op("dve", lambda e: e.tensor_copy(out=vsel[:, :, 0, 128:129], in_=validT[:, :].unsqueeze(2)), r=[validT_t], w=[vsel_t])
    P.op("dve", lambda e: e.tensor_copy(out=vsel[:, :, 1, 128:129], in_=validT[:, :].unsqueeze(2)), r=[validT_t], w=[vsel_t])
    m_a2 = A.mark()
    kvcT = A.alloc("kvcT", [128, 4, S], BF16)
    kvc_t = Tk("kvcT")

    def evac_a2_fm(st, j, ps, ps_t):
        eng = "act" if j % 2 == 0 else "dve"
        if j < 4:
            P.cp(eng, kvcT[:, j, st * 512:(st + 1) * 512], ps[:, :], r=[ps_t], w=[kvc_t])
        else:
            P.cp(eng, kselT[:, j - 4, st * 512:(st + 1) * 512], ps[:, :], r=[ps_t], w=[ksel_t])

    def evac_a2_v(st, tt, ps, ps_t):
        ch = st * 4 + tt
        P.cp("act" if tt % 2 else "dve", vsel[:, ch, :, 0:128], ps[:, 0:256].rearrange("p (h d) -> p h d", h=2),
             r=[ps_t], w=[vsel_t])

    proj_pass(wA2_d, 1024, list(range(8)),
              [dict(kind="fm", col0=0, ncols=768, evac=evac_a2_fm),
               dict(kind="tm", col0=768, ncols=256, evac=evac_a2_v)])

    def run_compress():
        m0 = A.mark()
        w1 = [A.alloc("w1", [128, 32, 128], BF16) for _ in range(2)]
        w1_t = [Tk("w1k"), Tk("w1v")]
        P.dma("pool", w1[0][:, :, :], w1k_d[:, :, :], w=[w1_t[0]])
        P.dma("pool", w1[1][:, :, :], w1v_d[:, :, :], w=[w1_t[1]])
        w2 = A.alloc("w2", [128, 2, 128], BF16)
        w2_t = Tk("w2")
        P.dma("pool", w2[:, 0, :], w2k_d[:, :], w=[w2_t])
        P.dma("pool", w2[:, 1, :], w2v_d[:, :], w=[w2_t])
        peT = A.alloc("peT", [128, 2, 32], BF16)
        peT_t = Tk("peT")
        P.dma("pool", peT[:, :, :], pe_d[:, :, :], w=[peT_t])
        cb = A.alloc("cb", [128, 4], F32)
        cb_t = Tk("cb")
        P.dma("sp", cb[:, 0:2], cmpb_d[:, :], w=[cb_t])
        vn = A.alloc("vn", [128, 2], F32)
        ovl = A.alloc("ovl", [128, 2, 64], F32)
        vn_t = Tk("vn")
        P.dma("sp", vn[:, :], validn_d[:, :], w=[vn_t])
        P.dma("sp", ovl[:, :, :], ovl_d[:, :, :], w=[vn_t])
        for ty in range(2):
            for l in range(32):
                P.mm(banks[2][:, ty:ty + 1], w1[ty][:, l, :], peT[:, ty, l:l + 1], l == 0, l == 31,
                     r=[w1_t[ty], peT_t], w=[bk[2]])
            P.tt("dve", cb[:, 2 + ty:3 + ty], banks[2][:, ty:ty + 1], cb[:, ty:ty + 1], ALU.add, r=[bk[2], cb_t], w=[cb_t])
        hid = [A.alloc("hid", [128, 256], F32) for _ in range(2)]
        hid_t = [Tk("hid0"), Tk("hid1")]
        hs = [A.alloc("hs", [128, 256], F32) for _ in range(2)]
        hs_t = [Tk("hs0"), Tk("hs1")]
        hp = [A.alloc("hp", [128, 256], F32) for _ in range(2)]
        hp_t = [Tk("hp0"), Tk("hp1")]
        hb = [A.alloc("hb", [128, 256], BF16) for _ in range(2)]
        hb_t = [Tk("hb0"), Tk("hb1")]
        for c2 in range(2):
            for hk in range(2):
                P.cp("dve", Rc[:, c2, hk, 0:1], vn[:, c2:c2 + 1], r=[vn_t], w=[Rc_t])
                P.ts("dve", Rc[:, c2, hk, 1:65], ovl[:, c2, :], vn[:, c2:c2 + 1], None, ALU.mult, r=[vn_t], w=[Rc_t])
        it = 0
        for hk in range(2):
            for ty in range(2):
                b = it % 2
                it += 1
                pb = 3 + b
                for l in range(32):
                    P.mm(banks[pb][:, 0:255], w1[ty][:, l, :], kvcT[:, ty * 2 + hk, l: l + 16 * 254 + 1: 16], l == 0, l == 31,
                         r=[w1_t[ty], kvc_t], w=[bk[pb]])
                P.ts("dve", hp[b][:, 0:255], banks[pb][:, 0:255], cb[:, 2 + ty:3 + ty], None, ALU.add, r=[bk[pb], cb_t], w=[hp_t[b]])
                gelu(hid[b][:, 0:255], hp[b][:, 0:255], hs[b][:, 0:255], [hp_t[b]], hid_t[b], hs_t[b])
                P.cp("dve", hb[b][:, 0:255], hid[b][:, 0:255], r=[hid_t[b]], w=[hb_t[b]])
                if ty == 0:
                    P.mm(banks[5][:, 0:255], w2[:, 0, :], hb[b][:, 0:255], True, True, r=[w2_t, hb_t[b]], w=[bk[5]])
                    P.cp("act", kcmpT[:, hk, 0:255], banks[5][:, 0:255], r=[bk[5]], w=[kcmp_t])
                else:
                    for c2 in range(2):
                        rows = 128 if c2 == 0 else 127
                        P.mm(banks[6 + c2][0:rows, 0:128], hb[b][:, c2 * 128: c2 * 128 + rows], w2[:, 1, :], True, True,
                             r=[w2_t, hb_t[b]], w=[bk[6 + c2]])
                        P.ts("dve", Rc[0:rows, c2, hk, 65:193], banks[6 + c2][0:rows, 0:128], vn[0:rows, c2:c2 + 1], None, ALU.mult,
                             r=[bk[6 + c2], vn_t], w=[Rc_t])
        P.barrier()
        A.release(m0)

    run_compress()
    A.release(m_a2)

    kwT = A.alloc("kwT", [128, 2, 2048], BF16)
    kw_t = Tk("kwT")
    vwin = A.alloc("vwin", [128, 16, 2, 129], BF16)
    vwin_t = Tk("vwin")
    gsig = A.alloc("gsig", [128, 9, 24], F32)
    gsig_t = Tk("gsig")
    qT = A.alloc("qT", [128, 8, NOWN], BF16)
    qT_t = Tk("qT")
    P.op("dve", lambda e: e.tensor_copy(out=vwin[:, :, 0, 128:129], in_=validT[:, 16:32].unsqueeze(2)), r=[validT_t], w=[vwin_t])
    P.op("dve", lambda e: e.tensor_copy(out=vwin[:, :, 1, 128:129], in_=validT[:, 16:32].unsqueeze(2)), r=[validT_t], w=[vwin_t])
    def evac_b1_k(st, j, ps, ps_t):
        P.cp("act" if j else "dve", kwT[:, j, (st - 4) * 512:(st - 3) * 512], ps[:, :], r=[ps_t], w=[kw_t])

    def evac_b1_v(st, tt, ps, ps_t):
        ch = (st - 4) * 4 + tt
        P.cp("dve", vwin[:, ch, :, 0:128], ps[:, 0:256].rearrange("p (h d) -> p h d", h=2), r=[ps_t], w=[vwin_t])
        if st >= 6:
            ti = 1 + (st - 6) * 4 + tt
            P.act(gsig[:, ti, :], ps[:, 256:280], AF.Sigmoid, r=[ps_t], w=[gsig_t])

    halo_ctx = {}

    proj_pass(wB1_d, 536, [4, 5, 6, 7],
              [dict(kind="fm", col0=0, ncols=256, evac=evac_b1_k),
               dict(kind="tm", col0=256, ncols=280, evac=evac_b1_v)])

    def setup_b2(ctx):
        halo_ctx.update(ctx)

    def evac_b2(st, j, ps, ps_t):
        if st == 5:
            lo, hi, o0 = 510, 512, 0
        else:
            lo, hi, o0 = 0, 512, 2 + (st - 6) * 512
        P.act(qT[:, j, o0:o0 + hi - lo], ps[:, lo:hi], AF.Copy, r=[ps_t], w=[qT_t], scale=float(128 ** -0.5))

    proj_pass(wB2_d, 1024, [5, 6, 7], [dict(kind="fm", col0=0, ncols=1024, evac=evac_b2)])

    def halo_gates():
        m0 = A.mark()
        Wg = A.alloc("Wg", [128, KC, 24], BF16)
        Wg_t = Tk("Wg")
        sg = A.alloc("sg", [128, KC, 24], F32)
        sg_t = Tk("sg")
        P.dma("sp", sg[:, :, :], wB1_d[:, :, 512:536], w=[sg_t])
        for k in range(KC):
            P.ts("dve", Wg[:, k, :], sg[:, k, :], gmixT[:, k:k + 1], None, ALU.mult, r=[sg_t, gmixT_t], w=[Wg_t])
        xh = A.alloc("xh", [2, D], F32)
        xh_t = Tk("xh")
        P.dma("sp", xh[:, :], x_loc[3070:3072, :], w=[xh_t])
        xq = A.alloc("xq", [2, D], BF16)
        xq_t = Tk("xq")
        sh = A.alloc("sh", [2, 4], F32)
        sh_t = Tk("sh")
        P.act(xq[:, :], xh[:, :], AF.Square, r=[xh_t], w=[xq_t, sh_t], accum_out=sh[:, 0:1])
        P.act(sh[:, 1:2], sh[:, 0:1], AF.Ln, r=[sh_t], w=[sh_t], scale=1.0 / D, bias=epsb[0:2, 0:1])
        P.act(sh[:, 2:3], sh[:, 1:2], AF.Exp, r=[sh_t], w=[sh_t], scale=-0.5)
        P.act(xq[:, :], xh[:, :], AF.Copy, r=[xh_t, sh_t], w=[xq_t], scale=sh[:, 2:3])
        xqT = A.alloc("xqT", [128, KC, 2], BF16)
        xqT_t = Tk("xqT")
        for j in range(KC):
            P.tr(bf(banks[0][:, :])[:, j * 2:(j + 1) * 2], xq[:, j * 128:(j + 1) * 128], ident[0:2, 0:2], r=[xq_t, ident_t], w=[bk[0]])
        P.cp("dve", xqT[:, :, :], bf(banks[0][:, :])[:, 0:32].rearrange("p (j t) -> p j t", j=KC), r=[bk[0]], w=[xqT_t])
        for k in range(KC):
            P.mm(banks[2][0:2, 0:24], xqT[:, k, :], Wg[:, k, :], k == 0, k == KC - 1, r=[xqT_t, Wg_t], w=[bk[2]])
        P.act(gsig[0:2, 0, :], banks[2][0:2, 0:24], AF.Sigmoid, r=[bk[2]], w=[gsig_t])
        P.barrier()
        A.release(m0)

    halo_gates()

    def run_attention():
        m0 = A.mark()
        ebig = A.alloc("ebig", [64, S], BF16)
        cw = A.alloc("cw", [128, 3, 128], F32)
        cwh = A.alloc("cwh", [2, 3, 128], F32)
        f0 = A.alloc("f0", [128, 64], F32)
        cst_t = Tk("attn_consts")
        P.dma("sp", ebig[:, :], ebig_d[:, :], w=[cst_t])
        P.dma("sp", cw[:, :, :], cwide_d[:, :, :], w=[cst_t])
        P.dma("sp", cwh[:, :, :], cwide_h_d[:, :, :], w=[cst_t])
        P.dma("sp", f0[:, :], f0_d[:, :], w=[cst_t])
        NE = 3
        Et = [A.alloc("E", [128, 4, 128], BF16) for _ in range(NE)]
        Et_t = [Tk("E%d" % i) for i in range(NE)]
        Pt = [A.alloc("Pm", [128, 4, 128], BF16) for _ in range(NE)]
        Pt_t = [Tk("Pm%d" % i) for i in range(NE)]
        Ec = [A.alloc("Ec", [128, 4, 128], BF16) for _ in range(2)]
        Ec_t = [Tk("Ec0"), Tk("Ec1")]
        sm = A.alloc("sm", [128, 64], F32)
        sm_t = Tk("sm")
        imp = A.alloc("imp", [128, 64], F32)
        sc2 = A.alloc("sc2", [128, 64], F32)
        top = A.alloc("top", [128, 16], F32)
        selb = A.alloc("selb", [128, 64], BF16)
        selT = A.alloc("selT", [64, 128], BF16)
        tk_t = Tk("topk")
        selT_t = Tk("selT")
        acc = A.alloc("acc", [128, 4, 128], F32)
        acc_t = Tk("acc")
        ob = A.alloc("ob", [128, 4, 128], BF16)
        ob_t = Tk("ob")
        ectr = [0]
        PVB = [3, 4, 5, 6]

        def qblock(Dq, q0, nq, qc, gti):
            cwm = cwh if nq == 2 else cw
            lo = 64 - 2 * Dq
            for hk in range(2):
                qv = qT[:, 4 * hk:4 * hk + 4, qc:qc + nq]
                for c2 in range(2):
                    rows = 128 if c2 == 0 else 127
                    sb = c2
                    P.mm(banks[sb][0:rows, 0:4 * nq].rearrange("p (g q) -> p g q", g=4), kcmpT[:, hk, c2 * 128:c2 * 128 + rows], qv,
                         True, True, r=[kcmp_t, qT_t], w=[bk[sb]])
                    P.act(Ec[c2][0:rows, :, 0:nq], banks[sb][0:rows, 0:4 * nq].rearrange("p (g q) -> p g q", g=4), AF.Exp,
                          r=[bk[sb]], w=[Ec_t[c2]])
                    basev = 128 * Dq + q0 - 31 - 2048 * c2
                    P.op("pool", lambda e, c2=c2, rows=rows, basev=basev: e.affine_select(
                        out=Ec[c2][0:rows, :, 0:nq], in_=Ec[c2][0:rows, :, 0:nq], pattern=[[0, 4], [1, nq]],
                        compare_op=ALU.is_ge, fill=0.0, base=basev, channel_multiplier=-16), r=[Ec_t[c2]], w=[Ec_t[c2]])
                for g in range(4):
                    pb = PVB[g // 2]
                    co = (g % 2) * 193
                    for c2 in range(2):
                        rows = 128 if c2 == 0 else 127
                        P.mm(banks[pb][0:nq, co:co + 193], Ec[c2][0:rows, g, 0:nq], Rc[0:rows, c2, hk, :], c2 == 0, c2 == 1,
                             r=[Ec_t[c2], Rc_t], w=[bk[pb]])
                for g in range(4):
                    pb = PVB[g // 2]
                    co = (g % 2) * 193
                    P.ts("dve", sm[0:nq, g:g + 1], banks[pb][0:nq, co:co + 1], 1e-30, None, ALU.max, r=[bk[pb]], w=[sm_t])
                P.op("dve", lambda e: e.reciprocal(out=sm[0:nq, 4:8], in_=sm[0:nq, 0:4]), r=[sm_t], w=[sm_t])
                P.tt("dve", sm[0:nq, 8:12], sm[0:nq, 4:8], gsig[0:nq, gti, 12 * hk:12 * hk + 10:3], ALU.mult, r=[sm_t, gsig_t], w=[sm_t])
                for g in range(4):
                    pb = PVB[g // 2]
                    co = (g % 2) * 193
                    if g == 0:
                        P.ts("dve", imp[0:nq, :], banks[pb][0:nq, co + 1:co + 65], sm[0:nq, 4:5], None, ALU.mult, r=[bk[pb], sm_t], w=[tk_t])
                    else:
                        P.stt(imp[0:nq, :], banks[pb][0:nq, co + 1:co + 65], sm[0:nq, 4 + g:5 + g], imp[0:nq, :], ALU.mult, ALU.add,
                              r=[bk[pb], sm_t, tk_t], w=[tk_t])
                    P.ts("dve", acc[0:nq, g, :], banks[pb][0:nq, co + 65:co + 193], sm[0:nq, 8 + g:9 + g], None, ALU.mult,
                         r=[bk[pb], sm_t], w=[acc_t])
                P.tt("dve", imp[0:nq, :], imp[0:nq, :], cwm[0:nq, 0, lo:lo + 64], ALU.mult, r=[tk_t, cst_t], w=[tk_t])
                P.tt("dve", imp[0:nq, :], imp[0:nq, :], cwm[0:nq, 1, lo:lo + 64], ALU.add, r=[tk_t, cst_t], w=[tk_t])
                P.tt("dve", imp[0:nq, :], imp[0:nq, :], cwm[0:nq, 2, lo:lo + 64], ALU.max, r=[tk_t, cst_t], w=[tk_t])
                P.tt("dve", imp[0:nq, :], imp[0:nq, :], f0[0:nq, :], ALU.max, r=[tk_t, cst_t], w=[tk_t])
                P.op("dve", lambda e: e.max(out=top[0:nq, 0:8], in_=imp[0:nq, :]), r=[tk_t], w=[tk_t])
                P.op("dve", lambda e: e.match_replace(out=sc2[0:nq, :], in_to_replace=top[0:nq, 0:8], in_values=imp[0:nq, :],
                                                      imm_value=-1e9), r=[tk_t], w=[tk_t])
                P.op("dve", lambda e: e.max(out=top[0:nq, 8:16], in_=sc2[0:nq, :]), r=[tk_t], w=[tk_t])
                P.ts("dve", sc2[0:nq, :], imp[0:nq, :], top[0:nq, 15:16], None, ALU.is_ge, r=[tk_t], w=[tk_t])
                P.tt("dve", selb[0:nq, :], sc2[0:nq, :], cwm[0:nq, 0, lo:lo + 64], ALU.mult, r=[tk_t, cst_t], w=[tk_t])
                P.tr(bf(banks[2][:, :])[0:64, 512:512 + nq], selb[0:nq, :], ident[0:nq, 0:nq], r=[tk_t, ident_t], w=[bk[2]])
                P.cp("dve", selT[:, 0:nq], bf(banks[2][:, :])[0:64, 512:512 + nq], r=[bk[2]], w=[selT_t])
                for kc in range(Dq + 1):
                    sb = kc % 2
                    ei = ectr[0] % NE
                    ectr[0] += 1
                    P.mm(banks[sb][:, 0:4 * nq].rearrange("p (g q) -> p g q", g=4), kselT[:, hk, kc * 128:(kc + 1) * 128], qv,
                         True, True, r=[ksel_t, qT_t], w=[bk[sb]])
                    mo = (kc % 2) * 128
                    P.mm(banks[2][:, mo:mo + nq], ebig[:, kc * 128:(kc + 1) * 128], selT[:, 0:nq], True, True,
                         r=[cst_t, selT_t], w=[bk[2]])
                    P.act(Et[ei][:, :, 0:nq], banks[sb][:, 0:4 * nq].rearrange("p (g q) -> p g q", g=4), AF.Exp,
                          r=[bk[sb]], w=[Et_t[ei]])
                    P.tt("dve", Pt[ei][:, :, 0:nq], Et[ei][:, :, 0:nq], banks[2][:, mo:mo + nq].unsqueeze(1).broadcast_to([128, 4, nq]), ALU.mult,
                         r=[Et_t[ei], bk[2]], w=[Pt_t[ei]])
                    if kc == Dq:
                        P.op("pool", lambda e, ei=ei: e.affine_select(
                            out=Pt[ei][:, :, 0:nq], in_=Pt[ei][:, :, 0:nq], pattern=[[0, 4], [1, nq]],
                            compare_op=ALU.is_ge, fill=0.0, base=q0, channel_multiplier=-1), r=[Pt_t[ei]], w=[Pt_t[ei]])
                    for g in range(4):
                        P.mm(banks[PVB[g]][0:nq, 0:129], Pt[ei][:, g, 0:nq], vsel[:, kc, hk, :], kc == 0, kc == Dq,
                             r=[Pt_t[ei], vsel_t], w=[bk[PVB[g]]])
                for g in range(4):
                    P.ts("dve", sm[0:nq, 16 + g:17 + g], banks[PVB[g]][0:nq, 128:129], 1e-30, None, ALU.max, r=[bk[PVB[g]]], w=[sm_t])
                P.op("dve", lambda e: e.reciprocal(out=sm[0:nq, 20:24], in_=sm[0:nq, 16:20]), r=[sm_t], w=[sm_t])
                P.tt("dve", sm[0:nq, 24:28], sm[0:nq, 20:24], gsig[0:nq, gti, 12 * hk + 1:12 * hk + 11:3], ALU.mult, r=[sm_t, gsig_t], w=[sm_t])
                for g in range(4):
                    P.stt(acc[0:nq, g, :], banks[PVB[g]][0:nq, 0:128], sm[0:nq, 24 + g:25 + g], acc[0:nq, g, :], ALU.mult, ALU.add,
                          r=[bk[PVB[g]], sm_t, acc_t], w=[acc_t])
                for kc in range(Dq - 4, Dq + 1):
                    sb = kc % 2
                    ei = ectr[0] % NE
                    ectr[0] += 1
                    wc = kc - 16
                    P.mm(banks[sb][:, 0:4 * nq].rearrange("p (g q) -> p g q", g=4), kwT[:, hk, wc * 128:(wc + 1) * 128], qv,
                         True, True, r=[kw_t, qT_t], w=[bk[sb]])
                    P.act(Et[ei][:, :, 0:nq], banks[sb][:, 0:4 * nq].rearrange("p (g q) -> p g q", g=4), AF.Exp,
                          r=[bk[sb]], w=[Et_t[ei]])
                    if kc == Dq:
                        P.op("pool", lambda e, ei=ei: e.affine_select(
                            out=Et[ei][:, :, 0:nq], in_=Et[ei][:, :, 0:nq], pattern=[[0, 4], [1, nq]],
                            compare_op=ALU.is_ge, fill=0.0, base=q0, channel_multiplier=-1), r=[Et_t[ei]], w=[Et_t[ei]])
                    if kc == Dq - 4:
                        P.op("pool", lambda e, ei=ei: e.affine_select(
                            out=Et[ei][:, :, 0:nq], in_=Et[ei][:, :, 0:nq], pattern=[[0, 4], [-1, nq]],
                            compare_op=ALU.is_ge, fill=0.0, base=-q0 - 1, channel_multiplier=1), r=[Et_t[ei]], w=[Et_t[ei]])
                    for g in range(4):
                        P.mm(banks[PVB[g]][0:nq, 0:129], Et[ei][:, g, 0:nq], vwin[:, wc, hk, :], kc == Dq - 4, kc == Dq,
                             r=[Et_t[ei], vwin_t], w=[bk[PVB[g]]])
                for g in range(4):
                    P.ts("dve", sm[0:nq, 32 + g:33 + g], banks[PVB[g]][0:nq, 128:129], 1e-30, None, ALU.max, r=[bk[PVB[g]]], w=[sm_t])
                P.op("dve", lambda e: e.reciprocal(out=sm[0:nq, 36:40], in_=sm[0:nq, 32:36]), r=[sm_t], w=[sm_t])
                P.tt("dve", sm[0:nq, 40:44], sm[0:nq, 36:40], gsig[0:nq, gti, 12 * hk + 2:12 * hk + 12:3], ALU.mult, r=[sm_t, gsig_t], w=[sm_t])
                for g in range(4):
                    P.stt(ob[0:nq, g, :], banks[PVB[g]][0:nq, 0:128], sm[0:nq, 40 + g:41 + g], acc[0:nq, g, :], ALU.mult, ALU.add,
                          r=[bk[PVB[g]], sm_t, acc_t], w=[ob_t])
                for g in range(4):
                    P.tr(bf(banks[7][:, :])[:, g * 128:g * 128 + nq], ob[0:nq, g, :], ident[0:nq, 0:nq], r=[ob_t, ident_t], w=[bk[7]])
                P.cp("act", yattT[:, 4 * hk:4 * hk + 4, qc:qc + nq],
                     bf(banks[7][:, :])[:, 0:512].rearrange("p (g q) -> p g q", g=4)[:, :, 0:nq], r=[bk[7]], w=[yatt_t])

        qblock(23, 126, 2, 0, 0)
        for i in range(8):
            qblock(24 + i, 0, 128, 2 + 128 * i, 1 + i)
        P.barrier()
        A.release(m0)

    run_attention()
    P.barrier()
    A.release(m_mixer)

    def run_ffn():
        gout = A.alloc("gout", [128, 2, 8], F32)
        gout_t = Tk("gout")
        P.dma("sp", gout[:, :, :], gout_d[:, :, :], w=[gout_t])
        gffn = A.alloc("gffn", [128, D], F32)
        gffn_t = Tk("gffn")
        P.dma("sp", gffn[:, :], gffn_d[:, :], w=[gffn_t])
        fcw = A.alloc("fcw", [128, 48, 3], F32)
        fcb = A.alloc("fcb", [128, 48], F32)
        fc_t = Tk("fc")
        P.dma("sp", fcw[:, :, :], fcw_d[:, :, :], w=[fc_t])
        P.dma("sp", fcb[:, :], fcb_d[:, :], w=[fc_t])
        h = A.alloc("h", [128, 8, D], F32)
        h_t = [Tk("h%d" % i) for i in range(8)]
        hh = A.alloc("hh", [2, D], F32)
        hh_t = Tk("hh")
        xnT = A.alloc("xnTf", [128, KC, NOWN], BF16)
        xnT_t = Tk("xnTf")
        m1 = A.mark()
        rs = A.alloc("rs", [128, 9, 8], F32)
        rs_t = Tk("rs")
        xnb = A.alloc("xnb", [128, D], BF16)
        xnb_t = Tk("xnb")
        ysq = [A.alloc("ysq", [128, 16, 128], BF16) for _ in range(2)]
        ysq_t = [Tk("ysq0"), Tk("ysq1")]
        wo = [A.alloc("wo", [128, 16, 512], BF16) for _ in range(2)]
        wo_t = [Tk("wo0"), Tk("wo1")]
        tiles = [(0, 2, hh[:, :], hh_t, 3070)] + [(2 + 128 * i, 128, h[:, i, :], h_t[i], 3072 + 128 * i) for i in range(8)]
        for ti, (c0, nt, hap, hapt, u0) in enumerate(tiles):
            P.dma("sp", hap, x_loc[u0:u0 + nt, :], w=[hapt])
            yb = ti % 2
            P.tt("pool", ysq[yb][:, 0:8, 0:nt], ylruT[:, :, c0:c0 + nt], ylruT[:, :, c0:c0 + nt], ALU.mult, r=ylru_t, w=[ysq_t[yb]])
            P.tt("dve", ysq[yb][:, 8:16, 0:nt], yattT[:, :, c0:c0 + nt], yattT[:, :, c0:c0 + nt], ALU.mult, r=[yatt_t], w=[ysq_t[yb]])
            for br in range(2):
                for c in range(8):
                    P.mm(banks[0][0:nt, br:br + 1], ysq[yb][:, br * 8 + c, 0:nt], ones_bf[:, 0:1], c == 0, c == 7,
                         r=[ysq_t[yb], ones_t], w=[bk[0]])
            P.act(rs[0:nt, ti, 0:2], banks[0][0:nt, 0:2], AF.Ln, r=[bk[0]], w=[rs_t], scale=1.0 / 1024, bias=epsb[0:nt, 0:1])
            P.act(rs[0:nt, ti, 2:4], rs[0:nt, ti, 0:2], AF.Exp, r=[rs_t], w=[rs_t], scale=-0.5)
        for c in range(8):
            P.ts("pool", ylruT[:, c, :], ylruT[:, c, :], gout[:, 0, c:c + 1], None, ALU.mult, r=[gout_t] + ysq_t, w=[ylru_t[c]])
            P.ts("dve", yattT[:, c, :], yattT[:, c, :], gout[:, 1, c:c + 1], None, ALU.mult, r=[gout_t] + ysq_t, w=[yatt_t])
        for obk in range(4):
            wb = obk % 2
            for c in range(8):
                P.dma("pool", wo[wb][:, c, :], wol_d[:, c, obk * 512:(obk + 1) * 512], w=[wo_t[wb]])
                P.dma("pool", wo[wb][:, 8 + c, :], woa_d[:, c, obk * 512:(obk + 1) * 512], w=[wo_t[wb]])
            for ti, (c0, nt, hap, hapt, u0) in enumerate(tiles):
                for br in range(2):
                    pb = 2 + (ti % 2) * 2 + br
                    ysrc = ylruT if br == 0 else yattT
                    for c in range(8):
                        P.mm(banks[pb][0:nt, :], ysrc[:, c, c0:c0 + nt], wo[wb][:, br * 8 + c, :], c == 0, c == 7,
                             r=[ylru_t[c] if br == 0 else yatt_t, wo_t[wb]], w=[bk[pb]])
                    P.stt(hap[:, obk * 512:(obk + 1) * 512], banks[pb][0:nt, :], rs[0:nt, ti, 2 + br:3 + br],
                          hap[:, obk * 512:(obk + 1) * 512], ALU.mult, ALU.add, r=[bk[pb], rs_t, hapt], w=[hapt])
        for ti, (c0, nt, hap, hapt, u0) in enumerate(tiles):
            P.act(xnb[0:nt, :], hap, AF.Square, r=[hapt], w=[xnb_t, rs_t], accum_out=rs[0:nt, ti, 4:5])
            P.act(rs[0:nt, ti, 5:6], rs[0:nt, ti, 4:5], AF.Ln, r=[rs_t], w=[rs_t], scale=1.0 / D, bias=epsb[0:nt, 0:1])
            P.act(rs[0:nt, ti, 6:7], rs[0:nt, ti, 5:6], AF.Exp, r=[rs_t], w=[rs_t], scale=-0.5)
            P.stt(xnb[0:nt, :], hap, rs[0:nt, ti, 6:7], gffn[0:nt, :], ALU.mult, ALU.mult, r=[hapt, rs_t, gffn_t], w=[xnb_t])
            for half in range(2):
                for j in range(8):
                    jj = half * 8 + j
                    P.tr(bf(banks[6 + half][:, :])[:, j * 128:j * 128 + nt],
                         xnb[0:nt, jj * 128:(jj + 1) * 128], ident[0:nt, 0:nt], r=[xnb_t, ident_t], w=[bk[6 + half]])
                P.cp("dve" if half == 0 else "act", xnT[:, half * 8:(half + 1) * 8, c0:c0 + nt],
                     bf(banks[6 + half][:, :]).rearrange("p (j t) -> p j t", j=8)[:, :, 0:nt], r=[bk[6 + half]], w=[xnT_t])
        P.barrier()
        A.release(m1)
        wup = [nc.alloc_sbuf_tensor_at("wup%d" % i, [128, KC, 512], BF16, offset=y_base + i * 16384) for i in range(2)]
        assert y_base + 2 * 16384 <= y_end
        wup_t = [Tk("wup0"), Tk("wup1")]
        m2 = A.mark()
        wdn = [A.alloc("wdn", [128, 2, D], BF16) for _ in range(2)]
        wdn_t = [Tk("wdn0"), Tk("wdn1")]
        gT = [A.alloc("gT", [128, 2, 1024], BF16) for _ in range(2)]
        gT_t = [Tk("gT0"), Tk("gT1")]
        usb = [A.alloc("usb", [128, NOWN], F32) for _ in range(2)]
        usb_t = [Tk("usb0"), Tk("usb1")]
        vsb = [A.alloc("vsb", [128, NOWN], F32) for _ in range(2)]
        vsb_t = [Tk("vsb0"), Tk("vsb1")]
        uc = [A.alloc("uc", [128, 1024], F32) for _ in range(2)]
        uc_t = [Tk("uc0"), Tk("uc1")]
        gg = [A.alloc("gg", [128, 1024], F32) for _ in range(2)]
        gg_t = [Tk("gg0"), Tk("gg1")]
        gs, gs_t = gg, gg_t
        CT = 342
        rotu = [0]
        P.dma("pool", wup[0][:, :, :], wup_d[0, :, :, :], w=[wup_t[0]])
        P.dma("pool", wdn[0][:, :, :], wdn_d[0, :, :, :], w=[wdn_t[0]])
        for G in range(24):
            gb = G % 2
            if G + 1 < 24:
                P.dma("pool", wup[1 - gb][:, :, :], wup_d[G + 1, :, :, :], w=[wup_t[1 - gb]])
                P.dma("pool", wdn[1 - gb][:, :, :], wdn_d[G + 1, :, :, :], w=[wdn_t[1 - gb]])
            for fc in range(2):
                fa = 2 * G + fc
                ub = fa % 2
                for ct in range(3):
                    pu = rotu[0] % 2
                    rotu[0] += 1
                    bu, bv = pu * 2, pu * 2 + 1
                    for k in range(KC):
                        P.mm(banks[bu][:, 0:CT], wup[gb][:, k, fc * 128:(fc + 1) * 128], xnT[:, k, ct * CT:(ct + 1) * CT], k == 0, k == KC - 1,
                             r=[wup_t[gb], xnT_t], w=[bk[bu]])
                    for k in range(KC):
                        P.mm(banks[bv][:, 0:CT], wup[gb][:, k, 256 + fc * 128:256 + (fc + 1) * 128], xnT[:, k, ct * CT:(ct + 1) * CT], k == 0, k == KC - 1,
                             r=[wup_t[gb], xnT_t], w=[bk[bv]])
                    P.cp("act", usb[ub][:, ct * CT:(ct + 1) * CT], banks[bu][:, 0:CT], r=[bk[bu]], w=[usb_t[ub]])
                    P.cp("act", vsb[ub][:, ct * CT:(ct + 1) * CT], banks[bv][:, 0:CT], r=[bk[bv]], w=[vsb_t[ub]])
                P.ts("dve", uc[ub][:, :], usb[ub][:, 2:1026], fcw[:, fa, 2:3], fcb[:, fa:fa + 1], ALU.mult, ALU.add, r=[usb_t[ub], fc_t], w=[uc_t[ub]])
                P.stt(uc[ub][:, :], usb[ub][:, 1:1025], fcw[:, fa, 1:2], uc[ub][:, :], ALU.mult, ALU.add, r=[usb_t[ub], fc_t, uc_t[ub]], w=[uc_t[ub]])
                P.stt(uc[ub][:, :], usb[ub][:, 0:1024], fcw[:, fa, 0:1], uc[ub][:, :], ALU.mult, ALU.add, r=[usb_t[ub], fc_t, uc_t[ub]], w=[uc_t[ub]])
                gelu(gg[ub][:, :], uc[ub][:, :], gs[ub][:, :], [uc_t[ub]], gg_t[ub], gs_t[ub])
                P.tt("pool", gT[gb][:, fc, :], gg[ub][:, :], vsb[ub][:, 2:1026], ALU.mult, r=[gg_t[ub], vsb_t[ub]], w=[gT_t[gb]])
            for tt in range(8):
                for obk in range(4):
                    pb = 4 + (tt * 4 + obk) % 4
                    for fc in range(2):
                        P.mm(banks[pb][:, :], gT[gb][:, fc, tt * 128:(tt + 1) * 128], wdn[gb][:, fc, obk * 512:(obk + 1) * 512], fc == 0, fc == 1,
                             r=[gT_t[gb], wdn_t[gb]], w=[bk[pb]])
                    P.tt("dve", h[:, tt, obk * 512:(obk + 1) * 512], banks[pb][:, :], h[:, tt, obk * 512:(obk + 1) * 512], ALU.add,
                         r=[bk[pb], h_t[tt]], w=[h_t[tt]])
        P.barrier()
        A.release(m2)
        gfin = A.alloc("gfin", [128, D], F32)
        gfin_t = Tk("gfin")
        P.dma("sp", gfin[:, :], gfin_d[:, :], w=[gfin_t])
        fs = A.alloc("fs", [128, 8, 4], F32)
        fs_t = Tk("fs")
        xs2 = A.alloc("xs2", [128, D], BF16)
        xs2_t = Tk("xs2")
        for tt in range(8):
            P.act(xs2[:, :], h[:, tt, :], AF.Square, r=[h_t[tt]], w=[xs2_t, fs_t], accum_out=fs[:, tt, 0:1])
            P.act(fs[:, tt, 1:2], fs[:, tt, 0:1], AF.Ln, r=[fs_t], w=[fs_t], scale=1.0 / D, bias=epsb[:, 0:1])
            P.act(fs[:, tt, 2:3], fs[:, tt, 1:2], AF.Exp, r=[fs_t], w=[fs_t], scale=-0.5)
            P.stt(h[:, tt, :], h[:, tt, :], fs[:, tt, 2:3], gfin[:, :], ALU.mult, ALU.mult, r=[h_t[tt], fs_t, gfin_t], w=[h_t[tt]])
            P.dma("sp", y_out[tt * 128:(tt + 1) * 128, :], h[:, tt, :], r=[h_t[tt]], is_output=True)

    run_ffn()
    P.finish()
    return nc


def _tile_k(w):
    n = w.shape[1]
    return np.ascontiguousarray(w.reshape(KC, 128, n).transpose(1, 0, 2))


def _host_consts():
    c = {}
    c["ident"] = np.eye(128, dtype=np.float32).astype(ml_dtypes.bfloat16)
    eb = np.zeros((64, S), np.float32)
    for s in range(64):
        eb[s, s * 64:(s + 1) * 64] = 1.0
    c["ebig"] = eb.astype(ml_dtypes.bfloat16)
    n = np.arange(256)
    s = np.arange(64)
    ov = np.clip(np.minimum(n[:, None] * 16 + 32, s[None, :] * 64 + 64) - np.maximum(n[:, None] * 16, s[None, :] * 64), 0, None)
    ov = (ov / 32.0).astype(np.float32)
    ov[255] = 0.0
    c["ovl"] = np.ascontiguousarray(ov.reshape(2, 128, 64).transpose(1, 0, 2))

    def wide(hi):
        rel = np.arange(128)[None, :] - 64
        causal = (rel <= hi[:, None]).astype(np.float32)
        forced = ((rel <= hi[:, None]) & (rel > hi[:, None] - 2)).astype(np.float32)
        return np.ascontiguousarray(np.stack([causal, causal - 1.0, forced * 1e4], axis=1).astype(np.float32))

    c["cwide"] = wide((np.arange(128) >= 64).astype(np.int64))
    c["cwide_h"] = wide(np.array([1, 1], np.int64))
    return c


_NC_CACHE = {}


def kernel(**inp):
    f32 = np.float32
    g = {k: np.asarray(v, dtype=f32) for k, v in inp.items()}
    x = g["x"]
    w_in = g["w_in"][0]
    rep = {}
    rep["wA1"] = _tile_k(w_in[:, 0:1024])
    rep["wB3"] = _tile_k(w_in[:, 1024:2048])
    rep["wB2"] = _tile_k(w_in[:, 2048:3072])
    rep["wA2"] = _tile_k(np.concatenate([w_in[:, 3072:3584], w_in[:, 3584:3840], w_in[:, 3840:4096]], axis=1))
    rep["wB1"] = _tile_k(np.concatenate([w_in[:, 4096:4352], w_in[:, 4352:4608], w_in[:, 4608:4632]], axis=1))
    rep["gmixT"] = np.ascontiguousarray(g["g_mix"][0].reshape(KC, 128).T)
    rep["lcw"] = np.ascontiguousarray(g["lru_conv_w"][0].reshape(4, 8, 128).transpose(2, 1, 0))
    rep["lvec"] = np.ascontiguousarray(np.stack([g["lru_conv_b"][0], g["lru_ba"][0], g["lru_bx"][0], g["lru_lambda"][0]], 0)
                                       .reshape(4, 8, 128).transpose(2, 0, 1))
    rep["wa"] = np.ascontiguousarray(g["lru_wa"][0].transpose(1, 0, 2))
    rep["wx"] = np.ascontiguousarray(g["lru_wx"][0].transpose(1, 0, 2))
    rep["w1k"] = np.ascontiguousarray(g["cmp_w1_k"][0].reshape(32, 128, 128).transpose(1, 0, 2))
    rep["w1v"] = np.ascontiguousarray(g["cmp_w1_v"][0].reshape(32, 128, 128).transpose(1, 0, 2))
    rep["peT"] = np.ascontiguousarray(np.stack([g["cmp_pe_k"][0].T, g["cmp_pe_v"][0].T], axis=1))
    rep["cmpb"] = np.ascontiguousarray(np.stack([g["cmp_b1_k"][0], g["cmp_b1_v"][0]], axis=1))
    rep["w2k"] = np.ascontiguousarray(g["cmp_w2_k"][0])
    rep["w2v"] = np.ascontiguousarray(g["cmp_w2_v"][0])
    w_out = g["w_out"][0]
    rep["wol"] = np.ascontiguousarray(w_out[0:1024].reshape(8, 128, D).transpose(1, 0, 2))
    rep["woa"] = np.ascontiguousarray(w_out[1024:2048].reshape(8, 128, D).transpose(1, 0, 2))
    rep["gout"] = np.ascontiguousarray(np.stack([g["g_lru_out"][0].reshape(8, 128).T, g["g_attn_out"][0].reshape(8, 128).T], axis=1))
    rep["gffn_bc"] = np.ascontiguousarray(np.broadcast_to(g["g_ffn"][0][None, :], (128, D)))
    rep["gfin_bc"] = np.ascontiguousarray(np.broadcast_to(g["g_final"][None, :], (128, D)))
    w_up = g["w_up"][0]
    wu = w_up[:, 0:6144].reshape(KC, 128, 24, 256)
    wv = w_up[:, 6144:12288].reshape(KC, 128, 24, 256)
    rep["wup"] = np.ascontiguousarray(np.concatenate([wu, wv], axis=3).transpose(2, 1, 0, 3))
    rep["wdn"] = np.ascontiguousarray(g["w_down"][0].reshape(24, 2, 128, D).transpose(0, 2, 1, 3))
    rep["fcw"] = np.ascontiguousarray(g["ffn_conv_w"][0].reshape(3, 48, 128).transpose(2, 1, 0))
    rep["fcb"] = np.ascontiguousarray(g["ffn_conv_b"][0].reshape(48, 128).T)
    rep.update(_host_consts())

    in_maps = []
    for c in range(8):
        b, r = c // 4, c % 4
        pad = 1024 * (3 - r)
        xl = np.zeros((S, D), f32)
        xl[pad:] = x[b, 0:S - pad]
        valid = np.zeros((S,), f32)
        valid[pad:] = 1.0
        m = dict(rep)
        m["x_loc"] = xl
        m["stflag"] = np.ascontiguousarray(np.broadcast_to(valid[0:S:512][None, :], (128, 8)))
        m["validT"] = np.ascontiguousarray(valid.reshape(32, 128).T)
        vn = np.zeros((256,), f32)
        vn[pad // 16:255] = 1.0
        m["validn"] = np.ascontiguousarray(vn.reshape(2, 128).T)
        f0 = np.zeros((128, 64), f32)
        f0[:, pad // 64] = 1e4
        m["f0"] = f0
        in_maps.append(m)

    if "nc" not in _NC_CACHE:
        _NC_CACHE["nc"] = build_program()
    res = run_bass_kernel_spmd(_NC_CACHE["nc"], in_maps, core_ids=list(range(8)))
    out = np.zeros((2, S, D), f32)
    for c in range(8):
        b, r = c // 4, c % 4
        out[b, r * 1024:(r + 1) * 1024] = np.asarray(res.results[c]["y"], dtype=f32)
    return out
```

```python
import numpy as np
import ml_dtypes
import concourse.bass as bass
import concourse.mybir as mybir
from concourse.bass_utils import run_bass_kernel_spmd

F32 = mybir.dt.float32
BF16 = mybir.dt.bfloat16
AF = mybir.ActivationFunctionType
ALU = mybir.AluOpType

D = 2048
KC = 16
S = 4096
NOWN = 1026
EPS = 1e-6
NDMA = 12
USE_GELU_TANH_LUT = True


class Tk:
    __slots__ = ("name", "w", "r")

    def __init__(self, name):
        self.name = name
        self.w = None
        self.r = []


class Prog:
    def __init__(self, nc):
        self.nc = nc
        self.engs = ("pe", "act", "dve", "pool", "sp")
        self.sem = {k: nc.alloc_semaphore("sem_" + k) for k in self.engs}
        self.cnt = {k: 0 for k in self.engs}
        self.streams = {k: [] for k in self.engs}
        self.waited = {k: {} for k in self.engs}
        self.dq = {q: [[nc.alloc_semaphore("dq_%s_%d" % (q, i)), 0] for i in range(NDMA)] for q in ("sp", "pool")}
        self.dq_rr = {"sp": 0, "pool": 0}
        self.out_events = []

    def _wait(self, eng, evs):
        best = {}
        for ev in evs:
            if ev is None:
                continue
            key, sem, val = ev
            if key == eng and eng == "pe":
                continue
            if self.waited[eng].get(key, 0) >= val:
                continue
            if key not in best or best[key][1] < val:
                best[key] = (sem, val)
        for key, (sem, val) in best.items():
            self.waited[eng][key] = val
            self.streams[eng].append(lambda e, sem=sem, val=val: e.wait_ge(sem, val))

    def _deps(self, r, w):
        evs = []
        for t in r:
            evs.append(t.w)
        for t in w:
            evs.append(t.w)
            evs.extend(t.r)
        return evs

    def op(self, eng, fn, r=(), w=()):
        self._wait(eng, self._deps(r, w))
        self.cnt[eng] += 1
        sem = self.sem[eng]
        self.streams[eng].append(lambda e, fn=fn, sem=sem: fn(e).then_inc(sem, 1))
        ev = (eng, sem, self.cnt[eng])
        for t in r:
            t.r.append(ev)
        for t in w:
            t.w = ev
            t.r = []
        return ev

    def dma(self, q, out, in_, r=(), w=(), is_output=False):
        slot = self.dq[q][self.dq_rr[q] % NDMA]
        self.dq_rr[q] += 1
        sem = slot[0]
        key = "dma_" + str(id(slot))
        evs = self._deps(r, w)
        if slot[1] > 0:
            evs.append((key, sem, slot[1]))
        self._wait(q, evs)
        slot[1] += 16
        val = slot[1]
        self.streams[q].append(lambda e, out=out, in_=in_, sem=sem: e.dma_start(out=out, in_=in_).then_inc(sem, 16))
        ev = (key, sem, val)
        for t in r:
            t.r.append(ev)
        for t in w:
            t.w = ev
            t.r = []
        if is_output:
            self.out_events.append(ev)
        return ev

    def barrier(self):
        evs = [(k, self.sem[k], self.cnt[k]) for k in self.engs if self.cnt[k] > 0]
        for q in ("sp", "pool"):
            for slot in self.dq[q]:
                if slot[1] > 0:
                    evs.append(("dma_" + str(id(slot)), slot[0], slot[1]))
        for e in self.engs:
            self._wait(e, [ev for ev in evs if ev[0] != e])

    def finish(self):
        self.barrier()
        self._wait("sp", self.out_events)
        nc = self.nc
        st = self.streams
        with nc.Block() as block:
            @block.tensor
            def _(e):
                for f in st["pe"]:
                    f(e)

            @block.scalar
            def _(e):
                for f in st["act"]:
                    f(e)

            @block.vector
            def _(e):
                for f in st["dve"]:
                    f(e)

            @block.gpsimd
            def _(e):
                for f in st["pool"]:
                    f(e)

            @block.sync
            def _(e):
                for f in st["sp"]:
                    f(e)

    def act(self, out, in_, func, r=(), w=(), **kw):
        return self.op("act", lambda e: e.activation(out=out, in_=in_, func=func, **kw), r, w)

    def mm(self, out, lhsT, rhs, start, stop, r=(), w=()):
        return self.op("pe", lambda e: e.matmul(out, lhsT, rhs, start=start, stop=stop), r, w)

    def tr(self, out, in_, ident, r=(), w=()):
        return self.op("pe", lambda e: e.transpose(out, in_, ident), r, w)

    def tt(self, eng, out, in0, in1, op, r=(), w=()):
        return self.op(eng, lambda e: e.tensor_tensor(out=out, in0=in0, in1=in1, op=op), r, w)

    def ts(self, eng, out, in0, s1, s2, op0, op1=None, r=(), w=()):
        if op1 is None:
            return self.op(eng, lambda e: e.tensor_scalar(out=out, in0=in0, scalar1=s1, scalar2=None, op0=op0), r, w)
        return self.op(eng, lambda e: e.tensor_scalar(out=out, in0=in0, scalar1=s1, scalar2=s2, op0=op0, op1=op1), r, w)

    def stt(self, out, in0, scalar, in1, op0, op1, r=(), w=()):
        return self.op("dve", lambda e: e.scalar_tensor_tensor(out=out, in0=in0, scalar=scalar, in1=in1, op0=op0, op1=op1), r, w)

    def cp(self, eng, out, in_, r=(), w=()):
        if eng == "act":
            return self.act(out, in_, AF.Copy, r, w)
        return self.op(eng, lambda e: e.tensor_copy(out=out, in_=in_), r, w)


class Arena:
    def __init__(self, nc, base, limit):
        self.nc = nc
        self.off = base
        self.limit = limit
        self.n = 0

    def alloc(self, name, shape, dt):
        esz = 2 if dt == BF16 else 4
        nb = esz
        for s in shape[1:]:
            nb *= s
        nb = (nb + 31) // 32 * 32
        t = self.nc.alloc_sbuf_tensor_at("%s_%d" % (name, self.n), list(shape), dt, offset=self.off)
        self.n += 1
        self.off += nb
        assert self.off <= self.limit, ("SBUF overflow", name, self.off, self.limit)
        return t

    def mark(self):
        return self.off

    def release(self, m):
        self.off = m


def build_program():
    nc = bass.Bass("TRN2", target_bir_lowering=False)
    P = Prog(nc)

    def din(name, shape, dt=F32):
        return nc.dram_tensor(name, list(shape), dt, kind="ExternalInput").ap()

    x_loc = din("x_loc", [S, D])
    stflag_d = din("stflag", [128, 8])
    validT_d = din("validT", [128, 32])
    validn_d = din("validn", [128, 2])
    f0_d = din("f0", [128, 64])
    wA1_d = din("wA1", [128, KC, 1024])
    wB3_d = din("wB3", [128, KC, 1024])
    wA2_d = din("wA2", [128, KC, 1024])
    wB1_d = din("wB1", [128, KC, 536])
    wB2_d = din("wB2", [128, KC, 1024])
    gmixT_d = din("gmixT", [128, KC])
    lcw_d = din("lcw", [128, 8, 4])
    lvec_d = din("lvec", [128, 4, 8])
    wa_d = din("wa", [128, 8, 128])
    wx_d = din("wx", [128, 8, 128])
    w1k_d = din("w1k", [128, 32, 128])
    w1v_d = din("w1v", [128, 32, 128])
    pe_d = din("peT", [128, 2, 32])
    cmpb_d = din("cmpb", [128, 2])
    w2k_d = din("w2k", [128, 128])
    w2v_d = din("w2v", [128, 128])
    ident_d = din("ident", [128, 128], BF16)
    ebig_d = din("ebig", [64, S], BF16)
    ovl_d = din("ovl", [128, 2, 64])
    cwide_d = din("cwide", [128, 3, 128])
    cwide_h_d = din("cwide_h", [2, 3, 128])
    wol_d = din("wol", [128, 8, D])
    woa_d = din("woa", [128, 8, D])
    gout_d = din("gout", [128, 2, 8])
    gffn_d = din("gffn_bc", [128, D])
    gfin_d = din("gfin_bc", [128, D])
    wup_d = din("wup", [24, 128, KC, 512])
    wdn_d = din("wdn", [24, 128, 2, D])
    fcw_d = din("fcw", [128, 48, 3])
    fcb_d = din("fcb", [128, 48])
    y_out = nc.dram_tensor("y", [1024, D], F32, kind="ExternalOutput").ap()

    base = (int(nc.sbuf_base) + 63) // 64 * 64
    A = Arena(nc, base, int(nc.sbuf_base) + int(nc.sbuf_bytes_remaining) - 64)
    banks = [nc.alloc_psum_tensor("bank%d" % i, [128, 512], F32) for i in range(8)]
    bk = [Tk("bank%d" % i) for i in range(8)]

    def bf(bank_ap):
        return bank_ap.bitcast(BF16)

    ident = A.alloc("ident", [128, 128], BF16)
    ident_t = Tk("ident")
    P.dma("sp", ident[:, :], ident_d[:, :], w=[ident_t])
    gmixT = A.alloc("gmixT", [128, KC], F32)
    gmixT_t = Tk("gmixT")
    P.dma("sp", gmixT[:, :], gmixT_d[:, :], w=[gmixT_t])
    ones_bf = A.alloc("ones", [128, 2], BF16)
    ones_t = Tk("ones")
    P.op("dve", lambda e: e.memset(ones_bf[:, :], 1.0), w=[ones_t])

    epsb = A.alloc("epsb", [128, 1], F32)
    epsb_t = Tk("epsb")
    P.op("dve", lambda e: e.memset(epsb[:, :], EPS), w=[epsb_t])
    oneb = A.alloc("oneb", [128, 1], F32)
    oneb_t = Tk("oneb")
    P.op("dve", lambda e: e.memset(oneb[:, :], 1.0), w=[oneb_t])
    y_base = A.mark()
    ylruT = A.alloc("ylruT", [128, 8, NOWN], BF16)
    ylru_t = [Tk("ylru%d" % c) for c in range(8)]
    yattT = A.alloc("yattT", [128, 8, NOWN], BF16)
    yatt_t = Tk("yatt")
    y_end = A.mark()

    m_mixer = A.mark()

    def proj_pass(w_dram, ncols, sts, groups, nrot=6):
        m0 = A.mark()
        W = A.alloc("W", [128, KC, ncols], BF16)
        W_t = Tk("W")
        xt = [A.alloc("xt", [128, D], F32) for _ in range(2)]
        xt_t = [Tk("xt0"), Tk("xt1")]
        for k in range(KC):
            P.dma("sp", xt[k % 2][:, 0:ncols], w_dram[:, k, :], w=[xt_t[k % 2]])
            P.ts("dve" if k % 2 == 0 else "pool", W[:, k, :], xt[k % 2][:, 0:ncols], gmixT[:, k:k + 1], None, ALU.mult,
                 r=[xt_t[k % 2], gmixT_t], w=[W_t])
        xn = [A.alloc("xn", [128, D], BF16) for _ in range(2)]
        xn_t = [Tk("xn0"), Tk("xn1")]
        ss = [A.alloc("ss", [128, 4], F32) for _ in range(2)]
        ss_t = [Tk("ss0"), Tk("ss1")]
        xnT = [A.alloc("xnT", [128, KC, 512], BF16) for _ in range(2)]
        xnT_t = [Tk("xnT0"), Tk("xnT1")]
        ctx = dict(W=W, W_t=W_t)
        for g in groups:
            if "setup" in g:
                g["setup"](ctx)
        rot = [2]

        def prep_tile(si, tt):
            tk = sts[si] * 4 + tt
            b = tk % 2
            P.dma("sp", xt[b][:, :], x_loc[tk * 128:(tk + 1) * 128, :], w=[xt_t[b]])
            P.act(xn[b][:, :], xt[b][:, :], AF.Square, r=[xt_t[b]], w=[xn_t[b], ss_t[b]], accum_out=ss[b][:, 0:1])
            P.act(ss[b][:, 1:2], ss[b][:, 0:1], AF.Ln, r=[ss_t[b]], w=[ss_t[b]], scale=1.0 / D, bias=epsb[:, 0:1])
            P.act(ss[b][:, 2:3], ss[b][:, 1:2], AF.Exp, r=[ss_t[b]], w=[ss_t[b]], scale=-0.5)
            P.act(xn[b][:, :], xt[b][:, :], AF.Copy, r=[xt_t[b], ss_t[b]], w=[xn_t[b]], scale=ss[b][:, 2:3])

        def prep_tr(si, tt):
            tk = sts[si] * 4 + tt
            b = tk % 2
            xb = si % 2
            for half in range(2):
                pb = banks[half]
                for j in range(8):
                    jj = half * 8 + j
                    P.tr(bf(pb[:, :])[:, j * 128:(j + 1) * 128], xn[b][:, jj * 128:(jj + 1) * 128], ident[:, :],
                         r=[xn_t[b], ident_t], w=[bk[half]])
                P.cp("dve" if half == 0 else "act",
                     xnT[xb][:, half * 8:(half + 1) * 8, tt * 128:(tt + 1) * 128],
                     bf(pb[:, :]).rearrange("p (j t) -> p j t", j=8),
                     r=[bk[half]], w=[xnT_t[xb]])

        def make_units(si):
            st = sts[si]
            xb = si % 2
            units = []
            for g in groups:
                if st not in g.get("sts", sts):
                    continue
                if g["kind"] == "fm":
                    for j in range(g["ncols"] // 128):
                        def mmf(g=g, j=j):
                            bi = rot[0]
                            rot[0] = 2 + (rot[0] - 1) % nrot
                            for k in range(KC):
                                P.mm(banks[bi][:, :], W[:, k, g["col0"] + j * 128: g["col0"] + (j + 1) * 128], xnT[xb][:, k, :],
                                     k == 0, k == KC - 1, r=[W_t, xnT_t[xb]], w=[bk[bi]])
                            return bi
                        units.append((mmf, lambda bi, g=g, j=j: g["evac"](st, j, banks[bi], bk[bi])))
                else:
                    nco = g["ncols"]
                    for tt in range(4):
                        def mmf(g=g, tt=tt, nco=nco):
                            bi = rot[0]
                            rot[0] = 2 + (rot[0] - 1) % nrot
                            for k in range(KC):
                                P.mm(banks[bi][:, 0:nco], xnT[xb][:, k, tt * 128:(tt + 1) * 128], W[:, k, g["col0"]: g["col0"] + nco],
                                     k == 0, k == KC - 1, r=[W_t, xnT_t[xb]], w=[bk[bi]])
                            return bi
                        units.append((mmf, lambda bi, g=g, tt=tt: g["evac"](st, tt, banks[bi], bk[bi])))
            return units

        for tt in range(4):
            prep_tile(0, tt)
            prep_tr(0, tt)
        for si in range(len(sts)):
            units = make_units(si)
            n = len(units)
            sched = {}
            if si + 1 < len(sts):
                for k in range(4):
                    u_tile = (k * n) // 4
                    sched.setdefault(u_tile, []).append(("tile", k))
                    u_tr = min(n - 1, u_tile + max(1, n // 8))
                    sched.setdefault(u_tr, []).append(("tr", k))
            pending = None
            for ui, (mmf, evf) in enumerate(units):
                for kind, k in sched.get(ui, []):
                    if kind == "tile":
                        prep_tile(si + 1, k)
                    else:
                        prep_tr(si + 1, k)
                bi = mmf()
                if pending is not None:
                    pending[0](pending[1])
                pending = (evf, bi)
            pending[0](pending[1])
        P.barrier()
        A.release(m0)


    def run_lru():
        m0 = A.mark()
        h_own = A.alloc("h_own", [128, 8, NOWN], F32)
        h_own_t = [Tk("h_own%d" % c) for c in range(8)]
        m_l = A.mark()
        lcw = A.alloc("lcw", [128, 8, 4], F32)
        lvec = A.alloc("lvec", [128, 4, 8], F32)
        c12 = A.alloc("c12", [128, 2, 8], F32)
        lsm_t = Tk("lsm")
        P.dma("sp", lcw[:, :, :], lcw_d[:, :, :], w=[lsm_t])
        P.dma("sp", lvec[:, :, :], lvec_d[:, :, :], w=[lsm_t])
        P.act(c12[:, 0, :], lvec[:, 3, :], AF.Exp, r=[lsm_t], w=[lsm_t], scale=-1.0)
        P.act(c12[:, 0, :], c12[:, 0, :], AF.Ln, r=[lsm_t], w=[lsm_t], bias=oneb[:, 0:1])
        P.ts("dve", c12[:, 1, :], c12[:, 0, :], -16.0, None, ALU.mult, r=[lsm_t], w=[lsm_t])
        P.ts("dve", c12[:, 0, :], c12[:, 0, :], -8.0, None, ALU.mult, r=[lsm_t], w=[lsm_t])
        wa = A.alloc("wa", [128, 8, 128], F32)
        wx = A.alloc("wx", [128, 8, 128], F32)
        wax_t = Tk("wax")
        P.dma("sp", wa[:, :, :], wa_d[:, :, :], w=[wax_t])
        P.dma("sp", wx[:, :, :], wx_d[:, :, :], w=[wax_t])
        stf = A.alloc("stf", [128, 8], F32)
        stf_t = Tk("stf")
        P.dma("sp", stf[:, :], stflag_d[:, :], w=[stf_t])
        hin = A.alloc("hin", [128, 8], F32)
        hin_t = [Tk("hin%d" % c) for c in range(8)]
        xbuf = A.alloc("xbuf", [128, 8, 515], F32)
        xbuf_t = [Tk("xbuf%d" % c) for c in range(8)]
        hst = A.alloc("hst", [128, 8], F32)
        hst_t = [Tk("hst%d" % c) for c in range(8)]
        for c in range(8):
            P.op("pool", lambda e, c=c: e.memset(xbuf[:, c, 0:3], 0.0), w=[xbuf_t[c]])
        NB = 2
        tmp = {}
        tmp_t = {}
        for nm in ("u", "r", "i", "a", "s", "v", "h"):
            tmp[nm] = [A.alloc("l" + nm, [128, 512], F32) for _ in range(NB)]
            tmp_t[nm] = [Tk("l%s%d" % (nm, b)) for b in range(NB)]
        gps = [6, 7]

        def evac(st, c, ps, ps_t):
            b = c % NB
            xb = xbuf[:, c, :]
            if st > 0:
                P.cp("dve", xbuf[:, c, 0:3], xbuf[:, c, 512:515], r=[xbuf_t[c]], w=[xbuf_t[c]])
            P.cp("act", xbuf[:, c, 3:515], ps[:, :], r=[ps_t], w=[xbuf_t[c]])
            u = tmp["u"][b]
            ut = tmp_t["u"][b]
            P.ts("dve", u[:, :], xbuf[:, c, 3:515], lcw[:, c, 3:4], lvec[:, 0, c:c + 1], ALU.mult, ALU.add,
                 r=[xbuf_t[c], lsm_t], w=[ut])
            for j in range(3):
                P.stt(u[:, :], xbuf[:, c, j:j + 512], lcw[:, c, j:j + 1], u[:, :], ALU.mult, ALU.add,
                      r=[xbuf_t[c], lsm_t, ut], w=[ut])
            P.mm(banks[gps[0]][:, :], wa[:, c, :], u[:, :], True, True, r=[wax_t, ut], w=[bk[gps[0]]])
            P.mm(banks[gps[1]][:, :], wx[:, c, :], u[:, :], True, True, r=[wax_t, ut], w=[bk[gps[1]]])
            rr, ii, aa, sq, vv = tmp["r"][b], tmp["i"][b], tmp["a"][b], tmp["s"][b], tmp["v"][b]
            P.act(rr[:, :], banks[gps[0]][:, :], AF.Sigmoid, r=[bk[gps[0]], lsm_t], w=[tmp_t["r"][b]], bias=lvec[:, 1, c:c + 1])
            P.act(ii[:, :], banks[gps[1]][:, :], AF.Sigmoid, r=[bk[gps[1]], lsm_t], w=[tmp_t["i"][b]], bias=lvec[:, 2, c:c + 1])
            P.act(aa[:, :], rr[:, :], AF.Exp, r=[tmp_t["r"][b], lsm_t], w=[tmp_t["a"][b]], scale=c12[:, 0, c:c + 1])
            P.act(sq[:, :], rr[:, :], AF.Exp, r=[tmp_t["r"][b], lsm_t], w=[tmp_t["s"][b]], scale=c12[:, 1, c:c + 1])
            P.ts("dve", sq[:, :], sq[:, :], -1.0, 1.0, ALU.mult, ALU.add, r=[tmp_t["s"][b]], w=[tmp_t["s"][b]])
            P.ts("dve", sq[:, :], sq[:, :], 1e-18, None, ALU.max, r=[tmp_t["s"][b]], w=[tmp_t["s"][b]])
            P.act(sq[:, :], sq[:, :], AF.Ln, r=[tmp_t["s"][b]], w=[tmp_t["s"][b]])
            P.act(sq[:, :], sq[:, :], AF.Exp, r=[tmp_t["s"][b]], w=[tmp_t["s"][b]], scale=0.5)
            P.tt("dve", vv[:, :], ii[:, :], u[:, :], ALU.mult, r=[tmp_t["i"][b], ut], w=[tmp_t["v"][b]])
            P.tt("dve", vv[:, :], vv[:, :], sq[:, :], ALU.mult, r=[tmp_t["s"][b], tmp_t["v"][b]], w=[tmp_t["v"][b]])
            if st >= 6:
                hout = h_own[:, c, 2 + (st - 6) * 512: 2 + (st - 5) * 512]
                ht = h_own_t[c]
            else:
                hout = tmp["h"][b][:, :]
                ht = tmp_t["h"][b]
            if st == 0:
                init = 0.0
            else:
                P.tt("dve", hin[:, c:c + 1], hst[:, c:c + 1], stf[:, st - 1:st], ALU.mult, r=[hst_t[c], stf_t], w=[hin_t[c]])
                init = hin[:, c:c + 1]
            P.op("dve", lambda e, hout=hout, aa=aa, vv=vv, init=init: e.tensor_tensor_scan(
                out=hout, data0=aa[:, :], data1=vv[:, :], initial=init, op0=ALU.mult, op1=ALU.add),
                r=[tmp_t["a"][b], tmp_t["v"][b], hin_t[c]], w=[ht])
            P.cp("dve", hst[:, c:c + 1], hout[:, 511:512], r=[ht], w=[hst_t[c]])
            if st == 5:
                P.cp("dve", h_own[:, c, 0:2], hout[:, 510:512], r=[ht], w=[h_own_t[c]])

        proj_pass(wA1_d, 1024, list(range(8)), [dict(kind="fm", col0=0, ncols=1024, evac=evac)], nrot=4)
        A.release(m_l)

        gt = [A.alloc("gt", [128, 512], F32) for _ in range(2)]
        gt_t = [Tk("gt0"), Tk("gt1")]
        gt2 = [A.alloc("gt2", [128, 512], F32) for _ in range(2)]
        gt2_t = [Tk("gt20"), Tk("gt21")]

        def evac_gate(st, c, ps, ps_t):
            b = c % 2
            if st == 5:
                lo, hi, o0 = 510, 512, 0
            else:
                lo, hi, o0 = 0, 512, 2 + (st - 6) * 512
            n = hi - lo
            gelu(gt[b][:, 0:n], ps[:, lo:hi], gt2[b][:, 0:n], [ps_t], gt_t[b], gt2_t[b])
            P.tt("dve", ylruT[:, c, o0:o0 + n], gt[b][:, 0:n], h_own[:, c, o0:o0 + n], ALU.mult,
                 r=[gt_t[b], h_own_t[c]], w=[ylru_t[c]])

        proj_pass(wB3_d, 1024, [5, 6, 7], [dict(kind="fm", col0=0, ncols=1024, evac=evac_gate)])
        A.release(m0)

    def gelu(out, in_, scratch, in_t, out_t, scratch_t):
        if USE_GELU_TANH_LUT:
            P.act(out, in_, AF.Gelu_apprx_tanh, r=in_t, w=[out_t])
            return
        P.act(scratch, in_, AF.Square, r=in_t, w=[scratch_t])
        P.ts("dve", scratch, scratch, 0.044715, 1.0, ALU.mult, ALU.add, r=[scratch_t], w=[scratch_t])
        P.tt("dve", scratch, scratch, in_, ALU.mult, r=[scratch_t] + list(in_t), w=[scratch_t])
        P.act(scratch, scratch, AF.Sigmoid, r=[scratch_t], w=[scratch_t], scale=1.5957691216057308)
        P.tt("dve", out, scratch, in_, ALU.mult, r=[scratch_t] + list(in_t), w=[out_t])


    run_lru()

    kselT = A.alloc("kselT", [128, 2, S], BF16)
    ksel_t = Tk("kselT")
    vsel = A.alloc("vsel", [128, 32, 2, 129], BF16)
    vsel_t = Tk("vsel")
    kcmpT = A.alloc("kcmpT", [128, 2, 256], BF16)
    kcmp_t = Tk("kcmpT")
    Rc = A.alloc("Rc", [128, 2, 2, 193], BF16)
    Rc_t = Tk("Rc")
    validT = A.alloc("validT", [128, 32], F32)
    validT_t = Tk("validT")
    P.dma("sp", validT[:, :], validT_d[:, :], w=[validT_t])
    P.op("dve", lambda e: e.tensor_copy(out=vsel[:, :, 0, 128:129], in_=validT[:, :].unsqueeze(2)), r=[validT_t], w=[vsel_t])
    P.op("dve", lambda e: e.tensor_copy(out=vsel[:, :, 1, 128:129], in_=validT[:, :].unsqueeze(2)), r=[validT_t], w=[vsel_t])
    m_a2 = A.mark()
    kvcT = A.alloc("kvcT", [128, 4, S], BF16)
    kvc_t = Tk("kvcT")

    def evac_a2_fm(st, j, ps, ps_t):
        eng = "act" if j % 2 == 0 else "dve"
        if j < 4:
            P.cp(eng, kvcT[:, j, st * 512:(st + 1) * 512], ps[:, :], r=[ps_t], w=[kvc_t])
        else:
            P.cp(eng, kselT[:, j - 4, st * 512:(st + 1) * 512], ps[:, :], r=[ps_t], w=[ksel_t])

    def evac_a2_v(st, tt, ps, ps_t):
        ch = st * 4 + tt
        P.cp("act" if tt % 2 else "dve", vsel[:, ch, :, 0:128], ps[:, 0:256].rearrange("p (h d) -> p h d", h=2),
             r=[ps_t], w=[vsel_t])

    proj_pass(wA2_d, 1024, list(range(8)),
              [dict(kind="fm", col0=0, ncols=768, evac=evac_a2_fm),
               dict(kind="tm", col0=768, ncols=256, evac=evac_a2_v)])

    def run_compress():
        m0 = A.mark()
        w1 = [A.alloc("w1", [128, 32, 128], BF16) for _ in range(2)]
        w1_t = [Tk("w1k"), Tk("w1v")]
        P.dma("pool", w1[0][:, :, :], w1k_d[:, :, :], w=[w1_t[0]])
        P.dma("pool", w1[1][:, :, :], w1v_d[:, :, :], w=[w1_t[1]])
        w2 = A.alloc("w2", [128, 2, 128], BF16)
        w2_t = Tk("w2")
        P.dma("pool", w2[:, 0, :], w2k_d[:, :], w=[w2_t])
        P.dma("pool", w2[:, 1, :], w2v_d[:, :], w=[w2_t])
        peT = A.alloc("peT", [128, 2, 32], BF16)
        peT_t = Tk("peT")
        P.dma("pool", peT[:, :, :], pe_d[:, :, :], w=[peT_t])
        cb = A.alloc("cb", [128, 4], F32)
        cb_t = Tk("cb")
        P.dma("sp", cb[:, 0:2], cmpb_d[:, :], w=[cb_t])
        vn = A.alloc("vn", [128, 2], F32)
        ovl = A.alloc("ovl", [128, 2, 64], F32)
        vn_t = Tk("vn")
        P.dma("sp", vn[:, :], validn_d[:, :], w=[vn_t])
        P.dma("sp", ovl[:, :, :], ovl_d[:, :, :], w=[vn_t])
        for ty in range(2):
            for l in range(32):
                P.mm(banks[2][:, ty:ty + 1], w1[ty][:, l, :], peT[:, ty, l:l + 1], l == 0, l == 31,
                     r=[w1_t[ty], peT_t], w=[bk[2]])
            P.tt("dve", cb[:, 2 + ty:3 + ty], banks[2][:, ty:ty + 1], cb[:, ty:ty + 1], ALU.add, r=[bk[2], cb_t], w=[cb_t])
        hid = [A.alloc("hid", [128, 256], F32) for _ in range(2)]
        hid_t = [Tk("hid0"), Tk("hid1")]
        hs = [A.alloc("hs", [128, 256], F32) for _ in range(2)]
        hs_t = [Tk("hs0"), Tk("hs1")]
        hp = [A.alloc("hp", [128, 256], F32) for _ in range(2)]
        hp_t = [Tk("hp0"), Tk("hp1")]
        hb = [A.alloc("hb", [128, 256], BF16) for _ in range(2)]
        hb_t = [Tk("hb0"), Tk("hb1")]
        for c2 in range(2):
            for hk in range(2):
                P.cp("dve", Rc[:, c2, hk, 0:1], vn[:, c2:c2 + 1], r=[vn_t], w=[Rc_t])
                P.ts("dve", Rc[:, c2, hk, 1:65], ovl[:, c2, :], vn[:, c2:c2 + 1], None, ALU.mult, r=[vn_t], w=[Rc_t])
        it = 0
        for hk in range(2):
            for ty in range(2):
                b = it % 2
                it += 1
                pb = 3 + b
                for l in range(32):
                    P.mm(banks[pb][:, 0:255], w1[ty][:, l, :], kvcT[:, ty * 2 + hk, l: l + 16 * 254 + 1: 16], l == 0, l == 31,
                         r=[w1_t[ty], kvc_t], w=[bk[pb]])
                P.ts("dve", hp[b][:, 0:255], banks[pb][:, 0:255], cb[:, 2 + ty:3 + ty], None, ALU.add, r=[bk[pb], cb_t], w=[hp_t[b]])
                gelu(hid[b][:, 0:255], hp[b][:, 0:255], hs[b][:, 0:255], [hp_t[b]], hid_t[b], hs_t[b])
                P.cp("dve", hb[b][:, 0:255], hid[b][:, 0:255], r=[hid_t[b]], w=[hb_t[b]])
                if ty == 0:
                    P.mm(banks[5][:, 0:255], w2[:, 0, :], hb[b][:, 0:255], True, True, r=[w2_t, hb_t[b]], w=[bk[5]])
                    P.cp("act", kcmpT[:, hk, 0:255], banks[5][:, 0:255], r=[bk[5]], w=[kcmp_t])
                else:
                    for c2 in range(2):
                        rows = 128 if c2 == 0 else 127
                        P.mm(banks[6 + c2][0:rows, 0:128], hb[b][:, c2 * 128: c2 * 128 + rows], w2[:, 1, :], True, True,
                             r=[w2_t, hb_t[b]], w=[bk[6 + c2]])
                        P.ts("dve", Rc[0:rows, c2, hk, 65:193], banks[6 + c2][0:rows, 0:128], vn[0:rows, c2:c2 + 1], None, ALU.mult,
                             r=[bk[6 + c2], vn_t], w=[Rc_t])
        P.barrier()
        A.release(m0)

    run_compress()
    A.release(m_a2)

    kwT = A.alloc("kwT", [128, 2, 2048], BF16)
    kw_t = Tk("kwT")
    vwin = A.alloc("vwin", [128, 16, 2, 129], BF16)
    vwin_t = Tk("vwin")
    gsig = A.alloc("gsig", [128, 9, 24], F32)
    gsig_t = Tk("gsig")
    qT = A.alloc("qT", [128, 8, NOWN], BF16)
    qT_t = Tk("qT")
    P.op("dve", lambda e: e.tensor_copy(out=vwin[:, :, 0, 128:129], in_=validT[:, 16:32].unsqueeze(2)), r=[validT_t], w=[vwin_t])
    P.op("dve", lambda e: e.tensor_copy(out=vwin[:, :, 1, 128:129], in_=validT[:, 16:32].unsqueeze(2)), r=[validT_t], w=[vwin_t])
    def evac_b1_k(st, j, ps, ps_t):
        P.cp("act" if j else "dve", kwT[:, j, (st - 4) * 512:(st - 3) * 512], ps[:, :], r=[ps_t], w=[kw_t])

    def evac_b1_v(st, tt, ps, ps_t):
        ch = (st - 4) * 4 + tt
        P.cp("dve", vwin[:, ch, :, 0:128], ps[:, 0:256].rearrange("p (h d) -> p h d", h=2), r=[ps_t], w=[vwin_t])
        if st >= 6:
            ti = 1 + (st - 6) * 4 + tt
            P.act(gsig[:, ti, :], ps[:, 256:280], AF.Sigmoid, r=[ps_t], w=[gsig_t])

    halo_ctx = {}

    proj_pass(wB1_d, 536, [4, 5, 6, 7],
              [dict(kind="fm", col0=0, ncols=256, evac=evac_b1_k),
               dict(kind="tm", col0=256, ncols=280, evac=evac_b1_v)])

    def setup_b2(ctx):
        halo_ctx.update(ctx)

    def evac_b2(st, j, ps, ps_t):
        if st == 5:
            lo, hi, o0 = 510, 512, 0
        else:
            lo, hi, o0 = 0, 512, 2 + (st - 6) * 512
        P.act(qT[:, j, o0:o0 + hi - lo], ps[:, lo:hi], AF.Copy, r=[ps_t], w=[qT_t], scale=float(128 ** -0.5))

    proj_pass(wB2_d, 1024, [5, 6, 7], [dict(kind="fm", col0=0, ncols=1024, evac=evac_b2)])

    def halo_gates():
        m0 = A.mark()
        Wg = A.alloc("Wg", [128, KC, 24], BF16)
        Wg_t = Tk("Wg")
        sg = A.alloc("sg", [128, KC, 24], F32)
        sg_t = Tk("sg")
        P.dma("sp", sg[:, :, :], wB1_d[:, :, 512:536], w=[sg_t])
        for k in range(KC):
            P.ts("dve", Wg[:, k, :], sg[:, k, :], gmixT[:, k:k + 1], None, ALU.mult, r=[sg_t, gmixT_t], w=[Wg_t])
        xh = A.alloc("xh", [2, D], F32)
        xh_t = Tk("xh")
        P.dma("sp", xh[:, :], x_loc[3070:3072, :], w=[xh_t])
        xq = A.alloc("xq", [2, D], BF16)
        xq_t = Tk("xq")
        sh = A.alloc("sh", [2, 4], F32)
        sh_t = Tk("sh")
        P.act(xq[:, :], xh[:, :], AF.Square, r=[xh_t], w=[xq_t, sh_t], accum_out=sh[:, 0:1])
        P.act(sh[:, 1:2], sh[:, 0:1], AF.Ln, r=[sh_t], w=[sh_t], scale=1.0 / D, bias=epsb[0:2, 0:1])
        P.act(sh[:, 2:3], sh[:, 1:2], AF.Exp, r=[sh_t], w=[sh_t], scale=-0.5)
        P.act(xq[:, :], xh[:, :], AF.Copy, r=[xh_t, sh_t], w=[xq_t], scale=sh[:, 2:3])
        xqT = A.alloc("xqT", [128, KC, 2], BF16)
        xqT_t = Tk("xqT")
        for j in range(KC):
            P.tr(bf(banks[0][:, :])[:, j * 2:(j + 1) * 2], xq[:, j * 128:(j + 1) * 128], ident[0:2, 0:2], r=[xq_t, ident_t], w=[bk[0]])
        P.cp("dve", xqT[:, :, :], bf(banks[0][:, :])[:, 0:32].rearrange("p (j t) -> p j t", j=KC), r=[bk[0]], w=[xqT_t])
        for k in range(KC):
            P.mm(banks[2][0:2, 0:24], xqT[:, k, :], Wg[:, k, :], k == 0, k == KC - 1, r=[xqT_t, Wg_t], w=[bk[2]])
        P.act(gsig[0:2, 0, :], banks[2][0:2, 0:24], AF.Sigmoid, r=[bk[2]], w=[gsig_t])
        P.barrier()
        A.release(m0)

    halo_gates()

    def run_attention():
        m0 = A.mark()
        ebig = A.alloc("ebig", [64, S], BF16)
        cw = A.alloc("cw", [128, 3, 128], F32)
        cwh = A.alloc("cwh", [2, 3, 128], F32)
        f0 = A.alloc("f0", [128, 64], F32)
        cst_t = Tk("attn_consts")
        P.dma("sp", ebig[:, :], ebig_d[:, :], w=[cst_t])
        P.dma("sp", cw[:, :, :], cwide_d[:, :, :], w=[cst_t])
        P.dma("sp", cwh[:, :, :], cwide_h_d[:, :, :], w=[cst_t])
        P.dma("sp", f0[:, :], f0_d[:, :], w=[cst_t])
        NE = 4
        Et = [A.alloc("E", [128, 4, 128], BF16) for _ in range(NE)]
        Et_t = [Tk("E%d" % i) for i in range(NE)]
        Pt = [A.alloc("Pm", [128, 4, 128], BF16) for _ in range(NE)]
        Pt_t = [Tk("Pm%d" % i) for i in range(NE)]
        Ec = [A.alloc("Ec", [128, 4, 128], BF16) for _ in range(2)]
        Ec_t = [Tk("Ec0"), Tk("Ec1")]
        sm = A.alloc("sm", [128, 64], F32)
        sm_t = Tk("sm")
        imp = A.alloc("imp", [128, 64], F32)
        sc2 = A.alloc("sc2", [128, 64], F32)
        top = A.alloc("top", [128, 16], F32)
        selb = A.alloc("selb", [128, 64], BF16)
        selT = A.alloc("selT", [64, 128], BF16)
        tk_t = Tk("topk")
        selT_t = Tk("selT")
        acc = A.alloc("acc", [128, 4, 128], F32)
        acc_t = Tk("acc")
        ob = A.alloc("ob", [128, 4, 128], BF16)
        ob_t = Tk("ob")
        ectr = [0]
        PVB = [3, 4, 5, 6]

        def qblock(Dq, q0, nq, qc, gti):
            cwm = cwh if nq == 2 else cw
            lo = 64 - 2 * Dq
            for hk in range(2):
                qv = qT[:, 4 * hk:4 * hk + 4, qc:qc + nq]
                for c2 in range(2):
                    rows = 128 if c2 == 0 else 127
                    sb = c2
                    P.mm(banks[sb][0:rows, 0:4 * nq].rearrange("p (g q) -> p g q", g=4), kcmpT[:, hk, c2 * 128:c2 * 128 + rows], qv,
                         True, True, r=[kcmp_t, qT_t], w=[bk[sb]])
                    P.act(Ec[c2][0:rows, :, 0:nq], banks[sb][0:rows, 0:4 * nq].rearrange("p (g q) -> p g q", g=4), AF.Exp,
                          r=[bk[sb]], w=[Ec_t[c2]])
                    basev = 128 * Dq + q0 - 31 - 2048 * c2
                    P.op("pool", lambda e, c2=c2, rows=rows, basev=basev: e.affine_select(
                        out=Ec[c2][0:rows, :, 0:nq], in_=Ec[c2][0:rows, :, 0:nq], pattern=[[0, 4], [1, nq]],
                        compare_op=ALU.is_ge, fill=0.0, base=basev, channel_multiplier=-16), r=[Ec_t[c2]], w=[Ec_t[c2]])
                for g in range(4):
                    pb = PVB[g // 2]
                    co = (g % 2) * 193
                    for c2 in range(2):
                        rows = 128 if c2 == 0 else 127
                        P.mm(banks[pb][0:nq, co:co + 193], Ec[c2][0:rows, g, 0:nq], Rc[0:rows, c2, hk, :], c2 == 0, c2 == 1,
                             r=[Ec_t[c2], Rc_t], w=[bk[pb]])
                for g in range(4):
                    pb = PVB[g // 2]
                    co = (g % 2) * 193
                    P.ts("dve", sm[0:nq, g:g + 1], banks[pb][0:nq, co:co + 1], 1e-30, None, ALU.max, r=[bk[pb]], w=[sm_t])
                P.op("dve", lambda e: e.reciprocal(out=sm[0:nq, 4:8], in_=sm[0:nq, 0:4]), r=[sm_t], w=[sm_t])
                P.tt("dve", sm[0:nq, 8:12], sm[0:nq, 4:8], gsig[0:nq, gti, 12 * hk:12 * hk + 10:3], ALU.mult, r=[sm_t, gsig_t], w=[sm_t])
                for g in range(4):
                    pb = PVB[g // 2]
                    co = (g % 2) * 193
                    if g == 0:
                        P.ts("dve", imp[0:nq, :], banks[pb][0:nq, co + 1:co + 65], sm[0:nq, 4:5], None, ALU.mult, r=[bk[pb], sm_t], w=[tk_t])
                    else:
                        P.stt(imp[0:nq, :], banks[pb][0:nq, co + 1:co + 65], sm[0:nq, 4 + g:5 + g], imp[0:nq, :], ALU.mult, ALU.add,
                              r=[bk[pb], sm_t, tk_t], w=[tk_t])
                    P.ts("dve", acc[0:nq, g, :], banks[pb][0:nq, co + 65:co + 193], sm[0:nq, 8 + g:9 + g], None, ALU.mult,
                         r=[bk[pb], sm_t], w=[acc_t])
                P.tt("dve", imp[0:nq, :], imp[0:nq, :], cwm[0:nq, 0, lo:lo + 64], ALU.mult, r=[tk_t, cst_t], w=[tk_t])
                P.tt("dve", imp[0:nq, :], imp[0:nq, :], cwm[0:nq, 1, lo:lo + 64], ALU.add, r=[tk_t, cst_t], w=[tk_t])
                P.tt("dve", imp[0:nq, :], imp[0:nq, :], cwm[0:nq, 2, lo:lo + 64], ALU.max, r=[tk_t, cst_t], w=[tk_t])
                P.tt("dve", imp[0:nq, :], imp[0:nq, :], f0[0:nq, :], ALU.max, r=[tk_t, cst_t], w=[tk_t])
                P.op("dve", lambda e: e.max(out=top[0:nq, 0:8], in_=imp[0:nq, :]), r=[tk_t], w=[tk_t])
                P.op("dve", lambda e: e.match_replace(out=sc2[0:nq, :], in_to_replace=top[0:nq, 0:8], in_values=imp[0:nq, :],
                                                      imm_value=-1e9), r=[tk_t], w=[tk_t])
                P.op("dve", lambda e: e.max(out=top[0:nq, 8:16], in_=sc2[0:nq, :]), r=[tk_t], w=[tk_t])
                P.ts("dve", sc2[0:nq, :], imp[0:nq, :], top[0:nq, 15:16], None, ALU.is_ge, r=[tk_t], w=[tk_t])
                P.tt("dve", selb[0:nq, :], sc2[0:nq, :], cwm[0:nq, 0, lo:lo + 64], ALU.mult, r=[tk_t, cst_t], w=[tk_t])
                P.tr(bf(banks[2][:, :])[0:64, 512:512 + nq], selb[0:nq, :], ident[0:nq, 0:nq], r=[tk_t, ident_t], w=[bk[2]])
                P.cp("dve", selT[:, 0:nq], bf(banks[2][:, :])[0:64, 512:512 + nq], r=[bk[2]], w=[selT_t])
                def sel_qk(kc):
                    sb = ectr[0] % 2
                    ei = ectr[0] % NE
                    ectr[0] += 1
                    P.mm(banks[sb][:, 0:4 * nq].rearrange("p (g q) -> p g q", g=4), kselT[:, hk, kc * 128:(kc + 1) * 128], qv,
                         True, True, r=[ksel_t, qT_t], w=[bk[sb]])
                    mo = (kc % 2) * 128
                    P.mm(banks[2][:, mo:mo + nq], ebig[:, kc * 128:(kc + 1) * 128], selT[:, 0:nq], True, True,
                         r=[cst_t, selT_t], w=[bk[2]])
                    P.act(Et[ei][:, :, 0:nq], banks[sb][:, 0:4 * nq].rearrange("p (g q) -> p g q", g=4), AF.Exp,
                          r=[bk[sb]], w=[Et_t[ei]])
                    P.tt("dve", Pt[ei][:, :, 0:nq], Et[ei][:, :, 0:nq], banks[2][:, mo:mo + nq].unsqueeze(1).broadcast_to([128, 4, nq]), ALU.mult,
                         r=[Et_t[ei], bk[2]], w=[Pt_t[ei]])
                    if kc == Dq:
                        P.op("pool", lambda e, ei=ei: e.affine_select(
                            out=Pt[ei][:, :, 0:nq], in_=Pt[ei][:, :, 0:nq], pattern=[[0, 4], [1, nq]],
                            compare_op=ALU.is_ge, fill=0.0, base=q0, channel_multiplier=-1), r=[Pt_t[ei]], w=[Pt_t[ei]])
                    return ei

                def sel_pv(kc, ei):
                    for g in range(4):
                        P.mm(banks[PVB[g]][0:nq, 0:129], Pt[ei][:, g, 0:nq], vsel[:, kc, hk, :], kc == 0, kc == Dq,
                             r=[Pt_t[ei], vsel_t], w=[bk[PVB[g]]])
                    if kc == Dq:
                        for g in range(4):
                            P.ts("dve", sm[0:nq, 16 + g:17 + g], banks[PVB[g]][0:nq, 128:129], 1e-30, None, ALU.max, r=[bk[PVB[g]]], w=[sm_t])
                        P.op("dve", lambda e: e.reciprocal(out=sm[0:nq, 20:24], in_=sm[0:nq, 16:20]), r=[sm_t], w=[sm_t])
                        P.tt("dve", sm[0:nq, 24:28], sm[0:nq, 20:24], gsig[0:nq, gti, 12 * hk + 1:12 * hk + 11:3], ALU.mult, r=[sm_t, gsig_t], w=[sm_t])
                        for g in range(4):
                            P.stt(acc[0:nq, g, :], banks[PVB[g]][0:nq, 0:128], sm[0:nq, 24 + g:25 + g], acc[0:nq, g, :], ALU.mult, ALU.add,
                                  r=[bk[PVB[g]], sm_t, acc_t], w=[acc_t])

                def win_qk(kc):
                    sb = ectr[0] % 2
                    ei = ectr[0] % NE
                    ectr[0] += 1
                    wc = kc - 16
                    P.mm(banks[sb][:, 0:4 * nq].rearrange("p (g q) -> p g q", g=4), kwT[:, hk, wc * 128:(wc + 1) * 128], qv,
                         True, True, r=[kw_t, qT_t], w=[bk[sb]])
                    P.act(Pt[ei][:, :, 0:nq], banks[sb][:, 0:4 * nq].rearrange("p (g q) -> p g q", g=4), AF.Exp,
                          r=[bk[sb]], w=[Pt_t[ei]])
                    if kc == Dq:
                        P.op("pool", lambda e, ei=ei: e.affine_select(
                            out=Pt[ei][:, :, 0:nq], in_=Pt[ei][:, :, 0:nq], pattern=[[0, 4], [1, nq]],
                            compare_op=ALU.is_ge, fill=0.0, base=q0, channel_multiplier=-1), r=[Pt_t[ei]], w=[Pt_t[ei]])
                    if kc == Dq - 4:
                        P.op("pool", lambda e, ei=ei: e.affine_select(
                            out=Pt[ei][:, :, 0:nq], in_=Pt[ei][:, :, 0:nq], pattern=[[0, 4], [-1, nq]],
                            compare_op=ALU.is_ge, fill=0.0, base=-q0 - 1, channel_multiplier=1), r=[Pt_t[ei]], w=[Pt_t[ei]])
                    return ei

                def win_pv(kc, ei):
                    wc = kc - 16
                    for g in range(4):
                        P.mm(banks[PVB[g]][0:nq, 0:129], Pt[ei][:, g, 0:nq], vwin[:, wc, hk, :], kc == Dq - 4, kc == Dq,
                             r=[Pt_t[ei], vwin_t], w=[bk[PVB[g]]])

                steps = [(sel_qk, sel_pv, kc) for kc in range(Dq + 1)] + [(win_qk, win_pv, kc) for kc in range(Dq - 4, Dq + 1)]
                ei_next = steps[0][0](steps[0][2])
                for si_, (fq, fp, kc) in enumerate(steps):
                    ei_cur = ei_next
                    if si_ + 1 < len(steps):
                        ei_next = steps[si_ + 1][0](steps[si_ + 1][2])
                    fp(kc, ei_cur)
                for g in range(4):
                    P.ts("dve", sm[0:nq, 32 + g:33 + g], banks[PVB[g]][0:nq, 128:129], 1e-30, None, ALU.max, r=[bk[PVB[g]]], w=[sm_t])
                P.op("dve", lambda e: e.reciprocal(out=sm[0:nq, 36:40], in_=sm[0:nq, 32:36]), r=[sm_t], w=[sm_t])
                P.tt("dve", sm[0:nq, 40:44], sm[0:nq, 36:40], gsig[0:nq, gti, 12 * hk + 2:12 * hk + 12:3], ALU.mult, r=[sm_t, gsig_t], w=[sm_t])
                for g in range(4):
                    P.stt(ob[0:nq, g, :], banks[PVB[g]][0:nq, 0:128], sm[0:nq, 40 + g:41 + g], acc[0:nq, g, :], ALU.mult, ALU.add,
                          r=[bk[PVB[g]], sm_t, acc_t], w=[ob_t])
                for g in range(4):
                    P.tr(bf(banks[7][:, :])[:, g * 128:g * 128 + nq], ob[0:nq, g, :], ident[0:nq, 0:nq], r=[ob_t, ident_t], w=[bk[7]])
                P.cp("act", yattT[:, 4 * hk:4 * hk + 4, qc:qc + nq],
                     bf(banks[7][:, :])[:, 0:512].rearrange("p (g q) -> p g q", g=4)[:, :, 0:nq], r=[bk[7]], w=[yatt_t])

        qblock(23, 126, 2, 0, 0)
        for i in range(8):
            qblock(24 + i, 0, 128, 2 + 128 * i, 1 + i)
        P.barrier()
        A.release(m0)

    run_attention()
    P.barrier()
    A.release(m_mixer)

    def run_ffn():
        gout = A.alloc("gout", [128, 2, 8], F32)
        gout_t = Tk("gout")
        P.dma("sp", gout[:, :, :], gout_d[:, :, :], w=[gout_t])
        gffn = A.alloc("gffn", [128, D], F32)
        gffn_t = Tk("gffn")
        P.dma("sp", gffn[:, :], gffn_d[:, :], w=[gffn_t])
        fcw = A.alloc("fcw", [128, 48, 3], F32)
        fcb = A.alloc("fcb", [128, 48], F32)
        fc_t = Tk("fc")
        P.dma("sp", fcw[:, :, :], fcw_d[:, :, :], w=[fc_t])
        P.dma("sp", fcb[:, :], fcb_d[:, :], w=[fc_t])
        h = A.alloc("h", [128, 8, D], F32)
        h_t = [Tk("h%d" % i) for i in range(8)]
        hh = A.alloc("hh", [2, D], F32)
        hh_t = Tk("hh")
        xnT = A.alloc("xnTf", [128, KC, NOWN], BF16)
        xnT_t = Tk("xnTf")
        m1 = A.mark()
        rs = A.alloc("rs", [128, 9, 8], F32)
        rs_t = Tk("rs")
        xnb = A.alloc("xnb", [128, D], BF16)
        xnb_t = Tk("xnb")
        ysq = [A.alloc("ysq", [128, 16, 128], BF16) for _ in range(2)]
        ysq_t = [Tk("ysq0"), Tk("ysq1")]
        wo = [A.alloc("wo", [128, 16, 512], BF16) for _ in range(2)]
        wo_t = [Tk("wo0"), Tk("wo1")]
        tiles = [(0, 2, hh[:, :], hh_t, 3070)] + [(2 + 128 * i, 128, h[:, i, :], h_t[i], 3072 + 128 * i) for i in range(8)]
        for ti, (c0, nt, hap, hapt, u0) in enumerate(tiles):
            P.dma("sp", hap, x_loc[u0:u0 + nt, :], w=[hapt])
            yb = ti % 2
            P.tt("pool", ysq[yb][:, 0:8, 0:nt], ylruT[:, :, c0:c0 + nt], ylruT[:, :, c0:c0 + nt], ALU.mult, r=ylru_t, w=[ysq_t[yb]])
            P.tt("dve", ysq[yb][:, 8:16, 0:nt], yattT[:, :, c0:c0 + nt], yattT[:, :, c0:c0 + nt], ALU.mult, r=[yatt_t], w=[ysq_t[yb]])
            for br in range(2):
                for c in range(8):
                    P.mm(banks[0][0:nt, br:br + 1], ysq[yb][:, br * 8 + c, 0:nt], ones_bf[:, 0:1], c == 0, c == 7,
                         r=[ysq_t[yb], ones_t], w=[bk[0]])
            P.act(rs[0:nt, ti, 0:2], banks[0][0:nt, 0:2], AF.Ln, r=[bk[0]], w=[rs_t], scale=1.0 / 1024, bias=epsb[0:nt, 0:1])
            P.act(rs[0:nt, ti, 2:4], rs[0:nt, ti, 0:2], AF.Exp, r=[rs_t], w=[rs_t], scale=-0.5)
        for c in range(8):
            P.ts("pool", ylruT[:, c, :], ylruT[:, c, :], gout[:, 0, c:c + 1], None, ALU.mult, r=[gout_t] + ysq_t, w=[ylru_t[c]])
            P.ts("dve", yattT[:, c, :], yattT[:, c, :], gout[:, 1, c:c + 1], None, ALU.mult, r=[gout_t] + ysq_t, w=[yatt_t])
        for obk in range(4):
            wb = obk % 2
            for c in range(8):
                P.dma("pool", wo[wb][:, c, :], wol_d[:, c, obk * 512:(obk + 1) * 512], w=[wo_t[wb]])
                P.dma("pool", wo[wb][:, 8 + c, :], woa_d[:, c, obk * 512:(obk + 1) * 512], w=[wo_t[wb]])
            for ti, (c0, nt, hap, hapt, u0) in enumerate(tiles):
                for br in range(2):
                    pb = 2 + (ti % 2) * 2 + br
                    ysrc = ylruT if br == 0 else yattT
                    for c in range(8):
                        P.mm(banks[pb][0:nt, :], ysrc[:, c, c0:c0 + nt], wo[wb][:, br * 8 + c, :], c == 0, c == 7,
                             r=[ylru_t[c] if br == 0 else yatt_t, wo_t[wb]], w=[bk[pb]])
                    P.stt(hap[:, obk * 512:(obk + 1) * 512], banks[pb][0:nt, :], rs[0:nt, ti, 2 + br:3 + br],
                          hap[:, obk * 512:(obk + 1) * 512], ALU.mult, ALU.add, r=[bk[pb], rs_t, hapt], w=[hapt])
        for ti, (c0, nt, hap, hapt, u0) in enumerate(tiles):
            P.act(xnb[0:nt, :], hap, AF.Square, r=[hapt], w=[xnb_t, rs_t], accum_out=rs[0:nt, ti, 4:5])
            P.act(rs[0:nt, ti, 5:6], rs[0:nt, ti, 4:5], AF.Ln, r=[rs_t], w=[rs_t], scale=1.0 / D, bias=epsb[0:nt, 0:1])
            P.act(rs[0:nt, ti, 6:7], rs[0:nt, ti, 5:6], AF.Exp, r=[rs_t], w=[rs_t], scale=-0.5)
            P.stt(xnb[0:nt, :], hap, rs[0:nt, ti, 6:7], gffn[0:nt, :], ALU.mult, ALU.mult, r=[hapt, rs_t, gffn_t], w=[xnb_t])
            for half in range(2):
                for j in range(8):
                    jj = half * 8 + j
                    P.tr(bf(banks[6 + half][:, :])[:, j * 128:j * 128 + nt],
                         xnb[0:nt, jj * 128:(jj + 1) * 128], ident[0:nt, 0:nt], r=[xnb_t, ident_t], w=[bk[6 + half]])
                P.cp("dve" if half == 0 else "act", xnT[:, half * 8:(half + 1) * 8, c0:c0 + nt],
                     bf(banks[6 + half][:, :]).rearrange("p (j t) -> p j t", j=8)[:, :, 0:nt], r=[bk[6 + half]], w=[xnT_t])
        P.barrier()
        A.release(m1)
        wup = [nc.alloc_sbuf_tensor_at("wup%d" % i, [128, KC, 512], BF16, offset=y_base + i * 16384) for i in range(2)]
        assert y_base + 2 * 16384 <= y_end
        wup_t = [Tk("wup0"), Tk("wup1")]
        m2 = A.mark()
        wdn = [A.alloc("wdn", [128, 2, D], BF16) for _ in range(3)]
        wdn_t = [Tk("wdn0"), Tk("wdn1"), Tk("wdn2")]
        gT = [A.alloc("gT", [128, 2, 1024], BF16) for _ in range(2)]
        gT_t = [Tk("gT0"), Tk("gT1")]
        usb = [A.alloc("usb", [128, NOWN], F32) for _ in range(2)]
        usb_t = [Tk("usb0"), Tk("usb1")]
        vsb = [A.alloc("vsb", [128, NOWN], F32) for _ in range(2)]
        vsb_t = [Tk("vsb0"), Tk("vsb1")]
        uc = A.alloc("uc", [128, 1024], F32)
        uc_t = Tk("uc")
        gg = A.alloc("gg", [128, 1024], F32)
        gg_t = Tk("gg")
        CT = 342
        rotu = [0]
        def prefetch(G):
            P.dma("pool", wup[G % 2][:, :, :], wup_d[G, :, :, :], w=[wup_t[G % 2]])
            P.dma("pool", wdn[G % 3][:, :, :], wdn_d[G, :, :, :], w=[wdn_t[G % 3]])

        def up(G):
            gb = G % 2
            for fc in range(2):
                fa = 2 * G + fc
                ub = fa % 2
                for ct in range(3):
                    pu = rotu[0] % 2
                    rotu[0] += 1
                    bu, bv = pu * 2, pu * 2 + 1
                    for k in range(KC):
                        P.mm(banks[bu][:, 0:CT], wup[gb][:, k, fc * 128:(fc + 1) * 128], xnT[:, k, ct * CT:(ct + 1) * CT], k == 0, k == KC - 1,
                             r=[wup_t[gb], xnT_t], w=[bk[bu]])
                    for k in range(KC):
                        P.mm(banks[bv][:, 0:CT], wup[gb][:, k, 256 + fc * 128:256 + (fc + 1) * 128], xnT[:, k, ct * CT:(ct + 1) * CT], k == 0, k == KC - 1,
                             r=[wup_t[gb], xnT_t], w=[bk[bv]])
                    P.cp("act", usb[ub][:, ct * CT:(ct + 1) * CT], banks[bu][:, 0:CT], r=[bk[bu]], w=[usb_t[ub]])
                    P.cp("act", vsb[ub][:, ct * CT:(ct + 1) * CT], banks[bv][:, 0:CT], r=[bk[bv]], w=[vsb_t[ub]])
                P.ts("dve", uc[:, :], usb[ub][:, 2:1026], fcw[:, fa, 2:3], fcb[:, fa:fa + 1], ALU.mult, ALU.add, r=[usb_t[ub], fc_t], w=[uc_t])
                P.stt(uc[:, :], usb[ub][:, 1:1025], fcw[:, fa, 1:2], uc[:, :], ALU.mult, ALU.add, r=[usb_t[ub], fc_t, uc_t], w=[uc_t])
                P.stt(uc[:, :], usb[ub][:, 0:1024], fcw[:, fa, 0:1], uc[:, :], ALU.mult, ALU.add, r=[usb_t[ub], fc_t, uc_t], w=[uc_t])
                gelu(gg[:, :], uc[:, :], gg[:, :], [uc_t], gg_t, gg_t)
                P.tt("pool", gT[gb][:, fc, :], gg[:, :], vsb[ub][:, 2:1026], ALU.mult, r=[gg_t, vsb_t[ub]], w=[gT_t[gb]])

        def down(G):
            gb = G % 2
            wd = G % 3
            for tt in range(8):
                for obk in range(4):
                    pb = 4 + (tt * 4 + obk) % 4
                    for fc in range(2):
                        P.mm(banks[pb][:, :], gT[gb][:, fc, tt * 128:(tt + 1) * 128], wdn[wd][:, fc, obk * 512:(obk + 1) * 512], fc == 0, fc == 1,
                             r=[gT_t[gb], wdn_t[wd]], w=[bk[pb]])
                    P.tt("dve", h[:, tt, obk * 512:(obk + 1) * 512], banks[pb][:, :], h[:, tt, obk * 512:(obk + 1) * 512], ALU.add,
                         r=[bk[pb], h_t[tt]], w=[h_t[tt]])

        prefetch(0)
        for G in range(24):
            if G + 1 < 24:
                prefetch(G + 1)
            up(G)
            if G > 0:
                down(G - 1)
        down(23)
        P.barrier()
        A.release(m2)
        gfin = A.alloc("gfin", [128, D], F32)
        gfin_t = Tk("gfin")
        P.dma("sp", gfin[:, :], gfin_d[:, :], w=[gfin_t])
        fs = A.alloc("fs", [128, 8, 4], F32)
        fs_t = Tk("fs")
        xs2 = A.alloc("xs2", [128, D], BF16)
        xs2_t = Tk("xs2")
        for tt in range(8):
            P.act(xs2[:, :], h[:, tt, :], AF.Square, r=[h_t[tt]], w=[xs2_t, fs_t], accum_out=fs[:, tt, 0:1])
            P.act(fs[:, tt, 1:2], fs[:, tt, 0:1], AF.Ln, r=[fs_t], w=[fs_t], scale=1.0 / D, bias=epsb[:, 0:1])
            P.act(fs[:, tt, 2:3], fs[:, tt, 1:2], AF.Exp, r=[fs_t], w=[fs_t], scale=-0.5)
            P.stt(h[:, tt, :], h[:, tt, :], fs[:, tt, 2:3], gfin[:, :], ALU.mult, ALU.mult, r=[h_t[tt], fs_t, gfin_t], w=[h_t[tt]])
            P.dma("sp", y_out[tt * 128:(tt + 1) * 128, :], h[:, tt, :], r=[h_t[tt]], is_output=True)

    run_ffn()
    P.finish()
    return nc


def _tile_k(w):
    n = w.shape[1]
    return np.ascontiguousarray(w.reshape(KC, 128, n).transpose(1, 0, 2))


def _host_consts():
    c = {}
    c["ident"] = np.eye(128, dtype=np.float32).astype(ml_dtypes.bfloat16)
    eb = np.zeros((64, S), np.float32)
    for s in range(64):
        eb[s, s * 64:(s + 1) * 64] = 1.0
    c["ebig"] = eb.astype(ml_dtypes.bfloat16)
    n = np.arange(256)
    s = np.arange(64)
    ov = np.clip(np.minimum(n[:, None] * 16 + 32, s[None, :] * 64 + 64) - np.maximum(n[:, None] * 16, s[None, :] * 64), 0, None)
    ov = (ov / 32.0).astype(np.float32)
    ov[255] = 0.0
    c["ovl"] = np.ascontiguousarray(ov.reshape(2, 128, 64).transpose(1, 0, 2))

    def wide(hi):
        rel = np.arange(128)[None, :] - 64
        causal = (rel <= hi[:, None]).astype(np.float32)
        forced = ((rel <= hi[:, None]) & (rel > hi[:, None] - 2)).astype(np.float32)
        return np.ascontiguousarray(np.stack([causal, causal - 1.0, forced * 1e4], axis=1).astype(np.float32))

    c["cwide"] = wide((np.arange(128) >= 64).astype(np.int64))
    c["cwide_h"] = wide(np.array([1, 1], np.int64))
    return c


_NC_CACHE = {}


def kernel(**inp):
    f32 = np.float32
    g = {k: np.asarray(v, dtype=f32) for k, v in inp.items()}
    x = g["x"]
    w_in = g["w_in"][0]
    rep = {}
    rep["wA1"] = _tile_k(w_in[:, 0:1024])
    rep["wB3"] = _tile_k(w_in[:, 1024:2048])
    rep["wB2"] = _tile_k(w_in[:, 2048:3072])
    rep["wA2"] = _tile_k(np.concatenate([w_in[:, 3072:3584], w_in[:, 3584:3840], w_in[:, 3840:4096]], axis=1))
    rep["wB1"] = _tile_k(np.concatenate([w_in[:, 4096:4352], w_in[:, 4352:4608], w_in[:, 4608:4632]], axis=1))
    rep["gmixT"] = np.ascontiguousarray(g["g_mix"][0].reshape(KC, 128).T)
    rep["lcw"] = np.ascontiguousarray(g["lru_conv_w"][0].reshape(4, 8, 128).transpose(2, 1, 0))
    rep["lvec"] = np.ascontiguousarray(np.stack([g["lru_conv_b"][0], g["lru_ba"][0], g["lru_bx"][0], g["lru_lambda"][0]], 0)
                                       .reshape(4, 8, 128).transpose(2, 0, 1))
    rep["wa"] = np.ascontiguousarray(g["lru_wa"][0].transpose(1, 0, 2))
    rep["wx"] = np.ascontiguousarray(g["lru_wx"][0].transpose(1, 0, 2))
    rep["w1k"] = np.ascontiguousarray(g["cmp_w1_k"][0].reshape(32, 128, 128).transpose(1, 0, 2))
    rep["w1v"] = np.ascontiguousarray(g["cmp_w1_v"][0].reshape(32, 128, 128).transpose(1, 0, 2))
    rep["peT"] = np.ascontiguousarray(np.stack([g["cmp_pe_k"][0].T, g["cmp_pe_v"][0].T], axis=1))
    rep["cmpb"] = np.ascontiguousarray(np.stack([g["cmp_b1_k"][0], g["cmp_b1_v"][0]], axis=1))
    rep["w2k"] = np.ascontiguousarray(g["cmp_w2_k"][0])
    rep["w2v"] = np.ascontiguousarray(g["cmp_w2_v"][0])
    w_out = g["w_out"][0]
    rep["wol"] = np.ascontiguousarray(w_out[0:1024].reshape(8, 128, D).transpose(1, 0, 2))
    rep["woa"] = np.ascontiguousarray(w_out[1024:2048].reshape(8, 128, D).transpose(1, 0, 2))
    rep["gout"] = np.ascontiguousarray(np.stack([g["g_lru_out"][0].reshape(8, 128).T, g["g_attn_out"][0].reshape(8, 128).T], axis=1))
    rep["gffn_bc"] = np.ascontiguousarray(np.broadcast_to(g["g_ffn"][0][None, :], (128, D)))
    rep["gfin_bc"] = np.ascontiguousarray(np.broadcast_to(g["g_final"][None, :], (128, D)))
    w_up = g["w_up"][0]
    wu = w_up[:, 0:6144].reshape(KC, 128, 24, 256)
    wv = w_up[:, 6144:12288].reshape(KC, 128, 24, 256)
    rep["wup"] = np.ascontiguousarray(np.concatenate([wu, wv], axis=3).transpose(2, 1, 0, 3))
    rep["wdn"] = np.ascontiguousarray(g["w_down"][0].reshape(24, 2, 128, D).transpose(0, 2, 1, 3))
    rep["fcw"] = np.ascontiguousarray(g["ffn_conv_w"][0].reshape(3, 48, 128).transpose(2, 1, 0))
    rep["fcb"] = np.ascontiguousarray(g["ffn_conv_b"][0].reshape(48, 128).T)
    rep.update(_host_consts())

    in_maps = []
    for c in range(8):
        b, r = c // 4, c % 4
        pad = 1024 * (3 - r)
        xl = np.zeros((S, D), f32)
        xl[pad:] = x[b, 0:S - pad]
        valid = np.zeros((S,), f32)
        valid[pad:] = 1.0
        m = dict(rep)
        m["x_loc"] = xl
        m["stflag"] = np.ascontiguousarray(np.broadcast_to(valid[0:S:512][None, :], (128, 8)))
        m["validT"] = np.ascontiguousarray(valid.reshape(32, 128).T)
        vn = np.zeros((256,), f32)
        vn[pad // 16:255] = 1.0
        m["validn"] = np.ascontiguousarray(vn.reshape(2, 128).T)
        f0 = np.zeros((128, 64), f32)
        f0[:, pad // 64] = 1e4
        m["f0"] = f0
        in_maps.append(m)

    if "nc" not in _NC_CACHE:
        _NC_CACHE["nc"] = build_program()
    res = run_bass_kernel_spmd(_NC_CACHE["nc"], in_maps, core_ids=list(range(8)))
    out = np.zeros((2, S, D), f32)
    for c in range(8):
        b, r = c // 4, c % 4
        out[b, r * 1024:(r + 1) * 1024] = np.asarray(res.results[c]["y"], dtype=f32)
    return out
```

```python
import numpy as np
import ml_dtypes
import concourse.bass as bass
import concourse.mybir as mybir
from concourse.bass_utils import run_bass_kernel_spmd

F32 = mybir.dt.float32
BF16 = mybir.dt.bfloat16
AF = mybir.ActivationFunctionType
ALU = mybir.AluOpType

D = 2048
KC = 16
S = 4096
NOWN = 1026
EPS = 1e-6
NDMA = 12
USE_GELU_TANH_LUT = True


class Tk:
    __slots__ = ("name", "w", "r")

    def __init__(self, name):
        self.name = name
        self.w = None
        self.r = []


class Prog:
    def __init__(self, nc):
        self.nc = nc
        self.engs = ("pe", "act", "dve", "pool", "sp")
        self.sem = {k: nc.alloc_semaphore("sem_" + k) for k in self.engs}
        self.cnt = {k: 0 for k in self.engs}
        self.streams = {k: [] for k in self.engs}
        self.waited = {k: {} for k in self.engs}
        self.dq = {q: [[nc.alloc_semaphore("dq_%s_%d" % (q, i)), 0] for i in range(NDMA)] for q in ("sp", "pool")}
        self.dq_rr = {"sp": 0, "pool": 0}
        self.out_events = []

    def _wait(self, eng, evs):
        best = {}
        for ev in evs:
            if ev is None:
                continue
            key, sem, val = ev
            if key == eng and eng == "pe":
                continue
            if self.waited[eng].get(key, 0) >= val:
                continue
            if key not in best or best[key][1] < val:
                best[key] = (sem, val)
        for key, (sem, val) in best.items():
            self.waited[eng][key] = val
            self.streams[eng].append(lambda e, sem=sem, val=val: e.wait_ge(sem, val))

    def _deps(self, r, w):
        evs = []
        for t in r:
            evs.append(t.w)
        for t in w:
            evs.append(t.w)
            evs.extend(t.r)
        return evs

    def op(self, eng, fn, r=(), w=()):
        self._wait(eng, self._deps(r, w))
        self.cnt[eng] += 1
        sem = self.sem[eng]
        self.streams[eng].append(lambda e, fn=fn, sem=sem: fn(e).then_inc(sem, 1))
        ev = (eng, sem, self.cnt[eng])
        for t in r:
            t.r.append(ev)
        for t in w:
            t.w = ev
            t.r = []
        return ev

    def dma(self, q, out, in_, r=(), w=(), is_output=False):
        slot = self.dq[q][self.dq_rr[q] % NDMA]
        self.dq_rr[q] += 1
        sem = slot[0]
        key = "dma_" + str(id(slot))
        evs = self._deps(r, w)
        if slot[1] > 0:
            evs.append((key, sem, slot[1]))
        self._wait(q, evs)
        slot[1] += 16
        val = slot[1]
        self.streams[q].append(lambda e, out=out, in_=in_, sem=sem: e.dma_start(out=out, in_=in_).then_inc(sem, 16))
        ev = (key, sem, val)
        for t in r:
            t.r.append(ev)
        for t in w:
            t.w = ev
            t.r = []
        if is_output:
            self.out_events.append(ev)
        return ev

    def barrier(self):
        evs = [(k, self.sem[k], self.cnt[k]) for k in self.engs if self.cnt[k] > 0]
        for q in ("sp", "pool"):
            for slot in self.dq[q]:
                if slot[1] > 0:
                    evs.append(("dma_" + str(id(slot)), slot[0], slot[1]))
        for e in self.engs:
            self._wait(e, [ev for ev in evs if ev[0] != e])

    def finish(self):
        self.barrier()
        self._wait("sp", self.out_events)
        nc = self.nc
        st = self.streams
        with nc.Block() as block:
            @block.tensor
            def _(e):
                for f in st["pe"]:
                    f(e)

            @block.scalar
            def _(e):
                for f in st["act"]:
                    f(e)

            @block.vector
            def _(e):
                for f in st["dve"]:
                    f(e)

            @block.gpsimd
            def _(e):
                for f in st["pool"]:
                    f(e)

            @block.sync
            def _(e):
                for f in st["sp"]:
                    f(e)

    def act(self, out, in_, func, r=(), w=(), **kw):
        return self.op("act", lambda e: e.activation(out=out, in_=in_, func=func, **kw), r, w)

    def mm(self, out, lhsT, rhs, start, stop, r=(), w=()):
        return self.op("pe", lambda e: e.matmul(out, lhsT, rhs, start=start, stop=stop), r, w)

    def tr(self, out, in_, ident, r=(), w=()):
        return self.op("pe", lambda e: e.transpose(out, in_, ident), r, w)

    def tt(self, eng, out, in0, in1, op, r=(), w=()):
        return self.op(eng, lambda e: e.tensor_tensor(out=out, in0=in0, in1=in1, op=op), r, w)

    def ts(self, eng, out, in0, s1, s2, op0, op1=None, r=(), w=()):
        if op1 is None:
            return self.op(eng, lambda e: e.tensor_scalar(out=out, in0=in0, scalar1=s1, scalar2=None, op0=op0), r, w)
        return self.op(eng, lambda e: e.tensor_scalar(out=out, in0=in0, scalar1=s1, scalar2=s2, op0=op0, op1=op1), r, w)

    def stt(self, out, in0, scalar, in1, op0, op1, r=(), w=()):
        return self.op("dve", lambda e: e.scalar_tensor_tensor(out=out, in0=in0, scalar=scalar, in1=in1, op0=op0, op1=op1), r, w)

    def cp(self, eng, out, in_, r=(), w=()):
        if eng == "act":
            return self.act(out, in_, AF.Copy, r, w)
        return self.op(eng, lambda e: e.tensor_copy(out=out, in_=in_), r, w)


class Arena:
    def __init__(self, nc, base, limit):
        self.nc = nc
        self.off = base
        self.limit = limit
        self.n = 0

    def alloc(self, name, shape, dt):
        esz = 2 if dt == BF16 else 4
        nb = esz
        for s in shape[1:]:
            nb *= s
        nb = (nb + 31) // 32 * 32
        t = self.nc.alloc_sbuf_tensor_at("%s_%d" % (name, self.n), list(shape), dt, offset=self.off)
        self.n += 1
        self.off += nb
        assert self.off <= self.limit, ("SBUF overflow", name, self.off, self.limit)
        return t

    def mark(self):
        return self.off

    def release(self, m):
        self.off = m


def build_program():
    nc = bass.Bass("TRN2", target_bir_lowering=False)
    P = Prog(nc)

    def din(name, shape, dt=F32):
        return nc.dram_tensor(name, list(shape), dt, kind="ExternalInput").ap()

    x_loc = din("x_loc", [S, D])
    stflag_d = din("stflag", [128, 8])
    validT_d = din("validT", [128, 32])
    validn_d = din("validn", [128, 2])
    f0_d = din("f0", [128, 64])
    wA1_d = din("wA1", [128, KC, 1024])
    wB3_d = din("wB3", [128, KC, 1024])
    wA2_d = din("wA2", [128, KC, 1024])
    wB1_d = din("wB1", [128, KC, 536])
    wB2_d = din("wB2", [128, KC, 1024])
    gmixT_d = din("gmixT", [128, KC])
    lcw_d = din("lcw", [128, 8, 4])
    lvec_d = din("lvec", [128, 4, 8])
    wa_d = din("wa", [128, 8, 128])
    wx_d = din("wx", [128, 8, 128])
    w1k_d = din("w1k", [128, 32, 128])
    w1v_d = din("w1v", [128, 32, 128])
    pe_d = din("peT", [128, 2, 32])
    cmpb_d = din("cmpb", [128, 2])
    w2k_d = din("w2k", [128, 128])
    w2v_d = din("w2v", [128, 128])
    ident_d = din("ident", [128, 128], BF16)
    ebig_d = din("ebig", [64, S], BF16)
    ovl_d = din("ovl", [128, 2, 64])
    cwide_d = din("cwide", [128, 3, 128])
    cwide_h_d = din("cwide_h", [2, 3, 128])
    wol_d = din("wol", [128, 8, D])
    woa_d = din("woa", [128, 8, D])
    gout_d = din("gout", [128, 2, 8])
    gffn_d = din("gffn_bc", [128, D])
    gfin_d = din("gfin_bc", [128, D])
    wup_d = din("wup", [24, 128, KC, 512])
    wdn_d = din("wdn", [24, 128, 2, D])
    fcw_d = din("fcw", [128, 48, 3])
    fcb_d = din("fcb", [128, 48])
    y_out = nc.dram_tensor("y", [1024, D], F32, kind="ExternalOutput").ap()

    wsc = {}
    wsc_t = {}
    for nm, ncl in (("B3", 1024), ("A2", 1024), ("B1", 536), ("B2", 1024)):
        wsc[nm] = nc.dram_tensor("wsc_" + nm, [128, KC, ncl], BF16, kind="Internal").ap()
        wsc_t[nm] = Tk("wsc_" + nm)
    wraw = {"B3": wB3_d, "A2": wA2_d, "B1": wB1_d, "B2": wB2_d}
    prep_chunks = []

    base = (int(nc.sbuf_base) + 63) // 64 * 64
    A = Arena(nc, base, int(nc.sbuf_base) + int(nc.sbuf_bytes_remaining) - 64)
    banks = [nc.alloc_psum_tensor("bank%d" % i, [128, 512], F32) for i in range(8)]
    bk = [Tk("bank%d" % i) for i in range(8)]

    def bf(bank_ap):
        return bank_ap.bitcast(BF16)

    ident = A.alloc("ident", [128, 128], BF16)
    ident_t = Tk("ident")
    P.dma("sp", ident[:, :], ident_d[:, :], w=[ident_t])
    gmixT = A.alloc("gmixT", [128, KC], F32)
    gmixT_t = Tk("gmixT")
    P.dma("sp", gmixT[:, :], gmixT_d[:, :], w=[gmixT_t])
    ones_bf = A.alloc("ones", [128, 2], BF16)
    ones_t = Tk("ones")
    P.op("dve", lambda e: e.memset(ones_bf[:, :], 1.0), w=[ones_t])

    epsb = A.alloc("epsb", [128, 1], F32)
    epsb_t = Tk("epsb")
    P.op("dve", lambda e: e.memset(epsb[:, :], EPS), w=[epsb_t])
    oneb = A.alloc("oneb", [128, 1], F32)
    oneb_t = Tk("oneb")
    P.op("dve", lambda e: e.memset(oneb[:, :], 1.0), w=[oneb_t])
    y_base = A.mark()
    ylruT = A.alloc("ylruT", [128, 8, NOWN], BF16)
    ylru_t = [Tk("ylru%d" % c) for c in range(8)]
    yattT = A.alloc("yattT", [128, 8, NOWN], BF16)
    yatt_t = Tk("yatt")
    y_end = A.mark()

    m_mixer = A.mark()

    def proj_pass(w_dram, ncols, sts, groups, nrot=6, scratch=None):
        m0 = A.mark()
        W = A.alloc("W", [128, KC, ncols], BF16)
        W_q = [Tk("Wq%d" % i) for i in range(4)]
        W_t = [W_q[k // 4] for k in range(KC)] if scratch is not None else [Tk("W%d" % k) for k in range(KC)]
        xt = [A.alloc("xt", [128, D], F32) for _ in range(2)]
        xt_t = [Tk("xt0"), Tk("xt1")]
        if scratch is not None:
            for q4 in range(4):
                P.dma("sp", W[:, 4 * q4:4 * q4 + 4, :], wsc[scratch][:, 4 * q4:4 * q4 + 4, :], r=[wsc_t[scratch]], w=[W_q[q4]])
        else:
            slots = [xt[0][:, 0:1024], xt[0][:, 1024:2048], xt[1][:, 0:1024], xt[1][:, 1024:2048]]
            slot_t = [Tk("wslot%d" % i) for i in range(4)]
            for k in range(KC):
                i4 = k % 4
                P.dma("sp", slots[i4][:, 0:ncols], w_dram[:, k, :], w=[slot_t[i4]])
                P.ts("dve" if k % 2 == 0 else "pool", W[:, k, :], slots[i4][:, 0:ncols], gmixT[:, k:k + 1], None, ALU.mult,
                     r=[slot_t[i4], gmixT_t], w=[W_t[k]])
            for i4 in range(4):
                xt_t[i4 // 2].r.extend(slot_t[i4].r)
        xn = [A.alloc("xn", [128, D], BF16) for _ in range(2)]
        xn_t = [Tk("xn0"), Tk("xn1")]
        ss = [A.alloc("ss", [128, 4], F32) for _ in range(2)]
        ss_t = [Tk("ss0"), Tk("ss1")]
        xnT = [A.alloc("xnT", [128, KC, 512], BF16) for _ in range(2)]
        xnT_t = [Tk("xnT0"), Tk("xnT1")]
        ctx = dict(W=W, W_t=W_t)
        for g in groups:
            if "setup" in g:
                g["setup"](ctx)
        rot = [2]

        def prep_tile(si, tt):
            tk = sts[si] * 4 + tt
            b = tk % 2
            P.dma("sp", xt[b][:, :], x_loc[tk * 128:(tk + 1) * 128, :], w=[xt_t[b]])
            P.act(xn[b][:, :], xt[b][:, :], AF.Square, r=[xt_t[b]], w=[xn_t[b], ss_t[b]], accum_out=ss[b][:, 0:1])
            P.act(ss[b][:, 1:2], ss[b][:, 0:1], AF.Ln, r=[ss_t[b]], w=[ss_t[b]], scale=1.0 / D, bias=epsb[:, 0:1])
            P.act(ss[b][:, 2:3], ss[b][:, 1:2], AF.Exp, r=[ss_t[b]], w=[ss_t[b]], scale=-0.5)
            P.act(xn[b][:, :], xt[b][:, :], AF.Copy, r=[xt_t[b], ss_t[b]], w=[xn_t[b]], scale=ss[b][:, 2:3])

        def prep_tr(si, tt):
            tk = sts[si] * 4 + tt
            b = tk % 2
            xb = si % 2
            for half in range(2):
                pb = banks[half]
                for j in range(8):
                    jj = half * 8 + j
                    P.tr(bf(pb[:, :])[:, j * 128:(j + 1) * 128], xn[b][:, jj * 128:(jj + 1) * 128], ident[:, :],
                         r=[xn_t[b], ident_t], w=[bk[half]])
                P.cp("dve" if half == 0 else "act",
                     xnT[xb][:, half * 8:(half + 1) * 8, tt * 128:(tt + 1) * 128],
                     bf(pb[:, :]).rearrange("p (j t) -> p j t", j=8),
                     r=[bk[half]], w=[xnT_t[xb]])

        def make_units(si):
            st = sts[si]
            xb = si % 2
            units = []
            for g in groups:
                if st not in g.get("sts", sts):
                    continue
                if g["kind"] == "fm":
                    for j in range(g["ncols"] // 128):
                        def mmf(g=g, j=j):
                            bi = rot[0]
                            rot[0] = 2 + (rot[0] - 1) % nrot
                            for k in range(KC):
                                P.mm(banks[bi][:, :], W[:, k, g["col0"] + j * 128: g["col0"] + (j + 1) * 128], xnT[xb][:, k, :],
                                     k == 0, k == KC - 1, r=[W_t[k], xnT_t[xb]], w=[bk[bi]])
                            return bi
                        units.append((mmf, lambda bi, g=g, j=j: g["evac"](st, j, banks[bi], bk[bi])))
                else:
                    nco = g["ncols"]
                    for tt in range(4):
                        def mmf(g=g, tt=tt, nco=nco):
                            bi = rot[0]
                            rot[0] = 2 + (rot[0] - 1) % nrot
                            for k in range(KC):
                                P.mm(banks[bi][:, 0:nco], xnT[xb][:, k, tt * 128:(tt + 1) * 128], W[:, k, g["col0"]: g["col0"] + nco],
                                     k == 0, k == KC - 1, r=[W_t[k], xnT_t[xb]], w=[bk[bi]])
                            return bi
                        units.append((mmf, lambda bi, g=g, tt=tt: g["evac"](st, tt, banks[bi], bk[bi])))
            return units

        for tt in range(4):
            prep_tile(0, tt)
            prep_tr(0, tt)
        for si in range(len(sts)):
            units = make_units(si)
            n = len(units)
            sched = {}
            if si + 1 < len(sts):
                for k in range(4):
                    u_tile = (k * n) // 4
                    sched.setdefault(u_tile, []).append(("tile", k))
                    u_tr = min(n - 1, u_tile + max(1, n // 8))
                    sched.setdefault(u_tr, []).append(("tr", k))
            pending = None
            for ui, (mmf, evf) in enumerate(units):
                for kind, k in sched.get(ui, []):
                    if kind == "tile":
                        prep_tile(si + 1, k)
                    else:
                        prep_tr(si + 1, k)
                bi = mmf()
                if pending is not None:
                    pending[0](pending[1])
                pending = (evf, bi)
            pending[0](pending[1])
        P.barrier()
        A.release(m0)


    def run_lru():
        m0 = A.mark()
        h_own = A.alloc("h_own", [128, 8, NOWN], F32)
        h_own_t = [Tk("h_own%d" % c) for c in range(8)]
        m_l = A.mark()
        lcw = A.alloc("lcw", [128, 8, 4], F32)
        lvec = A.alloc("lvec", [128, 4, 8], F32)
        c12 = A.alloc("c12", [128, 2, 8], F32)
        lsm_t = Tk("lsm")
        P.dma("sp", lcw[:, :, :], lcw_d[:, :, :], w=[lsm_t])
        P.dma("sp", lvec[:, :, :], lvec_d[:, :, :], w=[lsm_t])
        P.act(c12[:, 0, :], lvec[:, 3, :], AF.Exp, r=[lsm_t], w=[lsm_t], scale=-1.0)
        P.act(c12[:, 0, :], c12[:, 0, :], AF.Ln, r=[lsm_t], w=[lsm_t], bias=oneb[:, 0:1])
        P.ts("dve", c12[:, 1, :], c12[:, 0, :], -16.0, None, ALU.mult, r=[lsm_t], w=[lsm_t])
        P.ts("dve", c12[:, 0, :], c12[:, 0, :], -8.0, None, ALU.mult, r=[lsm_t], w=[lsm_t])
        wa = A.alloc("wa", [128, 8, 128], F32)
        wx = A.alloc("wx", [128, 8, 128], F32)
        wax_t = Tk("wax")
        P.dma("sp", wa[:, :, :], wa_d[:, :, :], w=[wax_t])
        P.dma("sp", wx[:, :, :], wx_d[:, :, :], w=[wax_t])
        stf = A.alloc("stf", [128, 8], F32)
        stf_t = Tk("stf")
        P.dma("sp", stf[:, :], stflag_d[:, :], w=[stf_t])
        hin = A.alloc("hin", [128, 8], F32)
        hin_t = [Tk("hin%d" % c) for c in range(8)]
        xbuf = A.alloc("xbuf", [128, 8, 515], F32)
        xbuf_t = [Tk("xbuf%d" % c) for c in range(8)]
        hst = A.alloc("hst", [128, 8], F32)
        hst_t = [Tk("hst%d" % c) for c in range(8)]
        for c in range(8):
            P.op("pool", lambda e, c=c: e.memset(xbuf[:, c, 0:3], 0.0), w=[xbuf_t[c]])
        NB = 2
        tmp = {}
        tmp_t = {}
        for nm in ("u", "r", "i", "a", "h"):
            tmp[nm] = [A.alloc("l" + nm, [128, 512], F32) for _ in range(NB)]
            tmp_t[nm] = [Tk("l%s%d" % (nm, b)) for b in range(NB)]
        tmp["s"], tmp_t["s"] = tmp["r"], tmp_t["r"]
        tmp["v"], tmp_t["v"] = tmp["i"], tmp_t["i"]
        pf = [A.alloc("pf", [128, 512], F32) for _ in range(2)]
        pf_t = [Tk("pf0"), Tk("pf1")]
        pb_ = [A.alloc("pb", [128, 512], BF16) for _ in range(2)]
        pb_t = [Tk("pb0"), Tk("pb1")]
        pctr = [0]
        for nm_ in ("B3", "A2", "B1", "B2"):
            ncl = wraw[nm_].shape[2]
            for k in range(KC):
                for c0 in range(0, ncl, 512):
                    c1 = min(ncl, c0 + 512)

                    def chunk(nm_=nm_, k=k, c0=c0, c1=c1):
                        i2 = pctr[0] % 2
                        pctr[0] += 1
                        n_ = c1 - c0
                        P.dma("pool", pf[i2][:, 0:n_], wraw[nm_][:, k, c0:c1], w=[pf_t[i2]])
                        P.ts("pool", pb_[i2][:, 0:n_], pf[i2][:, 0:n_], gmixT[:, k:k + 1], None, ALU.mult,
                             r=[pf_t[i2], gmixT_t], w=[pb_t[i2]])
                        P.dma("pool", wsc[nm_][:, k, c0:c1], pb_[i2][:, 0:n_], r=[pb_t[i2]], w=[wsc_t[nm_]])
                    prep_chunks.append(chunk)
        gps = [6, 7]

        def evac(st, c, ps, ps_t):
            b = c % NB
            for _ in range(3):
                if prep_chunks:
                    prep_chunks.pop(0)()
            if st > 0:
                P.cp("dve", xbuf[:, c, 0:3], xbuf[:, c, 512:515], r=[xbuf_t[c]], w=[xbuf_t[c]])
            P.cp("act", xbuf[:, c, 3:515], ps[:, :], r=[ps_t], w=[xbuf_t[c]])
            u = tmp["u"][b]
            ut = tmp_t["u"][b]
            P.ts("dve", u[:, :], xbuf[:, c, 3:515], lcw[:, c, 3:4], lvec[:, 0, c:c + 1], ALU.mult, ALU.add,
                 r=[xbuf_t[c], lsm_t], w=[ut])
            for j in range(3):
                P.stt(u[:, :], xbuf[:, c, j:j + 512], lcw[:, c, j:j + 1], u[:, :], ALU.mult, ALU.add,
                      r=[xbuf_t[c], lsm_t, ut], w=[ut])
            P.mm(banks[gps[0]][:, :], wa[:, c, :], u[:, :], True, True, r=[wax_t, ut], w=[bk[gps[0]]])
            P.mm(banks[gps[1]][:, :], wx[:, c, :], u[:, :], True, True, r=[wax_t, ut], w=[bk[gps[1]]])
            rr, ii, aa, sq, vv = tmp["r"][b], tmp["i"][b], tmp["a"][b], tmp["s"][b], tmp["v"][b]
            P.act(rr[:, :], banks[gps[0]][:, :], AF.Sigmoid, r=[bk[gps[0]], lsm_t], w=[tmp_t["r"][b]], bias=lvec[:, 1, c:c + 1])
            P.act(ii[:, :], banks[gps[1]][:, :], AF.Sigmoid, r=[bk[gps[1]], lsm_t], w=[tmp_t["i"][b]], bias=lvec[:, 2, c:c + 1])
            P.act(aa[:, :], rr[:, :], AF.Exp, r=[tmp_t["r"][b], lsm_t], w=[tmp_t["a"][b]], scale=c12[:, 0, c:c + 1])
            P.act(sq[:, :], rr[:, :], AF.Exp, r=[tmp_t["r"][b], lsm_t], w=[tmp_t["s"][b]], scale=c12[:, 1, c:c + 1])
            P.act(sq[:, :], sq[:, :], AF.Ln, r=[tmp_t["s"][b], oneb_t], w=[tmp_t["s"][b]], scale=-1.0, bias=oneb[:, 0:1])
            P.act(sq[:, :], sq[:, :], AF.Exp, r=[tmp_t["s"][b]], w=[tmp_t["s"][b]], scale=0.5)
            P.tt("dve", vv[:, :], ii[:, :], u[:, :], ALU.mult, r=[tmp_t["i"][b], ut], w=[tmp_t["v"][b]])
            P.tt("dve", vv[:, :], vv[:, :], sq[:, :], ALU.mult, r=[tmp_t["s"][b], tmp_t["v"][b]], w=[tmp_t["v"][b]])
            if st >= 6:
                hout = h_own[:, c, 2 + (st - 6) * 512: 2 + (st - 5) * 512]
                ht = h_own_t[c]
            else:
                hout = tmp["h"][b][:, :]
                ht = tmp_t["h"][b]
            if st == 0:
                init = 0.0
            else:
                P.tt("dve", hin[:, c:c + 1], hst[:, c:c + 1], stf[:, st - 1:st], ALU.mult, r=[hst_t[c], stf_t], w=[hin_t[c]])
                init = hin[:, c:c + 1]
            P.op("dve", lambda e, hout=hout, aa=aa, vv=vv, init=init: e.tensor_tensor_scan(
                out=hout, data0=aa[:, :], data1=vv[:, :], initial=init, op0=ALU.mult, op1=ALU.add),
                r=[tmp_t["a"][b], tmp_t["v"][b], hin_t[c]], w=[ht])
            P.cp("dve", hst[:, c:c + 1], hout[:, 511:512], r=[ht], w=[hst_t[c]])
            if st == 5:
                P.cp("dve", h_own[:, c, 0:2], hout[:, 510:512], r=[ht], w=[h_own_t[c]])

        proj_pass(wA1_d, 1024, list(range(8)), [dict(kind="fm", col0=0, ncols=1024, evac=evac)], nrot=4)
        assert not prep_chunks
        A.release(m_l)

        gt = [A.alloc("gt", [128, 512], F32) for _ in range(2)]
        gt_t = [Tk("gt0"), Tk("gt1")]
        gt2 = [A.alloc("gt2", [128, 512], F32) for _ in range(2)]
        gt2_t = [Tk("gt20"), Tk("gt21")]

        def evac_gate(st, c, ps, ps_t):
            b = c % 2
            if st == 5:
                lo, hi, o0 = 510, 512, 0
            else:
                lo, hi, o0 = 0, 512, 2 + (st - 6) * 512
            n = hi - lo
            gelu(gt[b][:, 0:n], ps[:, lo:hi], gt2[b][:, 0:n], [ps_t], gt_t[b], gt2_t[b])
            P.tt("dve", ylruT[:, c, o0:o0 + n], gt[b][:, 0:n], h_own[:, c, o0:o0 + n], ALU.mult,
                 r=[gt_t[b], h_own_t[c]], w=[ylru_t[c]])

        proj_pass(wB3_d, 1024, [5, 6, 7], [dict(kind="fm", col0=0, ncols=1024, evac=evac_gate)], scratch="B3")
        A.release(m0)

    def gelu(out, in_, scratch, in_t, out_t, scratch_t):
        if USE_GELU_TANH_LUT:
            P.act(out, in_, AF.Gelu_apprx_tanh, r=in_t, w=[out_t])
            return
        P.act(scratch, in_, AF.Square, r=in_t, w=[scratch_t])
        P.ts("dve", scratch, scratch, 0.044715, 1.0, ALU.mult, ALU.add, r=[scratch_t], w=[scratch_t])
        P.tt("dve", scratch, scratch, in_, ALU.mult, r=[scratch_t] + list(in_t), w=[scratch_t])
        P.act(scratch, scratch, AF.Sigmoid, r=[scratch_t], w=[scratch_t], scale=1.5957691216057308)
        P.tt("dve", out, scratch, in_, ALU.mult, r=[scratch_t] + list(in_t), w=[out_t])


    run_lru()

    kselT = A.alloc("kselT", [128, 2, S], BF16)
    ksel_t = Tk("kselT")
    vsel = A.alloc("vsel", [128, 32, 2, 129], BF16)
    vsel_t = Tk("vsel")
    kcmpT = A.alloc("kcmpT", [128, 2, 256], BF16)
    kcmp_t = Tk("kcmpT")
    Rc = A.alloc("Rc", [128, 2, 2, 193], BF16)
    Rc_t = Tk("Rc")
    validT = A.alloc("validT", [128, 32], F32)
    validT_t = Tk("validT")
    P.dma("sp", validT[:, :], validT_d[:, :], w=[validT_t])
    P.op("dve", lambda e: e.tensor_copy(out=vsel[:, :, 0, 128:129], in_=validT[:, :].unsqueeze(2)), r=[validT_t], w=[vsel_t])
    P.op("dve", lambda e: e.tensor_copy(out=vsel[:, :, 1, 128:129], in_=validT[:, :].unsqueeze(2)), r=[validT_t], w=[vsel_t])
    m_a2 = A.mark()
    kvcT = A.alloc("kvcT", [128, 4, S], BF16)
    kvc_t = Tk("kvcT")

    def evac_a2_fm(st, j, ps, ps_t):
        eng = "act" if j % 2 == 0 else "dve"
        if j < 4:
            P.cp(eng, kvcT[:, j, st * 512:(st + 1) * 512], ps[:, :], r=[ps_t], w=[kvc_t])
        else:
            P.cp(eng, kselT[:, j - 4, st * 512:(st + 1) * 512], ps[:, :], r=[ps_t], w=[ksel_t])

    def evac_a2_v(st, tt, ps, ps_t):
        ch = st * 4 + tt
        P.cp("act" if tt % 2 else "dve", vsel[:, ch, :, 0:128], ps[:, 0:256].rearrange("p (h d) -> p h d", h=2),
             r=[ps_t], w=[vsel_t])

    proj_pass(wA2_d, 1024, list(range(8)),
              [dict(kind="fm", col0=0, ncols=768, evac=evac_a2_fm),
               dict(kind="tm", col0=768, ncols=256, evac=evac_a2_v)], scratch="A2")

    def run_compress():
        m0 = A.mark()
        w1 = [A.alloc("w1", [128, 32, 128], BF16) for _ in range(2)]
        w1_t = [Tk("w1k"), Tk("w1v")]
        P.dma("pool", w1[0][:, :, :], w1k_d[:, :, :], w=[w1_t[0]])
        P.dma("pool", w1[1][:, :, :], w1v_d[:, :, :], w=[w1_t[1]])
        w2 = A.alloc("w2", [128, 2, 128], BF16)
        w2_t = Tk("w2")
        P.dma("pool", w2[:, 0, :], w2k_d[:, :], w=[w2_t])
        P.dma("pool", w2[:, 1, :], w2v_d[:, :], w=[w2_t])
        peT = A.alloc("peT", [128, 2, 32], BF16)
        peT_t = Tk("peT")
        P.dma("pool", peT[:, :, :], pe_d[:, :, :], w=[peT_t])
        cb = A.alloc("cb", [128, 4], F32)
        cb_t = Tk("cb")
        P.dma("sp", cb[:, 0:2], cmpb_d[:, :], w=[cb_t])
        vn = A.alloc("vn", [128, 2], F32)
        ovl = A.alloc("ovl", [128, 2, 64], F32)
        vn_t = Tk("vn")
        P.dma("sp", vn[:, :], validn_d[:, :], w=[vn_t])
        P.dma("sp", ovl[:, :, :], ovl_d[:, :, :], w=[vn_t])
        for ty in range(2):
            for l in range(32):
                P.mm(banks[2][:, ty:ty + 1], w1[ty][:, l, :], peT[:, ty, l:l + 1], l == 0, l == 31,
                     r=[w1_t[ty], peT_t], w=[bk[2]])
            P.tt("dve", cb[:, 2 + ty:3 + ty], banks[2][:, ty:ty + 1], cb[:, ty:ty + 1], ALU.add, r=[bk[2], cb_t], w=[cb_t])
        hid = [A.alloc("hid", [128, 256], F32) for _ in range(2)]
        hid_t = [Tk("hid0"), Tk("hid1")]
        hs = [A.alloc("hs", [128, 256], F32) for _ in range(2)]
        hs_t = [Tk("hs0"), Tk("hs1")]
        hp = [A.alloc("hp", [128, 256], F32) for _ in range(2)]
        hp_t = [Tk("hp0"), Tk("hp1")]
        hb = [A.alloc("hb", [128, 256], BF16) for _ in range(2)]
        hb_t = [Tk("hb0"), Tk("hb1")]
        for c2 in range(2):
            for hk in range(2):
                P.cp("dve", Rc[:, c2, hk, 0:1], vn[:, c2:c2 + 1], r=[vn_t], w=[Rc_t])
                P.ts("dve", Rc[:, c2, hk, 1:65], ovl[:, c2, :], vn[:, c2:c2 + 1], None, ALU.mult, r=[vn_t], w=[Rc_t])
        it = 0
        for hk in range(2):
            for ty in range(2):
                b = it % 2
                it += 1
                pb = 3 + b
                for l in range(32):
                    P.mm(banks[pb][:, 0:255], w1[ty][:, l, :], kvcT[:, ty * 2 + hk, l: l + 16 * 254 + 1: 16], l == 0, l == 31,
                         r=[w1_t[ty], kvc_t], w=[bk[pb]])
                P.ts("dve", hp[b][:, 0:255], banks[pb][:, 0:255], cb[:, 2 + ty:3 + ty], None, ALU.add, r=[bk[pb], cb_t], w=[hp_t[b]])
                gelu(hid[b][:, 0:255], hp[b][:, 0:255], hs[b][:, 0:255], [hp_t[b]], hid_t[b], hs_t[b])
                P.cp("dve", hb[b][:, 0:255], hid[b][:, 0:255], r=[hid_t[b]], w=[hb_t[b]])
                if ty == 0:
                    P.mm(banks[5][:, 0:255], w2[:, 0, :], hb[b][:, 0:255], True, True, r=[w2_t, hb_t[b]], w=[bk[5]])
                    P.cp("act", kcmpT[:, hk, 0:255], banks[5][:, 0:255], r=[bk[5]], w=[kcmp_t])
                else:
                    for c2 in range(2):
                        rows = 128 if c2 == 0 else 127
                        P.mm(banks[6 + c2][0:rows, 0:128], hb[b][:, c2 * 128: c2 * 128 + rows], w2[:, 1, :], True, True,
                             r=[w2_t, hb_t[b]], w=[bk[6 + c2]])
                        P.ts("dve", Rc[0:rows, c2, hk, 65:193], banks[6 + c2][0:rows, 0:128], vn[0:rows, c2:c2 + 1], None, ALU.mult,
                             r=[bk[6 + c2], vn_t], w=[Rc_t])
        P.barrier()
        A.release(m0)

    run_compress()
    A.release(m_a2)

    kwT = A.alloc("kwT", [128, 2, 2048], BF16)
    kw_t = Tk("kwT")
    vwin = A.alloc("vwin", [128, 16, 2, 129], BF16)
    vwin_t = Tk("vwin")
    gsig = A.alloc("gsig", [128, 9, 24], F32)
    gsig_t = Tk("gsig")
    qT = A.alloc("qT", [128, 8, NOWN], BF16)
    qT_t = Tk("qT")
    P.op("dve", lambda e: e.tensor_copy(out=vwin[:, :, 0, 128:129], in_=validT[:, 16:32].unsqueeze(2)), r=[validT_t], w=[vwin_t])
    P.op("dve", lambda e: e.tensor_copy(out=vwin[:, :, 1, 128:129], in_=validT[:, 16:32].unsqueeze(2)), r=[validT_t], w=[vwin_t])
    def evac_b1_k(st, j, ps, ps_t):
        P.cp("act" if j else "dve", kwT[:, j, (st - 4) * 512:(st - 3) * 512], ps[:, :], r=[ps_t], w=[kw_t])

    def evac_b1_v(st, tt, ps, ps_t):
        ch = (st - 4) * 4 + tt
        P.cp("dve", vwin[:, ch, :, 0:128], ps[:, 0:256].rearrange("p (h d) -> p h d", h=2), r=[ps_t], w=[vwin_t])
        if st >= 6:
            ti = 1 + (st - 6) * 4 + tt
            P.act(gsig[:, ti, :], ps[:, 256:280], AF.Sigmoid, r=[ps_t], w=[gsig_t])

    halo_ctx = {}

    proj_pass(wB1_d, 536, [4, 5, 6, 7],
              [dict(kind="fm", col0=0, ncols=256, evac=evac_b1_k),
               dict(kind="tm", col0=256, ncols=280, evac=evac_b1_v)], scratch="B1")

    def setup_b2(ctx):
        halo_ctx.update(ctx)

    def evac_b2(st, j, ps, ps_t):
        if st == 5:
            lo, hi, o0 = 510, 512, 0
        else:
            lo, hi, o0 = 0, 512, 2 + (st - 6) * 512
        P.act(qT[:, j, o0:o0 + hi - lo], ps[:, lo:hi], AF.Copy, r=[ps_t], w=[qT_t], scale=float(128 ** -0.5))

    proj_pass(wB2_d, 1024, [5, 6, 7], [dict(kind="fm", col0=0, ncols=1024, evac=evac_b2)], scratch="B2")

    def halo_gates():
        m0 = A.mark()
        Wg = A.alloc("Wg", [128, KC, 24], BF16)
        Wg_t = Tk("Wg")
        sg = A.alloc("sg", [128, KC, 24], F32)
        sg_t = Tk("sg")
        P.dma("sp", sg[:, :, :], wB1_d[:, :, 512:536], w=[sg_t])
        for k in range(KC):
            P.ts("dve", Wg[:, k, :], sg[:, k, :], gmixT[:, k:k + 1], None, ALU.mult, r=[sg_t, gmixT_t], w=[Wg_t])
        xh = A.alloc("xh", [2, D], F32)
        xh_t = Tk("xh")
        P.dma("sp", xh[:, :], x_loc[3070:3072, :], w=[xh_t])
        xq = A.alloc("xq", [2, D], BF16)
        xq_t = Tk("xq")
        sh = A.alloc("sh", [2, 4], F32)
        sh_t = Tk("sh")
        P.act(xq[:, :], xh[:, :], AF.Square, r=[xh_t], w=[xq_t, sh_t], accum_out=sh[:, 0:1])
        P.act(sh[:, 1:2], sh[:, 0:1], AF.Ln, r=[sh_t], w=[sh_t], scale=1.0 / D, bias=epsb[0:2, 0:1])
        P.act(sh[:, 2:3], sh[:, 1:2], AF.Exp, r=[sh_t], w=[sh_t], scale=-0.5)
        P.act(xq[:, :], xh[:, :], AF.Copy, r=[xh_t, sh_t], w=[xq_t], scale=sh[:, 2:3])
        xqT = A.alloc("xqT", [128, KC, 2], BF16)
        xqT_t = Tk("xqT")
        for j in range(KC):
            P.tr(bf(banks[0][:, :])[:, j * 2:(j + 1) * 2], xq[:, j * 128:(j + 1) * 128], ident[0:2, 0:2], r=[xq_t, ident_t], w=[bk[0]])
        P.cp("dve", xqT[:, :, :], bf(banks[0][:, :])[:, 0:32].rearrange("p (j t) -> p j t", j=KC), r=[bk[0]], w=[xqT_t])
        for k in range(KC):
            P.mm(banks[2][0:2, 0:24], xqT[:, k, :], Wg[:, k, :], k == 0, k == KC - 1, r=[xqT_t, Wg_t], w=[bk[2]])
        P.act(gsig[0:2, 0, :], banks[2][0:2, 0:24], AF.Sigmoid, r=[bk[2]], w=[gsig_t])
        P.barrier()
        A.release(m0)

    halo_gates()

    def run_attention():
        m0 = A.mark()
        ebig = A.alloc("ebig", [64, S], BF16)
        cw = A.alloc("cw", [128, 3, 128], F32)
        cwh = A.alloc("cwh", [2, 3, 128], F32)
        f0 = A.alloc("f0", [128, 64], F32)
        cst_t = Tk("attn_consts")
        P.dma("sp", ebig[:, :], ebig_d[:, :], w=[cst_t])
        P.dma("sp", cw[:, :, :], cwide_d[:, :, :], w=[cst_t])
        P.dma("sp", cwh[:, :, :], cwide_h_d[:, :, :], w=[cst_t])
        P.dma("sp", f0[:, :], f0_d[:, :], w=[cst_t])
        NE = 4
        Et = [A.alloc("E", [128, 4, 128], BF16) for _ in range(NE)]
        Et_t = [Tk("E%d" % i) for i in range(NE)]
        Pt = [A.alloc("Pm", [128, 4, 128], BF16) for _ in range(NE)]
        Pt_t = [Tk("Pm%d" % i) for i in range(NE)]
        Ec = [[A.alloc("Ec", [128, 4, 128], BF16) for _ in range(2)] for _ in range(2)]
        Ec_t = [[Tk("Ec%d%d" % (a, b)) for b in range(2)] for a in range(2)]
        sm = [A.alloc("sm", [128, 64], F32) for _ in range(2)]
        sm_t = [Tk("sm0"), Tk("sm1")]
        imp = [A.alloc("imp", [128, 64], F32) for _ in range(2)]
        sc2 = [A.alloc("sc2", [128, 64], F32) for _ in range(2)]
        top = [A.alloc("top", [128, 16], F32) for _ in range(2)]
        selb = [A.alloc("selb", [128, 64], BF16) for _ in range(2)]
        selT = [A.alloc("selT", [64, 128], BF16) for _ in range(2)]
        tk_t = [Tk("topk0"), Tk("topk1")]
        selT_t = [Tk("selT0"), Tk("selT1")]
        acc = [A.alloc("acc", [128, 4, 128], F32) for _ in range(2)]
        acc_t = [Tk("acc0"), Tk("acc1")]
        ob = A.alloc("ob", [128, 4, 128], BF16)
        ob_t = Tk("ob")
        ectr = [0]
        PVB = [3, 4, 5, 6]

        def qblock(Dq, q0, nq, qc, gti):
            cwm = cwh if nq == 2 else cw
            lo = 64 - 2 * Dq

            def cmp_gen(hk):
                qv = qT[:, 4 * hk:4 * hk + 4, qc:qc + nq]
                cb0 = 3 + 2 * hk
                for c2 in range(2):
                    rows = 128 if c2 == 0 else 127
                    sb = c2
                    P.mm(banks[sb][0:rows, 0:4 * nq].rearrange("p (g q) -> p g q", g=4), kcmpT[:, hk, c2 * 128:c2 * 128 + rows], qv,
                         True, True, r=[kcmp_t, qT_t], w=[bk[sb]])
                    P.act(Ec[hk][c2][0:rows, :, 0:nq], banks[sb][0:rows, 0:4 * nq].rearrange("p (g q) -> p g q", g=4), AF.Exp,
                          r=[bk[sb]], w=[Ec_t[hk][c2]])
                    basev = 128 * Dq + q0 - 31 - 2048 * c2
                    P.op("pool", lambda e, c2=c2, rows=rows, basev=basev: e.affine_select(
                        out=Ec[hk][c2][0:rows, :, 0:nq], in_=Ec[hk][c2][0:rows, :, 0:nq], pattern=[[0, 4], [1, nq]],
                        compare_op=ALU.is_ge, fill=0.0, base=basev, channel_multiplier=-16), r=[Ec_t[hk][c2]], w=[Ec_t[hk][c2]])
                yield
                for g in range(4):
                    pb = cb0 + g // 2
                    co = (g % 2) * 193
                    for c2 in range(2):
                        rows = 128 if c2 == 0 else 127
                        P.mm(banks[pb][0:nq, co:co + 193], Ec[hk][c2][0:rows, g, 0:nq], Rc[0:rows, c2, hk, :], c2 == 0, c2 == 1,
                             r=[Ec_t[hk][c2], Rc_t], w=[bk[pb]])
                yield
                for g in range(4):
                    pb = cb0 + g // 2
                    co = (g % 2) * 193
                    P.ts("dve", sm[hk][0:nq, g:g + 1], banks[pb][0:nq, co:co + 1], 1e-30, None, ALU.max, r=[bk[pb]], w=[sm_t[hk]])
                P.op("dve", lambda e: e.reciprocal(out=sm[hk][0:nq, 4:8], in_=sm[hk][0:nq, 0:4]), r=[sm_t[hk]], w=[sm_t[hk]])
                P.tt("dve", sm[hk][0:nq, 8:12], sm[hk][0:nq, 4:8], gsig[0:nq, gti, 12 * hk:12 * hk + 10:3], ALU.mult, r=[sm_t[hk], gsig_t], w=[sm_t[hk]])
                yield
                for g in range(4):
                    pb = cb0 + g // 2
                    co = (g % 2) * 193
                    if g == 0:
                        P.ts("dve", imp[hk][0:nq, :], banks[pb][0:nq, co + 1:co + 65], sm[hk][0:nq, 4:5], None, ALU.mult, r=[bk[pb], sm_t[hk]], w=[tk_t[hk]])
                    else:
                        P.stt(imp[hk][0:nq, :], banks[pb][0:nq, co + 1:co + 65], sm[hk][0:nq, 4 + g:5 + g], imp[hk][0:nq, :], ALU.mult, ALU.add,
                              r=[bk[pb], sm_t[hk], tk_t[hk]], w=[tk_t[hk]])
                    P.ts("dve", acc[hk][0:nq, g, :], banks[pb][0:nq, co + 65:co + 193], sm[hk][0:nq, 8 + g:9 + g], None, ALU.mult,
                         r=[bk[pb], sm_t[hk]], w=[acc_t[hk]])
                    if g % 2 == 1:
                        yield
                P.tt("dve", imp[hk][0:nq, :], imp[hk][0:nq, :], cwm[0:nq, 0, lo:lo + 64], ALU.mult, r=[tk_t[hk], cst_t], w=[tk_t[hk]])
                P.tt("dve", imp[hk][0:nq, :], imp[hk][0:nq, :], cwm[0:nq, 1, lo:lo + 64], ALU.add, r=[tk_t[hk], cst_t], w=[tk_t[hk]])
                yield
                P.tt("dve", imp[hk][0:nq, :], imp[hk][0:nq, :], cwm[0:nq, 2, lo:lo + 64], ALU.max, r=[tk_t[hk], cst_t], w=[tk_t[hk]])
                P.tt("dve", imp[hk][0:nq, :], imp[hk][0:nq, :], f0[0:nq, :], ALU.max, r=[tk_t[hk], cst_t], w=[tk_t[hk]])
                yield
                P.op("dve", lambda e: e.max(out=top[hk][0:nq, 0:8], in_=imp[hk][0:nq, :]), r=[tk_t[hk]], w=[tk_t[hk]])
                yield
                P.op("dve", lambda e: e.match_replace(out=sc2[hk][0:nq, :], in_to_replace=top[hk][0:nq, 0:8], in_values=imp[hk][0:nq, :],
                                                      imm_value=-1e9), r=[tk_t[hk]], w=[tk_t[hk]])
                yield
                P.op("dve", lambda e: e.max(out=top[hk][0:nq, 8:16], in_=sc2[hk][0:nq, :]), r=[tk_t[hk]], w=[tk_t[hk]])
                yield
                P.ts("dve", sc2[hk][0:nq, :], imp[hk][0:nq, :], top[hk][0:nq, 15:16], None, ALU.is_ge, r=[tk_t[hk]], w=[tk_t[hk]])
                yield
                P.tt("dve", selb[hk][0:nq, :], sc2[hk][0:nq, :], cwm[0:nq, 0, lo:lo + 64], ALU.mult, r=[tk_t[hk], cst_t], w=[tk_t[hk]])
                yield
                to = 512 + hk * 128
                P.tr(bf(banks[2][:, :])[0:64, to:to + nq], selb[hk][0:nq, :], ident[0:nq, 0:nq], r=[tk_t[hk], ident_t], w=[bk[2]])
                yield
                P.cp("dve", selT[hk][:, 0:nq], bf(banks[2][:, :])[0:64, to:to + nq], r=[bk[2]], w=[selT_t[hk]])

            gens = [cmp_gen(0), cmp_gen(1)]
            while gens:
                for gen in list(gens):
                    try:
                        next(gen)
                    except StopIteration:
                        gens.remove(gen)

            for hk in range(2):
                qv = qT[:, 4 * hk:4 * hk + 4, qc:qc + nq]
                def sel_qk(kc):
                    sb = ectr[0] % 2
                    ei = ectr[0] % NE
                    ectr[0] += 1
                    P.mm(banks[sb][:, 0:4 * nq].rearrange("p (g q) -> p g q", g=4), kselT[:, hk, kc * 128:(kc + 1) * 128], qv,
                         True, True, r=[ksel_t, qT_t], w=[bk[sb]])
                    mo = (kc % 2) * 128
                    P.mm(banks[2][:, mo:mo + nq], ebig[:, kc * 128:(kc + 1) * 128], selT[hk][:, 0:nq], True, True,
                         r=[cst_t, selT_t[hk]], w=[bk[2]])
                    P.act(Et[ei][:, :, 0:nq], banks[sb][:, 0:4 * nq].rearrange("p (g q) -> p g q", g=4), AF.Exp,
                          r=[bk[sb]], w=[Et_t[ei]])
                    P.tt("dve", Pt[ei][:, :, 0:nq], Et[ei][:, :, 0:nq], banks[2][:, mo:mo + nq].unsqueeze(1).broadcast_to([128, 4, nq]), ALU.mult,
                         r=[Et_t[ei], bk[2]], w=[Pt_t[ei]])
                    if kc == Dq:
                        P.op("pool", lambda e, ei=ei: e.affine_select(
                            out=Pt[ei][:, :, 0:nq], in_=Pt[ei][:, :, 0:nq], pattern=[[0, 4], [1, nq]],
                            compare_op=ALU.is_ge, fill=0.0, base=q0, channel_multiplier=-1), r=[Pt_t[ei]], w=[Pt_t[ei]])
                    return ei

                def sel_pv(kc, ei):
                    for g in range(4):
                        P.mm(banks[PVB[g]][0:nq, 0:129], Pt[ei][:, g, 0:nq], vsel[:, kc, hk, :], kc == 0, kc == Dq,
                             r=[Pt_t[ei], vsel_t], w=[bk[PVB[g]]])
                    if kc == Dq:
                        for g in range(4):
                            P.ts("dve", sm[hk][0:nq, 16 + g:17 + g], banks[PVB[g]][0:nq, 128:129], 1e-30, None, ALU.max, r=[bk[PVB[g]]], w=[sm_t[hk]])
                        P.op("dve", lambda e, hk=hk: e.reciprocal(out=sm[hk][0:nq, 20:24], in_=sm[hk][0:nq, 16:20]), r=[sm_t[hk]], w=[sm_t[hk]])
                        P.tt("dve", sm[hk][0:nq, 24:28], sm[hk][0:nq, 20:24], gsig[0:nq, gti, 12 * hk + 1:12 * hk + 11:3], ALU.mult, r=[sm_t[hk], gsig_t], w=[sm_t[hk]])
                        for g in range(4):
                            P.stt(acc[hk][0:nq, g, :], banks[PVB[g]][0:nq, 0:128], sm[hk][0:nq, 24 + g:25 + g], acc[hk][0:nq, g, :], ALU.mult, ALU.add,
                                  r=[bk[PVB[g]], sm_t[hk], acc_t[hk]], w=[acc_t[hk]])

                def win_qk(kc):
                    sb = ectr[0] % 2
                    ei = ectr[0] % NE
                    ectr[0] += 1
                    wc = kc - 16
                    P.mm(banks[sb][:, 0:4 * nq].rearrange("p (g q) -> p g q", g=4), kwT[:, hk, wc * 128:(wc + 1) * 128], qv,
                         True, True, r=[kw_t, qT_t], w=[bk[sb]])
                    P.act(Pt[ei][:, :, 0:nq], banks[sb][:, 0:4 * nq].rearrange("p (g q) -> p g q", g=4), AF.Exp,
                          r=[bk[sb]], w=[Pt_t[ei]])
                    if kc == Dq:
                        P.op("pool", lambda e, ei=ei: e.affine_select(
                            out=Pt[ei][:, :, 0:nq], in_=Pt[ei][:, :, 0:nq], pattern=[[0, 4], [1, nq]],
                            compare_op=ALU.is_ge, fill=0.0, base=q0, channel_multiplier=-1), r=[Pt_t[ei]], w=[Pt_t[ei]])
                    if kc == Dq - 4:
                        P.op("pool", lambda e, ei=ei: e.affine_select(
                            out=Pt[ei][:, :, 0:nq], in_=Pt[ei][:, :, 0:nq], pattern=[[0, 4], [-1, nq]],
                            compare_op=ALU.is_ge, fill=0.0, base=-q0 - 1, channel_multiplier=1), r=[Pt_t[ei]], w=[Pt_t[ei]])
                    return ei

                def win_pv(kc, ei):
                    wc = kc - 16
                    for g in range(4):
                        P.mm(banks[PVB[g]][0:nq, 0:129], Pt[ei][:, g, 0:nq], vwin[:, wc, hk, :], kc == Dq - 4, kc == Dq,
                             r=[Pt_t[ei], vwin_t], w=[bk[PVB[g]]])

                steps = [(sel_qk, sel_pv, kc) for kc in range(Dq + 1)] + [(win_qk, win_pv, kc) for kc in range(Dq - 4, Dq + 1)]
                ei_next = steps[0][0](steps[0][2])
                for si_, (fq, fp, kc) in enumerate(steps):
                    ei_cur = ei_next
                    if si_ + 1 < len(steps):
                        ei_next = steps[si_ + 1][0](steps[si_ + 1][2])
                    fp(kc, ei_cur)
                for g in range(4):
                    P.ts("dve", sm[hk][0:nq, 32 + g:33 + g], banks[PVB[g]][0:nq, 128:129], 1e-30, None, ALU.max, r=[bk[PVB[g]]], w=[sm_t[hk]])
                P.op("dve", lambda e, hk=hk: e.reciprocal(out=sm[hk][0:nq, 36:40], in_=sm[hk][0:nq, 32:36]), r=[sm_t[hk]], w=[sm_t[hk]])
                P.tt("dve", sm[hk][0:nq, 40:44], sm[hk][0:nq, 36:40], gsig[0:nq, gti, 12 * hk + 2:12 * hk + 12:3], ALU.mult, r=[sm_t[hk], gsig_t], w=[sm_t[hk]])
                for g in range(4):
                    P.stt(ob[0:nq, g, :], banks[PVB[g]][0:nq, 0:128], sm[hk][0:nq, 40 + g:41 + g], acc[hk][0:nq, g, :], ALU.mult, ALU.add,
                          r=[bk[PVB[g]], sm_t[hk], acc_t[hk]], w=[ob_t])
                for g in range(4):
                    P.tr(bf(banks[7][:, :])[:, g * 128:g * 128 + nq], ob[0:nq, g, :], ident[0:nq, 0:nq], r=[ob_t, ident_t], w=[bk[7]])
                P.cp("act", yattT[:, 4 * hk:4 * hk + 4, qc:qc + nq],
                     bf(banks[7][:, :])[:, 0:512].rearrange("p (g q) -> p g q", g=4)[:, :, 0:nq], r=[bk[7]], w=[yatt_t])

        qblock(23, 126, 2, 0, 0)
        for i in range(8):
            qblock(24 + i, 0, 128, 2 + 128 * i, 1 + i)
        P.barrier()
        A.release(m0)

    wo_top = (A.limit - 2 * 16384) // 64 * 64
    wo = [nc.alloc_sbuf_tensor_at("wo%d" % i, [128, 16, 512], BF16, offset=wo_top + i * 16384) for i in range(2)]
    wo_t = [Tk("wo0"), Tk("wo1")]

    def load_wo(obk):
        wb = obk % 2
        for c in range(8):
            P.dma("pool", wo[wb][:, c, :], wol_d[:, c, obk * 512:(obk + 1) * 512], w=[wo_t[wb]])
            P.dma("pool", wo[wb][:, 8 + c, :], woa_d[:, c, obk * 512:(obk + 1) * 512], w=[wo_t[wb]])

    load_wo(0)
    load_wo(1)
    run_attention()
    P.barrier()
    A.release(m_mixer)

    def run_ffn():
        gout = A.alloc("gout", [128, 2, 8], F32)
        gout_t = Tk("gout")
        P.dma("sp", gout[:, :, :], gout_d[:, :, :], w=[gout_t])
        gffn = A.alloc("gffn", [128, D], F32)
        gffn_t = Tk("gffn")
        P.dma("sp", gffn[:, :], gffn_d[:, :], w=[gffn_t])
        fcw = A.alloc("fcw", [128, 48, 3], F32)
        fcb = A.alloc("fcb", [128, 48], F32)
        fc_t = Tk("fc")
        P.dma("sp", fcw[:, :, :], fcw_d[:, :, :], w=[fc_t])
        P.dma("sp", fcb[:, :], fcb_d[:, :], w=[fc_t])
        h = A.alloc("h", [128, 8, D], F32)
        h_t = [Tk("h%d" % i) for i in range(8)]
        hh = A.alloc("hh", [2, D], F32)
        hh_t = Tk("hh")
        xnT = A.alloc("xnTf", [128, KC, NOWN], BF16)
        xnT_t = Tk("xnTf")
        m1 = A.mark()
        rs = A.alloc("rs", [128, 9, 8], F32)
        rs_t = Tk("rs")
        xnb = A.alloc("xnb", [128, D], BF16)
        xnb_t = Tk("xnb")
        ysq = [A.alloc("ysq", [128, 16, 128], BF16) for _ in range(2)]
        ysq_t = [Tk("ysq0"), Tk("ysq1")]
        assert A.off <= wo_top
        tiles = [(0, 2, hh[:, :], hh_t, 3070)] + [(2 + 128 * i, 128, h[:, i, :], h_t[i], 3072 + 128 * i) for i in range(8)]
        for ti, (c0, nt, hap, hapt, u0) in enumerate(tiles):
            P.dma("sp", hap, x_loc[u0:u0 + nt, :], w=[hapt])
            yb = ti % 2
            P.tt("pool", ysq[yb][:, 0:8, 0:nt], ylruT[:, :, c0:c0 + nt], ylruT[:, :, c0:c0 + nt], ALU.mult, r=ylru_t, w=[ysq_t[yb]])
            P.tt("dve", ysq[yb][:, 8:16, 0:nt], yattT[:, :, c0:c0 + nt], yattT[:, :, c0:c0 + nt], ALU.mult, r=[yatt_t], w=[ysq_t[yb]])
            for br in range(2):
                for c in range(8):
                    P.mm(banks[0][0:nt, br:br + 1], ysq[yb][:, br * 8 + c, 0:nt], ones_bf[:, 0:1], c == 0, c == 7,
                         r=[ysq_t[yb], ones_t], w=[bk[0]])
            P.act(rs[0:nt, ti, 0:2], banks[0][0:nt, 0:2], AF.Ln, r=[bk[0]], w=[rs_t], scale=1.0 / 1024, bias=epsb[0:nt, 0:1])
            P.act(rs[0:nt, ti, 2:4], rs[0:nt, ti, 0:2], AF.Exp, r=[rs_t], w=[rs_t], scale=-0.5)
        for c in range(8):
            P.ts("pool", ylruT[:, c, :], ylruT[:, c, :], gout[:, 0, c:c + 1], None, ALU.mult, r=[gout_t] + ysq_t, w=[ylru_t[c]])
            P.ts("dve", yattT[:, c, :], yattT[:, c, :], gout[:, 1, c:c + 1], None, ALU.mult, r=[gout_t] + ysq_t, w=[yatt_t])
        for obk in range(4):
            wb = obk % 2
            if obk >= 2:
                load_wo(obk)
            for ti, (c0, nt, hap, hapt, u0) in enumerate(tiles):
                for br in range(2):
                    pb = 2 + (ti % 2) * 2 + br
                    ysrc = ylruT if br == 0 else yattT
                    for c in range(8):
                        P.mm(banks[pb][0:nt, :], ysrc[:, c, c0:c0 + nt], wo[wb][:, br * 8 + c, :], c == 0, c == 7,
                             r=[ylru_t[c] if br == 0 else yatt_t, wo_t[wb]], w=[bk[pb]])
                    P.stt(hap[:, obk * 512:(obk + 1) * 512], banks[pb][0:nt, :], rs[0:nt, ti, 2 + br:3 + br],
                          hap[:, obk * 512:(obk + 1) * 512], ALU.mult, ALU.add, r=[bk[pb], rs_t, hapt], w=[hapt])
        for ti, (c0, nt, hap, hapt, u0) in enumerate(tiles):
            P.act(xnb[0:nt, :], hap, AF.Square, r=[hapt], w=[xnb_t, rs_t], accum_out=rs[0:nt, ti, 4:5])
            P.act(rs[0:nt, ti, 5:6], rs[0:nt, ti, 4:5], AF.Ln, r=[rs_t], w=[rs_t], scale=1.0 / D, bias=epsb[0:nt, 0:1])
            P.act(rs[0:nt, ti, 6:7], rs[0:nt, ti, 5:6], AF.Exp, r=[rs_t], w=[rs_t], scale=-0.5)
            P.stt(xnb[0:nt, :], hap, rs[0:nt, ti, 6:7], gffn[0:nt, :], ALU.mult, ALU.mult, r=[hapt, rs_t, gffn_t], w=[xnb_t])
            for half in range(2):
                for j in range(8):
                    jj = half * 8 + j
                    P.tr(bf(banks[6 + half][:, :])[:, j * 128:j * 128 + nt],
                         xnb[0:nt, jj * 128:(jj + 1) * 128], ident[0:nt, 0:nt], r=[xnb_t, ident_t], w=[bk[6 + half]])
                P.cp("dve" if half == 0 else "act", xnT[:, half * 8:(half + 1) * 8, c0:c0 + nt],
                     bf(banks[6 + half][:, :]).rearrange("p (j t) -> p j t", j=8)[:, :, 0:nt], r=[bk[6 + half]], w=[xnT_t])
        P.barrier()
        A.release(m1)
        wup = [nc.alloc_sbuf_tensor_at("wup%d" % i, [128, KC, 512], BF16, offset=y_base + i * 16384) for i in range(2)]
        assert y_base + 2 * 16384 <= y_end
        wup_t = [Tk("wup0"), Tk("wup1")]
        m2 = A.mark()
        wdn = [A.alloc("wdn", [128, 2, D], BF16) for _ in range(3)]
        wdn_t = [Tk("wdn0"), Tk("wdn1"), Tk("wdn2")]
        gT = [A.alloc("gT", [128, 2, 1024], BF16) for _ in range(2)]
        gT_t = [Tk("gT0"), Tk("gT1")]
        usb = [A.alloc("usb", [128, NOWN], F32) for _ in range(2)]
        usb_t = [Tk("usb0"), Tk("usb1")]
        vsb = [A.alloc("vsb", [128, NOWN], F32) for _ in range(2)]
        vsb_t = [Tk("vsb0"), Tk("vsb1")]
        uc = A.alloc("uc", [128, 1024], F32)
        uc_t = Tk("uc")
        gg = A.alloc("gg", [128, 1024], F32)
        gg_t = Tk("gg")
        CT = 342
        rotu = [0]
        def prefetch(G):
            P.dma("pool", wup[G % 2][:, :, :], wup_d[G, :, :, :], w=[wup_t[G % 2]])
            P.dma("pool", wdn[G % 3][:, :, :], wdn_d[G, :, :, :], w=[wdn_t[G % 3]])

        def up(G):
            gb = G % 2
            for fc in range(2):
                fa = 2 * G + fc
                ub = fa % 2
                for ct in range(3):
                    pu = rotu[0] % 2
                    rotu[0] += 1
                    bu, bv = pu * 2, pu * 2 + 1
                    for k in range(KC):
                        P.mm(banks[bu][:, 0:CT], wup[gb][:, k, fc * 128:(fc + 1) * 128], xnT[:, k, ct * CT:(ct + 1) * CT], k == 0, k == KC - 1,
                             r=[wup_t[gb], xnT_t], w=[bk[bu]])
                    for k in range(KC):
                        P.mm(banks[bv][:, 0:CT], wup[gb][:, k, 256 + fc * 128:256 + (fc + 1) * 128], xnT[:, k, ct * CT:(ct + 1) * CT], k == 0, k == KC - 1,
                             r=[wup_t[gb], xnT_t], w=[bk[bv]])
                    P.cp("act", usb[ub][:, ct * CT:(ct + 1) * CT], banks[bu][:, 0:CT], r=[bk[bu]], w=[usb_t[ub]])
                    P.cp("act", vsb[ub][:, ct * CT:(ct + 1) * CT], banks[bv][:, 0:CT], r=[bk[bv]], w=[vsb_t[ub]])
                P.ts("dve", uc[:, :], usb[ub][:, 2:1026], fcw[:, fa, 2:3], fcb[:, fa:fa + 1], ALU.mult, ALU.add, r=[usb_t[ub], fc_t], w=[uc_t])
                P.stt(uc[:, :], usb[ub][:, 1:1025], fcw[:, fa, 1:2], uc[:, :], ALU.mult, ALU.add, r=[usb_t[ub], fc_t, uc_t], w=[uc_t])
                P.stt(uc[:, :], usb[ub][:, 0:1024], fcw[:, fa, 0:1], uc[:, :], ALU.mult, ALU.add, r=[usb_t[ub], fc_t, uc_t], w=[uc_t])
                gelu(gg[:, :], uc[:, :], gg[:, :], [uc_t], gg_t, gg_t)
                P.tt("pool", gT[gb][:, fc, :], gg[:, :], vsb[ub][:, 2:1026], ALU.mult, r=[gg_t, vsb_t[ub]], w=[gT_t[gb]])

        def down(G):
            gb = G % 2
            wd = G % 3
            for tt in range(8):
                for obk in range(4):
                    pb = 4 + (tt * 4 + obk) % 4
                    for fc in range(2):
                        P.mm(banks[pb][:, :], gT[gb][:, fc, tt * 128:(tt + 1) * 128], wdn[wd][:, fc, obk * 512:(obk + 1) * 512], fc == 0, fc == 1,
                             r=[gT_t[gb], wdn_t[wd]], w=[bk[pb]])
                    P.tt("dve", h[:, tt, obk * 512:(obk + 1) * 512], banks[pb][:, :], h[:, tt, obk * 512:(obk + 1) * 512], ALU.add,
                         r=[bk[pb], h_t[tt]], w=[h_t[tt]])

        prefetch(0)
        for G in range(24):
            if G + 1 < 24:
                prefetch(G + 1)
            up(G)
            if G > 0:
                down(G - 1)
        down(23)
        P.barrier()
        A.release(m2)
        gfin = A.alloc("gfin", [128, D], F32)
        gfin_t = Tk("gfin")
        P.dma("sp", gfin[:, :], gfin_d[:, :], w=[gfin_t])
        fs = A.alloc("fs", [128, 8, 4], F32)
        fs_t = Tk("fs")
        xs2 = A.alloc("xs2", [128, D], BF16)
        xs2_t = Tk("xs2")
        for tt in range(8):
            P.act(xs2[:, :], h[:, tt, :], AF.Square, r=[h_t[tt]], w=[xs2_t, fs_t], accum_out=fs[:, tt, 0:1])
            P.act(fs[:, tt, 1:2], fs[:, tt, 0:1], AF.Ln, r=[fs_t], w=[fs_t], scale=1.0 / D, bias=epsb[:, 0:1])
            P.act(fs[:, tt, 2:3], fs[:, tt, 1:2], AF.Exp, r=[fs_t], w=[fs_t], scale=-0.5)
            P.stt(h[:, tt, :], h[:, tt, :], fs[:, tt, 2:3], gfin[:, :], ALU.mult, ALU.mult, r=[h_t[tt], fs_t, gfin_t], w=[h_t[tt]])
            P.dma("sp", y_out[tt * 128:(tt + 1) * 128, :], h[:, tt, :], r=[h_t[tt]], is_output=True)

    run_ffn()
    P.finish()
    return nc


def _tile_k(w):
    n = w.shape[1]
    return np.ascontiguousarray(w.reshape(KC, 128, n).transpose(1, 0, 2))


def _host_consts():
    c = {}
    c["ident"] = np.eye(128, dtype=np.float32).astype(ml_dtypes.bfloat16)
    eb = np.zeros((64, S), np.float32)
    for s in range(64):
        eb[s, s * 64:(s + 1) * 64] = 1.0
    c["ebig"] = eb.astype(ml_dtypes.bfloat16)
    n = np.arange(256)
    s = np.arange(64)
    ov = np.clip(np.minimum(n[:, None] * 16 + 32, s[None, :] * 64 + 64) - np.maximum(n[:, None] * 16, s[None, :] * 64), 0, None)
    ov = (ov / 32.0).astype(np.float32)
    ov[255] = 0.0
    c["ovl"] = np.ascontiguousarray(ov.reshape(2, 128, 64).transpose(1, 0, 2))

    def wide(hi):
        rel = np.arange(128)[None, :] - 64
        causal = (rel <= hi[:, None]).astype(np.float32)
        forced = ((rel <= hi[:, None]) & (rel > hi[:, None] - 2)).astype(np.float32)
        return np.ascontiguousarray(np.stack([causal, causal - 1.0, forced * 1e4], axis=1).astype(np.float32))

    c["cwide"] = wide((np.arange(128) >= 64).astype(np.int64))
    c["cwide_h"] = wide(np.array([1, 1], np.int64))
    return c


_NC_CACHE = {}


def kernel(**inp):
    f32 = np.float32
    g = {k: np.asarray(v, dtype=f32) for k, v in inp.items()}
    x = g["x"]
    w_in = g["w_in"][0]
    rep = {}
    rep["wA1"] = _tile_k(w_in[:, 0:1024])
    rep["wB3"] = _tile_k(w_in[:, 1024:2048])
    rep["wB2"] = _tile_k(w_in[:, 2048:3072])
    rep["wA2"] = _tile_k(np.concatenate([w_in[:, 3072:3584], w_in[:, 3584:3840], w_in[:, 3840:4096]], axis=1))
    rep["wB1"] = _tile_k(np.concatenate([w_in[:, 4096:4352], w_in[:, 4352:4608], w_in[:, 4608:4632]], axis=1))
    rep["gmixT"] = np.ascontiguousarray(g["g_mix"][0].reshape(KC, 128).T)
    rep["lcw"] = np.ascontiguousarray(g["lru_conv_w"][0].reshape(4, 8, 128).transpose(2, 1, 0))
    rep["lvec"] = np.ascontiguousarray(np.stack([g["lru_conv_b"][0], g["lru_ba"][0], g["lru_bx"][0], g["lru_lambda"][0]], 0)
                                       .reshape(4, 8, 128).transpose(2, 0, 1))
    rep["wa"] = np.ascontiguousarray(g["lru_wa"][0].transpose(1, 0, 2))
    rep["wx"] = np.ascontiguousarray(g["lru_wx"][0].transpose(1, 0, 2))
    rep["w1k"] = np.ascontiguousarray(g["cmp_w1_k"][0].reshape(32, 128, 128).transpose(1, 0, 2))
    rep["w1v"] = np.ascontiguousarray(g["cmp_w1_v"][0].reshape(32, 128, 128).transpose(1, 0, 2))
    rep["peT"] = np.ascontiguousarray(np.stack([g["cmp_pe_k"][0].T, g["cmp_pe_v"][0].T], axis=1))
    rep["cmpb"] = np.ascontiguousarray(np.stack([g["cmp_b1_k"][0], g["cmp_b1_v"][0]], axis=1))
    rep["w2k"] = np.ascontiguousarray(g["cmp_w2_k"][0])
    rep["w2v"] = np.ascontiguousarray(g["cmp_w2_v"][0])
    w_out = g["w_out"][0]
    rep["wol"] = np.ascontiguousarray(w_out[0:1024].reshape(8, 128, D).transpose(1, 0, 2))
    rep["woa"] = np.ascontiguousarray(w_out[1024:2048].reshape(8, 128, D).transpose(1, 0, 2))
    rep["gout"] = np.ascontiguousarray(np.stack([g["g_lru_out"][0].reshape(8, 128).T, g["g_attn_out"][0].reshape(8, 128).T], axis=1))
    rep["gffn_bc"] = np.ascontiguousarray(np.broadcast_to(g["g_ffn"][0][None, :], (128, D)))
    rep["gfin_bc"] = np.ascontiguousarray(np.broadcast_to(g["g_final"][None, :], (128, D)))
    w_up = g["w_up"][0]
    wu = w_up[:, 0:6144].reshape(KC, 128, 24, 256)
    wv = w_up[:, 6144:12288].reshape(KC, 128, 24, 256)
    rep["wup"] = np.ascontiguousarray(np.concatenate([wu, wv], axis=3).transpose(2, 1, 0, 3))
    rep["wdn"] = np.ascontiguousarray(g["w_down"][0].reshape(24, 2, 128, D).transpose(0, 2, 1, 3))
    rep["fcw"] = np.ascontiguousarray(g["ffn_conv_w"][0].reshape(3, 48, 128).transpose(2, 1, 0))
    rep["fcb"] = np.ascontiguousarray(g["ffn_conv_b"][0].reshape(48, 128).T)
    rep.update(_host_consts())

    in_maps = []
    for c in range(8):
        b, r = c // 4, c % 4
        pad = 1024 * (3 - r)
        xl = np.zeros((S, D), f32)
        xl[pad:] = x[b, 0:S - pad]
        valid = np.zeros((S,), f32)
        valid[pad:] = 1.0
        m = dict(rep)
        m["x_loc"] = xl
        m["stflag"] = np.ascontiguousarray(np.broadcast_to(valid[0:S:512][None, :], (128, 8)))
        m["validT"] = np.ascontiguousarray(valid.reshape(32, 128).T)
        vn = np.zeros((256,), f32)
        vn[pad // 16:255] = 1.0
        m["validn"] = np.ascontiguousarray(vn.reshape(2, 128).T)
        f0 = np.zeros((128, 64), f32)
        f0[:, pad // 64] = 1e4
        m["f0"] = f0
        in_maps.append(m)

    if "nc" not in _NC_CACHE:
        _NC_CACHE["nc"] = build_program()
    res = run_bass_kernel_spmd(_NC_CACHE["nc"], in_maps, core_ids=list(range(8)))
    out = np.zeros((2, S, D), f32)
    for c in range(8):
        b, r = c // 4, c % 4
        out[b, r * 1024:(r + 1) * 1024] = np.asarray(res.results[c]["y"], dtype=f32)
    return out
```

```python
import numpy as np
import ml_dtypes
import concourse.bass as bass
import concourse.mybir as mybir
from concourse.bass_utils import run_bass_kernel_spmd

F32 = mybir.dt.float32
BF16 = mybir.dt.bfloat16
AF = mybir.ActivationFunctionType
ALU = mybir.AluOpType

D = 2048
KC = 16
S = 4096
NOWN = 1026
EPS = 1e-6
NDMA = 12
USE_GELU_TANH_LUT = True


class Tk:
    __slots__ = ("name", "w", "r")

    def __init__(self, name):
        self.name = name
        self.w = None
        self.r = []


class Prog:
    def __init__(self, nc):
        self.nc = nc
        self.engs = ("pe", "act", "dve", "pool", "sp")
        self.sem = {k: nc.alloc_semaphore("sem_" + k) for k in self.engs}
        self.cnt = {k: 0 for k in self.engs}
        self.streams = {k: [] for k in self.engs}
        self.waited = {k: {} for k in self.engs}
        self.dq = {q: [[nc.alloc_semaphore("dq_%s_%d" % (q, i)), 0] for i in range(NDMA)] for q in ("sp", "pool")}
        self.dq_rr = {"sp": 0, "pool": 0}
        self.out_events = []

    def _wait(self, eng, evs):
        best = {}
        for ev in evs:
            if ev is None:
                continue
            key, sem, val = ev
            if key == eng and eng == "pe":
                continue
            if self.waited[eng].get(key, 0) >= val:
                continue
            if key not in best or best[key][1] < val:
                best[key] = (sem, val)
        for key, (sem, val) in best.items():
            self.waited[eng][key] = val
            self.streams[eng].append(lambda e, sem=sem, val=val: e.wait_ge(sem, val))

    def _deps(self, r, w):
        evs = []
        for t in r:
            evs.append(t.w)
        for t in w:
            evs.append(t.w)
            evs.extend(t.r)
        return evs

    def op(self, eng, fn, r=(), w=()):
        self._wait(eng, self._deps(r, w))
        self.cnt[eng] += 1
        sem = self.sem[eng]
        self.streams[eng].append(lambda e, fn=fn, sem=sem: fn(e).then_inc(sem, 1))
        ev = (eng, sem, self.cnt[eng])
        for t in r:
            t.r.append(ev)
        for t in w:
            t.w = ev
            t.r = []
        return ev

    def dma(self, q, out, in_, r=(), w=(), is_output=False):
        slot = self.dq[q][self.dq_rr[q] % NDMA]
        self.dq_rr[q] += 1
        sem = slot[0]
        key = "dma_" + str(id(slot))
        evs = self._deps(r, w)
        if slot[1] > 0:
            evs.append((key, sem, slot[1]))
        self._wait(q, evs)
        slot[1] += 16
        val = slot[1]
        self.streams[q].append(lambda e, out=out, in_=in_, sem=sem: e.dma_start(out=out, in_=in_).then_inc(sem, 16))
        ev = (key, sem, val)
        for t in r:
            t.r.append(ev)
        for t in w:
            t.w = ev
            t.r = []
        if is_output:
            self.out_events.append(ev)
        return ev

    def barrier(self):
        evs = [(k, self.sem[k], self.cnt[k]) for k in self.engs if self.cnt[k] > 0]
        for q in ("sp", "pool"):
            for slot in self.dq[q]:
                if slot[1] > 0:
                    evs.append(("dma_" + str(id(slot)), slot[0], slot[1]))
        for e in self.engs:
            self._wait(e, [ev for ev in evs if ev[0] != e])

    def finish(self):
        self.barrier()
        self._wait("sp", self.out_events)
        nc = self.nc
        st = self.streams
        with nc.Block() as block:
            @block.tensor
            def _(e):
                for f in st["pe"]:
                    f(e)

            @block.scalar
            def _(e):
                for f in st["act"]:
                    f(e)

            @block.vector
            def _(e):
                for f in st["dve"]:
                    f(e)

            @block.gpsimd
            def _(e):
                for f in st["pool"]:
                    f(e)

            @block.sync
            def _(e):
                for f in st["sp"]:
                    f(e)

    def act(self, out, in_, func, r=(), w=(), **kw):
        return self.op("act", lambda e: e.activation(out=out, in_=in_, func=func, **kw), r, w)

    def mm(self, out, lhsT, rhs, start, stop, r=(), w=()):
        return self.op("pe", lambda e: e.matmul(out, lhsT, rhs, start=start, stop=stop), r, w)

    def tr(self, out, in_, ident, r=(), w=()):
        return self.op("pe", lambda e: e.transpose(out, in_, ident), r, w)

    def tt(self, eng, out, in0, in1, op, r=(), w=()):
        return self.op(eng, lambda e: e.tensor_tensor(out=out, in0=in0, in1=in1, op=op), r, w)

    def ts(self, eng, out, in0, s1, s2, op0, op1=None, r=(), w=()):
        if op1 is None:
            return self.op(eng, lambda e: e.tensor_scalar(out=out, in0=in0, scalar1=s1, scalar2=None, op0=op0), r, w)
        return self.op(eng, lambda e: e.tensor_scalar(out=out, in0=in0, scalar1=s1, scalar2=s2, op0=op0, op1=op1), r, w)

    def stt(self, out, in0, scalar, in1, op0, op1, r=(), w=()):
        return self.op("dve", lambda e: e.scalar_tensor_tensor(out=out, in0=in0, scalar=scalar, in1=in1, op0=op0, op1=op1), r, w)

    def cp(self, eng, out, in_, r=(), w=()):
        if eng == "act":
            return self.act(out, in_, AF.Copy, r, w)
        return self.op(eng, lambda e: e.tensor_copy(out=out, in_=in_), r, w)


class Arena:
    def __init__(self, nc, base, limit):
        self.nc = nc
        self.off = base
        self.limit = limit
        self.n = 0

    def alloc(self, name, shape, dt):
        esz = 2 if dt == BF16 else 4
        nb = esz
        for s in shape[1:]:
            nb *= s
        nb = (nb + 31) // 32 * 32
        t = self.nc.alloc_sbuf_tensor_at("%s_%d" % (name, self.n), list(shape), dt, offset=self.off)
        self.n += 1
        self.off += nb
        assert self.off <= self.limit, ("SBUF overflow", name, self.off, self.limit)
        return t

    def mark(self):
        return self.off

    def release(self, m):
        self.off = m


def build_program():
    nc = bass.Bass("TRN2", target_bir_lowering=False)
    P = Prog(nc)

    def din(name, shape, dt=F32):
        return nc.dram_tensor(name, list(shape), dt, kind="ExternalInput").ap()

    x_loc = din("x_loc", [S, D])
    stflag_d = din("stflag", [128, 8])
    validT_d = din("validT", [128, 32])
    validn_d = din("validn", [128, 2])
    f0_d = din("f0", [128, 64])
    wA1_d = din("wA1", [128, KC, 1024])
    wB3_d = din("wB3", [128, KC, 1024])
    wA2_d = din("wA2", [128, KC, 1024])
    wB1_d = din("wB1", [128, KC, 536])
    wB2_d = din("wB2", [128, KC, 1024])
    gmixT_d = din("gmixT", [128, KC])
    lcw_d = din("lcw", [128, 8, 4])
    lvec_d = din("lvec", [128, 4, 8])
    wa_d = din("wa", [128, 8, 128])
    wx_d = din("wx", [128, 8, 128])
    w1k_d = din("w1k", [128, 32, 128])
    w1v_d = din("w1v", [128, 32, 128])
    pe_d = din("peT", [128, 2, 32])
    cmpb_d = din("cmpb", [128, 2])
    w2k_d = din("w2k", [128, 128])
    w2v_d = din("w2v", [128, 128])
    ident_d = din("ident", [128, 128], BF16)
    ebig_d = din("ebig", [128, S], BF16)
    ovl_d = din("ovl", [128, 2, 64])
    cwide_d = din("cwide", [128, 3, 128])
    cwide_h_d = din("cwide_h", [2, 3, 128])
    wol_d = din("wol", [128, 8, D])
    woa_d = din("woa", [128, 8, D])
    gout_d = din("gout", [128, 2, 8])
    gffn_d = din("gffn_bc", [128, D])
    gfin_d = din("gfin_bc", [128, D])
    wup_d = din("wup", [24, 128, KC, 512])
    wdn_d = din("wdn", [24, 128, 2, D])
    fcw_d = din("fcw", [128, 48, 3])
    fcb_d = din("fcb", [128, 48])
    y_out = nc.dram_tensor("y", [1024, D], F32, kind="ExternalOutput").ap()

    wsc = {}
    wsc_t = {}
    for nm, ncl in (("B3", 1024), ("A2", 1024), ("B1", 536), ("B2", 1024)):
        wsc[nm] = nc.dram_tensor("wsc_" + nm, [128, KC, ncl], BF16, kind="Internal").ap()
        wsc_t[nm] = [Tk("wsc_%s%d" % (nm, i)) for i in range(4)]
    wraw = {"B3": wB3_d, "A2": wA2_d, "B1": wB1_d, "B2": wB2_d}
    prep_chunks = []

    base = (int(nc.sbuf_base) + 63) // 64 * 64
    A = Arena(nc, base, int(nc.sbuf_base) + int(nc.sbuf_bytes_remaining) - 64)
    banks = [nc.alloc_psum_tensor("bank%d" % i, [128, 512], F32) for i in range(8)]
    bk = [Tk("bank%d" % i) for i in range(8)]

    def bf(bank_ap):
        return bank_ap.bitcast(BF16)

    ident = A.alloc("ident", [128, 128], BF16)
    ident_t = Tk("ident")
    P.dma("sp", ident[:, :], ident_d[:, :], w=[ident_t])
    gmixT = A.alloc("gmixT", [128, KC], F32)
    gmixT_t = Tk("gmixT")
    P.dma("sp", gmixT[:, :], gmixT_d[:, :], w=[gmixT_t])
    ones_bf = A.alloc("ones", [128, 2], BF16)
    ones_t = Tk("ones")
    P.op("dve", lambda e: e.memset(ones_bf[:, :], 1.0), w=[ones_t])

    epsb = A.alloc("epsb", [128, 1], F32)
    epsb_t = Tk("epsb")
    P.op("dve", lambda e: e.memset(epsb[:, :], EPS), w=[epsb_t])
    oneb = A.alloc("oneb", [128, 1], F32)
    oneb_t = Tk("oneb")
    P.op("dve", lambda e: e.memset(oneb[:, :], 1.0), w=[oneb_t])
    y_base = A.mark()
    ylruT = A.alloc("ylruT", [128, 8, NOWN], BF16)
    ylru_t = [Tk("ylru%d" % c) for c in range(8)]
    yattT = A.alloc("yattT", [128, 8, NOWN], BF16)
    yatt_t = Tk("yatt")
    y_end = A.mark()

    m_mixer = A.mark()

    def proj_pass(w_dram, ncols, sts, groups, nrot=6, scratch=None):
        m0 = A.mark()
        W = A.alloc("W", [128, KC, ncols], BF16)
        W_q = [Tk("Wq%d" % i) for i in range(4)]
        W_t = [W_q[k // 4] for k in range(KC)] if scratch is not None else [Tk("W%d" % k) for k in range(KC)]
        xt = [A.alloc("xt", [128, D], F32) for _ in range(2)]
        xt_t = [Tk("xt0"), Tk("xt1")]
        if scratch is not None:
            for q4 in range(4):
                P.dma("sp", W[:, 4 * q4:4 * q4 + 4, :], wsc[scratch][:, 4 * q4:4 * q4 + 4, :], r=[wsc_t[scratch][q4]], w=[W_q[q4]])
        else:
            slots = [xt[0][:, 0:1024], xt[0][:, 1024:2048], xt[1][:, 0:1024], xt[1][:, 1024:2048]]
            slot_t = [Tk("wslot%d" % i) for i in range(4)]
            for k in range(KC):
                i4 = k % 4
                P.dma("sp", slots[i4][:, 0:ncols], w_dram[:, k, :], w=[slot_t[i4]])
                P.ts("dve" if k % 2 == 0 else "pool", W[:, k, :], slots[i4][:, 0:ncols], gmixT[:, k:k + 1], None, ALU.mult,
                     r=[slot_t[i4], gmixT_t], w=[W_t[k]])
            for i4 in range(4):
                xt_t[i4 // 2].r.extend(slot_t[i4].r)
        xn = [A.alloc("xn", [128, D], BF16) for _ in range(2)]
        xn_t = [Tk("xn0"), Tk("xn1")]
        ss = [A.alloc("ss", [128, 4], F32) for _ in range(2)]
        ss_t = [Tk("ss0"), Tk("ss1")]
        xnT = [A.alloc("xnT", [128, KC, 512], BF16) for _ in range(2)]
        xnT_t = [Tk("xnT0"), Tk("xnT1")]
        ctx = dict(W=W, W_t=W_t)
        for g in groups:
            if "setup" in g:
                g["setup"](ctx)
        rot = [2]

        def prep_tile(si, tt):
            tk = sts[si] * 4 + tt
            b = tk % 2
            P.dma("sp", xt[b][:, :], x_loc[tk * 128:(tk + 1) * 128, :], w=[xt_t[b]])
            P.act(xn[b][:, :], xt[b][:, :], AF.Square, r=[xt_t[b]], w=[xn_t[b], ss_t[b]], accum_out=ss[b][:, 0:1])
            P.act(ss[b][:, 1:2], ss[b][:, 0:1], AF.Ln, r=[ss_t[b]], w=[ss_t[b]], scale=1.0 / D, bias=epsb[:, 0:1])
            P.act(ss[b][:, 2:3], ss[b][:, 1:2], AF.Exp, r=[ss_t[b]], w=[ss_t[b]], scale=-0.5)
            P.act(xn[b][:, :], xt[b][:, :], AF.Copy, r=[xt_t[b], ss_t[b]], w=[xn_t[b]], scale=ss[b][:, 2:3])

        def prep_tr(si, tt):
            tk = sts[si] * 4 + tt
            b = tk % 2
            xb = si % 2
            for half in range(2):
                pb = banks[half]
                for j in range(8):
                    jj = half * 8 + j
                    P.tr(bf(pb[:, :])[:, j * 128:(j + 1) * 128], xn[b][:, jj * 128:(jj + 1) * 128], ident[:, :],
                         r=[xn_t[b], ident_t], w=[bk[half]])
                P.cp("dve" if half == 0 else "act",
                     xnT[xb][:, half * 8:(half + 1) * 8, tt * 128:(tt + 1) * 128],
                     bf(pb[:, :]).rearrange("p (j t) -> p j t", j=8),
                     r=[bk[half]], w=[xnT_t[xb]])

        def make_units(si):
            st = sts[si]
            xb = si % 2
            units = []
            for g in groups:
                if st not in g.get("sts", sts):
                    continue
                if g["kind"] == "fm":
                    for j in range(g["ncols"] // 128):
                        def mmf(g=g, j=j):
                            bi = rot[0]
                            rot[0] = 2 + (rot[0] - 1) % nrot
                            for k in range(KC):
                                P.mm(banks[bi][:, :], W[:, k, g["col0"] + j * 128: g["col0"] + (j + 1) * 128], xnT[xb][:, k, :],
                                     k == 0, k == KC - 1, r=[W_t[k], xnT_t[xb]], w=[bk[bi]])
                            return bi
                        units.append((mmf, lambda bi, g=g, j=j: g["evac"](st, j, banks[bi], bk[bi])))
                else:
                    nco = g["ncols"]
                    for tt in range(4):
                        def mmf(g=g, tt=tt, nco=nco):
                            bi = rot[0]
                            rot[0] = 2 + (rot[0] - 1) % nrot
                            for k in range(KC):
                                P.mm(banks[bi][:, 0:nco], xnT[xb][:, k, tt * 128:(tt + 1) * 128], W[:, k, g["col0"]: g["col0"] + nco],
                                     k == 0, k == KC - 1, r=[W_t[k], xnT_t[xb]], w=[bk[bi]])
                            return bi
                        units.append((mmf, lambda bi, g=g, tt=tt: g["evac"](st, tt, banks[bi], bk[bi])))
            return units

        for tt in range(4):
            prep_tile(0, tt)
            prep_tr(0, tt)
        for si in range(len(sts)):
            units = make_units(si)
            n = len(units)
            sched = {}
            if si + 1 < len(sts):
                for k in range(4):
                    u_tile = (k * n) // 4
                    sched.setdefault(u_tile, []).append(("tile", k))
                    u_tr = min(n - 1, u_tile + max(1, n // 8))
                    sched.setdefault(u_tr, []).append(("tr", k))
            pending = None
            for ui, (mmf, evf) in enumerate(units):
                for kind, k in sched.get(ui, []):
                    if kind == "tile":
                        prep_tile(si + 1, k)
                    else:
                        prep_tr(si + 1, k)
                bi = mmf()
                if pending is not None:
                    pending[0](pending[1])
                pending = (evf, bi)
            pending[0](pending[1])
        P.barrier()
        A.release(m0)


    def run_lru():
        m0 = A.mark()
        h_own = A.alloc("h_own", [128, 8, NOWN], F32)
        h_own_t = [Tk("h_own%d" % c) for c in range(8)]
        m_l = A.mark()
        lcw = A.alloc("lcw", [128, 8, 4], F32)
        lvec = A.alloc("lvec", [128, 4, 8], F32)
        c12 = A.alloc("c12", [128, 2, 8], F32)
        lsm_t = Tk("lsm")
        P.dma("sp", lcw[:, :, :], lcw_d[:, :, :], w=[lsm_t])
        P.dma("sp", lvec[:, :, :], lvec_d[:, :, :], w=[lsm_t])
        P.act(c12[:, 0, :], lvec[:, 3, :], AF.Exp, r=[lsm_t], w=[lsm_t], scale=-1.0)
        P.act(c12[:, 0, :], c12[:, 0, :], AF.Ln, r=[lsm_t], w=[lsm_t], bias=oneb[:, 0:1])
        P.ts("dve", c12[:, 1, :], c12[:, 0, :], -16.0, None, ALU.mult, r=[lsm_t], w=[lsm_t])
        P.ts("dve", c12[:, 0, :], c12[:, 0, :], -8.0, None, ALU.mult, r=[lsm_t], w=[lsm_t])
        wa = A.alloc("wa", [128, 8, 128], F32)
        wx = A.alloc("wx", [128, 8, 128], F32)
        wax_t = Tk("wax")
        P.dma("sp", wa[:, :, :], wa_d[:, :, :], w=[wax_t])
        P.dma("sp", wx[:, :, :], wx_d[:, :, :], w=[wax_t])
        stf = A.alloc("stf", [128, 8], F32)
        stf_t = Tk("stf")
        P.dma("sp", stf[:, :], stflag_d[:, :], w=[stf_t])
        hin = A.alloc("hin", [128, 8], F32)
        hin_t = [Tk("hin%d" % c) for c in range(8)]
        xbuf = A.alloc("xbuf", [128, 8, 515], F32)
        xbuf_t = [Tk("xbuf%d" % c) for c in range(8)]
        hst = A.alloc("hst", [128, 8], F32)
        hst_t = [Tk("hst%d" % c) for c in range(8)]
        for c in range(8):
            P.op("pool", lambda e, c=c: e.memset(xbuf[:, c, 0:3], 0.0), w=[xbuf_t[c]])
        NB = 2
        tmp = {}
        tmp_t = {}
        for nm in ("u", "r", "i", "a", "h"):
            tmp[nm] = [A.alloc("l" + nm, [128, 512], F32) for _ in range(NB)]
            tmp_t[nm] = [Tk("l%s%d" % (nm, b)) for b in range(NB)]
        tmp["s"], tmp_t["s"] = tmp["r"], tmp_t["r"]
        tmp["v"], tmp_t["v"] = tmp["i"], tmp_t["i"]
        NPF = 3
        pf = [A.alloc("pf", [128, 512], F32) for _ in range(NPF)]
        pf_t = [Tk("pf%d" % i) for i in range(NPF)]
        pb_ = [A.alloc("pb", [128, 512], BF16) for _ in range(NPF)]
        pb_t = [Tk("pb%d" % i) for i in range(NPF)]
        chunk_list = []
        for nm_ in ("B3", "A2", "B1", "B2"):
            ncl = wraw[nm_].shape[2]
            for k in range(KC):
                for c0 in range(0, ncl, 512):
                    chunk_list.append((nm_, k, c0, min(ncl, c0 + 512)))
        pstate = [0]

        def prep_step():
            j = pstate[0]
            pstate[0] += 1
            if 0 <= j - 2 < len(chunk_list):
                nm_, k, c0, c1 = chunk_list[j - 2]
                i3 = (j - 2) % NPF
                P.dma("pool", wsc[nm_][:, k, c0:c1], pb_[i3][:, 0:c1 - c0], r=[pb_t[i3]], w=[wsc_t[nm_][k // 4]])
            if 0 <= j - 1 < len(chunk_list):
                nm_, k, c0, c1 = chunk_list[j - 1]
                i3 = (j - 1) % NPF
                P.act(pb_[i3][:, 0:c1 - c0], pf[i3][:, 0:c1 - c0], AF.Copy, r=[pf_t[i3], gmixT_t], w=[pb_t[i3]], scale=gmixT[:, k:k + 1])
            if j < len(chunk_list):
                nm_, k, c0, c1 = chunk_list[j]
                i3 = j % NPF
                P.dma("sp", pf[i3][:, 0:c1 - c0], wraw[nm_][:, k, c0:c1], w=[pf_t[i3]])
            return j - 2 < len(chunk_list)

        prep_chunks.append(prep_step)
        gps = [6, 7]

        def evac(st, c, ps, ps_t):
            b = c % NB
            for _ in range(3):
                if prep_chunks and not prep_chunks[0]():
                    prep_chunks.pop(0)
            if st > 0:
                P.cp("dve", xbuf[:, c, 0:3], xbuf[:, c, 512:515], r=[xbuf_t[c]], w=[xbuf_t[c]])
            P.cp("act", xbuf[:, c, 3:515], ps[:, :], r=[ps_t], w=[xbuf_t[c]])
            u = tmp["u"][b]
            ut = tmp_t["u"][b]
            P.ts("dve", u[:, :], xbuf[:, c, 3:515], lcw[:, c, 3:4], lvec[:, 0, c:c + 1], ALU.mult, ALU.add,
                 r=[xbuf_t[c], lsm_t], w=[ut])
            for j in range(3):
                P.stt(u[:, :], xbuf[:, c, j:j + 512], lcw[:, c, j:j + 1], u[:, :], ALU.mult, ALU.add,
                      r=[xbuf_t[c], lsm_t, ut], w=[ut])
            P.mm(banks[gps[0]][:, :], wa[:, c, :], u[:, :], True, True, r=[wax_t, ut], w=[bk[gps[0]]])
            P.mm(banks[gps[1]][:, :], wx[:, c, :], u[:, :], True, True, r=[wax_t, ut], w=[bk[gps[1]]])
            rr, ii, aa, sq, vv = tmp["r"][b], tmp["i"][b], tmp["a"][b], tmp["s"][b], tmp["v"][b]
            P.act(rr[:, :], banks[gps[0]][:, :], AF.Sigmoid, r=[bk[gps[0]], lsm_t], w=[tmp_t["r"][b]], bias=lvec[:, 1, c:c + 1])
            P.act(ii[:, :], banks[gps[1]][:, :], AF.Sigmoid, r=[bk[gps[1]], lsm_t], w=[tmp_t["i"][b]], bias=lvec[:, 2, c:c + 1])
            P.act(aa[:, :], rr[:, :], AF.Exp, r=[tmp_t["r"][b], lsm_t], w=[tmp_t["a"][b]], scale=c12[:, 0, c:c + 1])
            P.act(sq[:, :], rr[:, :], AF.Exp, r=[tmp_t["r"][b], lsm_t], w=[tmp_t["s"][b]], scale=c12[:, 1, c:c + 1])
            P.act(sq[:, :], sq[:, :], AF.Ln, r=[tmp_t["s"][b], oneb_t], w=[tmp_t["s"][b]], scale=-1.0, bias=oneb[:, 0:1])
            P.act(sq[:, :], sq[:, :], AF.Exp, r=[tmp_t["s"][b]], w=[tmp_t["s"][b]], scale=0.5)
            P.tt("dve", vv[:, :], ii[:, :], u[:, :], ALU.mult, r=[tmp_t["i"][b], ut], w=[tmp_t["v"][b]])
            P.tt("dve", vv[:, :], vv[:, :], sq[:, :], ALU.mult, r=[tmp_t["s"][b], tmp_t["v"][b]], w=[tmp_t["v"][b]])
            if st >= 6:
                hout = h_own[:, c, 2 + (st - 6) * 512: 2 + (st - 5) * 512]
                ht = h_own_t[c]
            else:
                hout = tmp["h"][b][:, :]
                ht = tmp_t["h"][b]
            if st == 0:
                init = 0.0
            else:
                P.tt("dve", hin[:, c:c + 1], hst[:, c:c + 1], stf[:, st - 1:st], ALU.mult, r=[hst_t[c], stf_t], w=[hin_t[c]])
                init = hin[:, c:c + 1]
            P.op("dve", lambda e, hout=hout, aa=aa, vv=vv, init=init: e.tensor_tensor_scan(
                out=hout, data0=aa[:, :], data1=vv[:, :], initial=init, op0=ALU.mult, op1=ALU.add),
                r=[tmp_t["a"][b], tmp_t["v"][b], hin_t[c]], w=[ht])
            P.cp("dve", hst[:, c:c + 1], hout[:, 511:512], r=[ht], w=[hst_t[c]])
            if st == 5:
                P.cp("dve", h_own[:, c, 0:2], hout[:, 510:512], r=[ht], w=[h_own_t[c]])

        proj_pass(wA1_d, 1024, list(range(8)), [dict(kind="fm", col0=0, ncols=1024, evac=evac)], nrot=4)
        assert not prep_chunks
        A.release(m_l)

        gt = [A.alloc("gt", [128, 512], F32) for _ in range(2)]
        gt_t = [Tk("gt0"), Tk("gt1")]
        gt2 = [A.alloc("gt2", [128, 512], F32) for _ in range(2)]
        gt2_t = [Tk("gt20"), Tk("gt21")]

        def evac_gate(st, c, ps, ps_t):
            b = c % 2
            if st == 5:
                lo, hi, o0 = 510, 512, 0
            else:
                lo, hi, o0 = 0, 512, 2 + (st - 6) * 512
            n = hi - lo
            gelu(gt[b][:, 0:n], ps[:, lo:hi], gt2[b][:, 0:n], [ps_t], gt_t[b], gt2_t[b])
            P.tt("dve", ylruT[:, c, o0:o0 + n], gt[b][:, 0:n], h_own[:, c, o0:o0 + n], ALU.mult,
                 r=[gt_t[b], h_own_t[c]], w=[ylru_t[c]])

        proj_pass(wB3_d, 1024, [5, 6, 7], [dict(kind="fm", col0=0, ncols=1024, evac=evac_gate)], scratch="B3")
        A.release(m0)

    def gelu(out, in_, scratch, in_t, out_t, scratch_t):
        if USE_GELU_TANH_LUT:
            P.act(out, in_, AF.Gelu_apprx_tanh, r=in_t, w=[out_t])
            return
        P.act(scratch, in_, AF.Square, r=in_t, w=[scratch_t])
        P.ts("dve", scratch, scratch, 0.044715, 1.0, ALU.mult, ALU.add, r=[scratch_t], w=[scratch_t])
        P.tt("dve", scratch, scratch, in_, ALU.mult, r=[scratch_t] + list(in_t), w=[scratch_t])
        P.act(scratch, scratch, AF.Sigmoid, r=[scratch_t], w=[scratch_t], scale=1.5957691216057308)
        P.tt("dve", out, scratch, in_, ALU.mult, r=[scratch_t] + list(in_t), w=[out_t])


    run_lru()

    kselT = A.alloc("kselT", [128, 2, S], BF16)
    ksel_t = Tk("kselT")
    vsel = A.alloc("vsel", [128, 32, 2, 129], BF16)
    vsel_t = Tk("vsel")
    kcmpT = A.alloc("kcmpT", [128, 2, 256], BF16)
    kcmp_t = Tk("kcmpT")
    Rc = A.alloc("Rc", [128, 2, 2, 193], BF16)
    Rc_t = Tk("Rc")
    validT = A.alloc("validT", [128, 32], F32)
    validT_t = Tk("validT")
    P.dma("sp", validT[:, :], validT_d[:, :], w=[validT_t])
    P.op("dve", lambda e: e.tensor_copy(out=vsel[:, :, 0, 128:129], in_=validT[:, :].unsqueeze(2)), r=[validT_t], w=[vsel_t])
    P.op("dve", lambda e: e.tensor_copy(out=vsel[:, :, 1, 128:129], in_=validT[:, :].unsqueeze(2)), r=[validT_t], w=[vsel_t])
    m_a2 = A.mark()
    kvcT = A.alloc("kvcT", [128, 4, S], BF16)
    kvc_t = Tk("kvcT")

    def evac_a2_fm(st, j, ps, ps_t):
        eng = "act" if j % 2 == 0 else "dve"
        if j < 4:
            P.cp(eng, kvcT[:, j, st * 512:(st + 1) * 512], ps[:, :], r=[ps_t], w=[kvc_t])
        else:
            P.cp(eng, kselT[:, j - 4, st * 512:(st + 1) * 512], ps[:, :], r=[ps_t], w=[ksel_t])

    def evac_a2_v(st, tt, ps, ps_t):
        ch = st * 4 + tt
        P.cp("act" if tt % 2 else "dve", vsel[:, ch, :, 0:128], ps[:, 0:256].rearrange("p (h d) -> p h d", h=2),
             r=[ps_t], w=[vsel_t])

    proj_pass(wA2_d, 1024, list(range(8)),
              [dict(kind="fm", col0=0, ncols=768, evac=evac_a2_fm),
               dict(kind="tm", col0=768, ncols=256, evac=evac_a2_v)], scratch="A2")

    def run_compress():
        m0 = A.mark()
        w1 = [A.alloc("w1", [128, 32, 128], BF16) for _ in range(2)]
        w1_t = [Tk("w1k"), Tk("w1v")]
        P.dma("pool", w1[0][:, :, :], w1k_d[:, :, :], w=[w1_t[0]])
        P.dma("pool", w1[1][:, :, :], w1v_d[:, :, :], w=[w1_t[1]])
        w2 = A.alloc("w2", [128, 2, 128], BF16)
        w2_t = Tk("w2")
        P.dma("pool", w2[:, 0, :], w2k_d[:, :], w=[w2_t])
        P.dma("pool", w2[:, 1, :], w2v_d[:, :], w=[w2_t])
        peT = A.alloc("peT", [128, 2, 32], BF16)
        peT_t = Tk("peT")
        P.dma("pool", peT[:, :, :], pe_d[:, :, :], w=[peT_t])
        cb = A.alloc("cb", [128, 4], F32)
        cb_t = Tk("cb")
        P.dma("sp", cb[:, 0:2], cmpb_d[:, :], w=[cb_t])
        vn = A.alloc("vn", [128, 2], F32)
        ovl = A.alloc("ovl", [128, 2, 64], F32)
        vn_t = Tk("vn")
        P.dma("sp", vn[:, :], validn_d[:, :], w=[vn_t])
        P.dma("sp", ovl[:, :, :], ovl_d[:, :, :], w=[vn_t])
        for ty in range(2):
            for l in range(32):
                P.mm(banks[2][:, ty:ty + 1], w1[ty][:, l, :], peT[:, ty, l:l + 1], l == 0, l == 31,
                     r=[w1_t[ty], peT_t], w=[bk[2]])
            P.tt("dve", cb[:, 2 + ty:3 + ty], banks[2][:, ty:ty + 1], cb[:, ty:ty + 1], ALU.add, r=[bk[2], cb_t], w=[cb_t])
        hid = [A.alloc("hid", [128, 256], F32) for _ in range(2)]
        hid_t = [Tk("hid0"), Tk("hid1")]
        hs = [A.alloc("hs", [128, 256], F32) for _ in range(2)]
        hs_t = [Tk("hs0"), Tk("hs1")]
        hp = [A.alloc("hp", [128, 256], F32) for _ in range(2)]
        hp_t = [Tk("hp0"), Tk("hp1")]
        hb = [A.alloc("hb", [128, 256], BF16) for _ in range(2)]
        hb_t = [Tk("hb0"), Tk("hb1")]
        for c2 in range(2):
            for hk in range(2):
                P.cp("dve", Rc[:, c2, hk, 0:1], vn[:, c2:c2 + 1], r=[vn_t], w=[Rc_t])
                P.ts("dve", Rc[:, c2, hk, 1:65], ovl[:, c2, :], vn[:, c2:c2 + 1], None, ALU.mult, r=[vn_t], w=[Rc_t])
        it = 0
        for hk in range(2):
            for ty in range(2):
                b = it % 2
                it += 1
                pb = 3 + b
                for l in range(32):
                    P.mm(banks[pb][:, 0:255], w1[ty][:, l, :], kvcT[:, ty * 2 + hk, l: l + 16 * 254 + 1: 16], l == 0, l == 31,
                         r=[w1_t[ty], kvc_t], w=[bk[pb]])
                P.ts("dve", hp[b][:, 0:255], banks[pb][:, 0:255], cb[:, 2 + ty:3 + ty], None, ALU.add, r=[bk[pb], cb_t], w=[hp_t[b]])
                gelu(hid[b][:, 0:255], hp[b][:, 0:255], hs[b][:, 0:255], [hp_t[b]], hid_t[b], hs_t[b])
                P.cp("dve", hb[b][:, 0:255], hid[b][:, 0:255], r=[hid_t[b]], w=[hb_t[b]])
                if ty == 0:
                    P.mm(banks[5][:, 0:255], w2[:, 0, :], hb[b][:, 0:255], True, True, r=[w2_t, hb_t[b]], w=[bk[5]])
                    P.cp("act", kcmpT[:, hk, 0:255], banks[5][:, 0:255], r=[bk[5]], w=[kcmp_t])
                else:
                    for c2 in range(2):
                        rows = 128 if c2 == 0 else 127
                        P.mm(banks[6 + c2][0:rows, 0:128], hb[b][:, c2 * 128: c2 * 128 + rows], w2[:, 1, :], True, True,
                             r=[w2_t, hb_t[b]], w=[bk[6 + c2]])
                        P.ts("dve", Rc[0:rows, c2, hk, 65:193], banks[6 + c2][0:rows, 0:128], vn[0:rows, c2:c2 + 1], None, ALU.mult,
                             r=[bk[6 + c2], vn_t], w=[Rc_t])
        P.barrier()
        A.release(m0)

    run_compress()
    A.release(m_a2)

    kwT = A.alloc("kwT", [128, 2, 2048], BF16)
    kw_t = Tk("kwT")
    vwin = A.alloc("vwin", [128, 16, 2, 129], BF16)
    vwin_t = Tk("vwin")
    gsig = A.alloc("gsig", [128, 9, 24], F32)
    gsig_t = Tk("gsig")
    qT = A.alloc("qT", [128, 8, NOWN], BF16)
    qT_t = Tk("qT")
    P.op("dve", lambda e: e.tensor_copy(out=vwin[:, :, 0, 128:129], in_=validT[:, 16:32].unsqueeze(2)), r=[validT_t], w=[vwin_t])
    P.op("dve", lambda e: e.tensor_copy(out=vwin[:, :, 1, 128:129], in_=validT[:, 16:32].unsqueeze(2)), r=[validT_t], w=[vwin_t])
    def evac_b1_k(st, j, ps, ps_t):
        P.cp("act" if j else "dve", kwT[:, j, (st - 4) * 512:(st - 3) * 512], ps[:, :], r=[ps_t], w=[kw_t])

    def evac_b1_v(st, tt, ps, ps_t):
        ch = (st - 4) * 4 + tt
        P.cp("dve", vwin[:, ch, :, 0:128], ps[:, 0:256].rearrange("p (h d) -> p h d", h=2), r=[ps_t], w=[vwin_t])
        if st >= 6:
            ti = 1 + (st - 6) * 4 + tt
            P.act(gsig[:, ti, :], ps[:, 256:280], AF.Sigmoid, r=[ps_t], w=[gsig_t])

    halo_ctx = {}

    proj_pass(wB1_d, 536, [4, 5, 6, 7],
              [dict(kind="fm", col0=0, ncols=256, evac=evac_b1_k),
               dict(kind="tm", col0=256, ncols=280, evac=evac_b1_v)], scratch="B1")

    def setup_b2(ctx):
        halo_ctx.update(ctx)

    def evac_b2(st, j, ps, ps_t):
        if st == 5:
            lo, hi, o0 = 510, 512, 0
        else:
            lo, hi, o0 = 0, 512, 2 + (st - 6) * 512
        P.act(qT[:, j, o0:o0 + hi - lo], ps[:, lo:hi], AF.Copy, r=[ps_t], w=[qT_t], scale=float(128 ** -0.5))

    proj_pass(wB2_d, 1024, [5, 6, 7], [dict(kind="fm", col0=0, ncols=1024, evac=evac_b2)], scratch="B2")

    def halo_gates():
        m0 = A.mark()
        Wg = A.alloc("Wg", [128, KC, 24], BF16)
        Wg_t = Tk("Wg")
        sg = A.alloc("sg", [128, KC, 24], F32)
        sg_t = Tk("sg")
        P.dma("sp", sg[:, :, :], wB1_d[:, :, 512:536], w=[sg_t])
        for k in range(KC):
            P.ts("dve", Wg[:, k, :], sg[:, k, :], gmixT[:, k:k + 1], None, ALU.mult, r=[sg_t, gmixT_t], w=[Wg_t])
        xh = A.alloc("xh", [2, D], F32)
        xh_t = Tk("xh")
        P.dma("sp", xh[:, :], x_loc[3070:3072, :], w=[xh_t])
        xq = A.alloc("xq", [2, D], BF16)
        xq_t = Tk("xq")
        sh = A.alloc("sh", [2, 4], F32)
        sh_t = Tk("sh")
        P.act(xq[:, :], xh[:, :], AF.Square, r=[xh_t], w=[xq_t, sh_t], accum_out=sh[:, 0:1])
        P.act(sh[:, 1:2], sh[:, 0:1], AF.Ln, r=[sh_t], w=[sh_t], scale=1.0 / D, bias=epsb[0:2, 0:1])
        P.act(sh[:, 2:3], sh[:, 1:2], AF.Exp, r=[sh_t], w=[sh_t], scale=-0.5)
        P.act(xq[:, :], xh[:, :], AF.Copy, r=[xh_t, sh_t], w=[xq_t], scale=sh[:, 2:3])
        xqT = A.alloc("xqT", [128, KC, 2], BF16)
        xqT_t = Tk("xqT")
        for j in range(KC):
            P.tr(bf(banks[0][:, :])[:, j * 2:(j + 1) * 2], xq[:, j * 128:(j + 1) * 128], ident[0:2, 0:2], r=[xq_t, ident_t], w=[bk[0]])
        P.cp("dve", xqT[:, :, :], bf(banks[0][:, :])[:, 0:32].rearrange("p (j t) -> p j t", j=KC), r=[bk[0]], w=[xqT_t])
        for k in range(KC):
            P.mm(banks[2][0:2, 0:24], xqT[:, k, :], Wg[:, k, :], k == 0, k == KC - 1, r=[xqT_t, Wg_t], w=[bk[2]])
        P.act(gsig[0:2, 0, :], banks[2][0:2, 0:24], AF.Sigmoid, r=[bk[2]], w=[gsig_t])
        P.barrier()
        A.release(m0)

    halo_gates()

    def run_attention():
        m0 = A.mark()
        ebig = A.alloc("ebig", [128, S], BF16)
        cw = A.alloc("cw", [128, 3, 128], F32)
        cwh = A.alloc("cwh", [2, 3, 128], F32)
        f0 = A.alloc("f0", [128, 64], F32)
        cst_t = Tk("attn_consts")
        P.dma("sp", ebig[:, :], ebig_d[:, :], w=[cst_t])
        P.dma("sp", cw[:, :, :], cwide_d[:, :, :], w=[cst_t])
        P.dma("sp", cwh[:, :, :], cwide_h_d[:, :, :], w=[cst_t])
        P.dma("sp", f0[:, :], f0_d[:, :], w=[cst_t])
        NE = 4
        Pt = [A.alloc("Pm", [128, 4, 128], BF16) for _ in range(NE)]
        Pt_t = [Tk("Pm%d" % i) for i in range(NE)]
        Ec = [[A.alloc("Ec", [128, 4, 128], BF16) for _ in range(2)] for _ in range(2)]
        Ec_t = [[Tk("Ec%d%d" % (a, b)) for b in range(2)] for a in range(2)]
        sm = [A.alloc("sm", [128, 64], F32) for _ in range(2)]
        sm_t = [Tk("sm0"), Tk("sm1")]
        imp = [A.alloc("imp", [128, 64], F32) for _ in range(2)]
        sc2 = [A.alloc("sc2", [128, 64], F32) for _ in range(2)]
        top = [A.alloc("top", [128, 16], F32) for _ in range(2)]
        selb = [A.alloc("selb", [128, 64], BF16) for _ in range(2)]
        negm = [A.alloc("negm", [128, 4, 128], BF16) for _ in range(2)]
        tk_t = [Tk("topk0"), Tk("topk1")]
        selT_t = [Tk("selT0"), Tk("selT1")]
        acc = [A.alloc("acc", [128, 4, 128], F32) for _ in range(2)]
        acc_t = [Tk("acc0"), Tk("acc1")]
        ob = A.alloc("ob", [128, 4, 128], BF16)
        ob_t = Tk("ob")
        for hk_ in range(2):
            P.op("pool", lambda e, hk_=hk_: e.memset(negm[hk_][:, :, :], 0.0), w=[selT_t[hk_]])
        ectr = [0]
        PVB = [3, 4, 5, 6]

        def qblock(Dq, q0, nq, qc, gti):
            cwm = cwh if nq == 2 else cw
            lo = 64 - 2 * Dq

            def cmp_gen(hk):
                qv = qT[:, 4 * hk:4 * hk + 4, qc:qc + nq]
                cb0 = 3 + 2 * hk
                for c2 in range(2):
                    rows = 128 if c2 == 0 else 127
                    sb = c2
                    P.mm(banks[sb][0:rows, 0:4 * nq].rearrange("p (g q) -> p g q", g=4), kcmpT[:, hk, c2 * 128:c2 * 128 + rows], qv,
                         True, True, r=[kcmp_t, qT_t], w=[bk[sb]])
                    P.act(Ec[hk][c2][0:rows, :, 0:nq], banks[sb][0:rows, 0:4 * nq].rearrange("p (g q) -> p g q", g=4), AF.Exp,
                          r=[bk[sb]], w=[Ec_t[hk][c2]])
                    basev = 128 * Dq + q0 - 31 - 2048 * c2
                    P.op("pool", lambda e, c2=c2, rows=rows, basev=basev: e.affine_select(
                        out=Ec[hk][c2][0:rows, :, 0:nq], in_=Ec[hk][c2][0:rows, :, 0:nq], pattern=[[0, 4], [1, nq]],
                        compare_op=ALU.is_ge, fill=0.0, base=basev, channel_multiplier=-16), r=[Ec_t[hk][c2]], w=[Ec_t[hk][c2]])
                yield
                for g in range(4):
                    pb = cb0 + g // 2
                    co = (g % 2) * 193
                    for c2 in range(2):
                        rows = 128 if c2 == 0 else 127
                        P.mm(banks[pb][0:nq, co:co + 193], Ec[hk][c2][0:rows, g, 0:nq], Rc[0:rows, c2, hk, :], c2 == 0, c2 == 1,
                             r=[Ec_t[hk][c2], Rc_t], w=[bk[pb]])
                yield
                for g in range(4):
                    pb = cb0 + g // 2
                    co = (g % 2) * 193
                    P.ts("dve", sm[hk][0:nq, g:g + 1], banks[pb][0:nq, co:co + 1], 1e-30, None, ALU.max, r=[bk[pb]], w=[sm_t[hk]])
                P.op("dve", lambda e: e.reciprocal(out=sm[hk][0:nq, 4:8], in_=sm[hk][0:nq, 0:4]), r=[sm_t[hk]], w=[sm_t[hk]])
                P.tt("dve", sm[hk][0:nq, 8:12], sm[hk][0:nq, 4:8], gsig[0:nq, gti, 12 * hk:12 * hk + 10:3], ALU.mult, r=[sm_t[hk], gsig_t], w=[sm_t[hk]])
                yield
                for g in range(4):
                    pb = cb0 + g // 2
                    co = (g % 2) * 193
                    if g == 0:
                        P.ts("dve", imp[hk][0:nq, :], banks[pb][0:nq, co + 1:co + 65], sm[hk][0:nq, 4:5], None, ALU.mult, r=[bk[pb], sm_t[hk]], w=[tk_t[hk]])
                    else:
                        P.stt(imp[hk][0:nq, :], banks[pb][0:nq, co + 1:co + 65], sm[hk][0:nq, 4 + g:5 + g], imp[hk][0:nq, :], ALU.mult, ALU.add,
                              r=[bk[pb], sm_t[hk], tk_t[hk]], w=[tk_t[hk]])
                    P.ts("dve", acc[hk][0:nq, g, :], banks[pb][0:nq, co + 65:co + 193], sm[hk][0:nq, 8 + g:9 + g], None, ALU.mult,
                         r=[bk[pb], sm_t[hk]], w=[acc_t[hk]])
                    if g % 2 == 1:
                        yield
                P.tt("dve", imp[hk][0:nq, :], imp[hk][0:nq, :], cwm[0:nq, 0, lo:lo + 64], ALU.mult, r=[tk_t[hk], cst_t], w=[tk_t[hk]])
                P.tt("dve", imp[hk][0:nq, :], imp[hk][0:nq, :], cwm[0:nq, 1, lo:lo + 64], ALU.add, r=[tk_t[hk], cst_t], w=[tk_t[hk]])
                yield
                P.tt("dve", imp[hk][0:nq, :], imp[hk][0:nq, :], cwm[0:nq, 2, lo:lo + 64], ALU.max, r=[tk_t[hk], cst_t], w=[tk_t[hk]])
                P.tt("dve", imp[hk][0:nq, :], imp[hk][0:nq, :], f0[0:nq, :], ALU.max, r=[tk_t[hk], cst_t], w=[tk_t[hk]])
                yield
                P.op("dve", lambda e: e.max(out=top[hk][0:nq, 0:8], in_=imp[hk][0:nq, :]), r=[tk_t[hk]], w=[tk_t[hk]])
                yield
                P.op("dve", lambda e: e.match_replace(out=sc2[hk][0:nq, :], in_to_replace=top[hk][0:nq, 0:8], in_values=imp[hk][0:nq, :],
                                                      imm_value=-1e9), r=[tk_t[hk]], w=[tk_t[hk]])
                yield
                P.op("dve", lambda e: e.max(out=top[hk][0:nq, 8:16], in_=sc2[hk][0:nq, :]), r=[tk_t[hk]], w=[tk_t[hk]])
                yield
                P.ts("dve", sc2[hk][0:nq, :], imp[hk][0:nq, :], top[hk][0:nq, 15:16], None, ALU.is_ge, r=[tk_t[hk]], w=[tk_t[hk]])
                yield
                P.tt("dve", selb[hk][0:nq, :], sc2[hk][0:nq, :], cwm[0:nq, 0, lo:lo + 64], ALU.mult, r=[tk_t[hk], cst_t], w=[tk_t[hk]])
                yield
                to = 512 + hk * 128
                P.tr(bf(banks[7][:, :])[0:64, to:to + nq], selb[hk][0:nq, :], ident[0:nq, 0:nq], r=[tk_t[hk], ident_t], w=[bk[7]])
                yield
                P.ts("dve", negm[hk][0:64, :, 0:nq], bf(banks[7][:, :])[0:64, to:to + nq].unsqueeze(1).broadcast_to([64, 4, nq]),
                     -1.0, 30000.0, ALU.add, ALU.mult, r=[bk[7]], w=[selT_t[hk]])

            gens = [cmp_gen(0), cmp_gen(1)]
            while gens:
                for gen in list(gens):
                    try:
                        next(gen)
                    except StopIteration:
                        gens.remove(gen)

            for hk in range(2):
                qv = qT[:, 4 * hk:4 * hk + 4, qc:qc + nq]
                def sel_qk(kc):
                    sb = ectr[0] % 3
                    ei = ectr[0] % NE
                    ectr[0] += 1
                    so = banks[sb][:, 0:4 * nq].rearrange("p (g q) -> p g q", g=4)
                    P.mm(so, kselT[:, hk, kc * 128:(kc + 1) * 128], qv, True, False, r=[ksel_t, qT_t], w=[bk[sb]])
                    P.mm(so, ebig[:, kc * 128:(kc + 1) * 128], negm[hk][:, :, 0:nq], False, True, r=[cst_t, selT_t[hk]], w=[bk[sb]])
                    P.act(Pt[ei][:, :, 0:nq], so, AF.Exp, r=[bk[sb]], w=[Pt_t[ei]])
                    if kc == Dq:
                        P.op("pool", lambda e, ei=ei: e.affine_select(
                            out=Pt[ei][:, :, 0:nq], in_=Pt[ei][:, :, 0:nq], pattern=[[0, 4], [1, nq]],
                            compare_op=ALU.is_ge, fill=0.0, base=q0, channel_multiplier=-1), r=[Pt_t[ei]], w=[Pt_t[ei]])
                    return ei

                def sel_pv(kc, ei):
                    for g in range(4):
                        P.mm(banks[PVB[g]][0:nq, 0:129], Pt[ei][:, g, 0:nq], vsel[:, kc, hk, :], kc == 0, kc == Dq,
                             r=[Pt_t[ei], vsel_t], w=[bk[PVB[g]]])
                    if kc == Dq:
                        for g in range(4):
                            P.ts("dve", sm[hk][0:nq, 16 + g:17 + g], banks[PVB[g]][0:nq, 128:129], 1e-30, None, ALU.max, r=[bk[PVB[g]]], w=[sm_t[hk]])
                        P.op("dve", lambda e, hk=hk: e.reciprocal(out=sm[hk][0:nq, 20:24], in_=sm[hk][0:nq, 16:20]), r=[sm_t[hk]], w=[sm_t[hk]])
                        P.tt("dve", sm[hk][0:nq, 24:28], sm[hk][0:nq, 20:24], gsig[0:nq, gti, 12 * hk + 1:12 * hk + 11:3], ALU.mult, r=[sm_t[hk], gsig_t], w=[sm_t[hk]])
                        for g in range(4):
                            P.stt(acc[hk][0:nq, g, :], banks[PVB[g]][0:nq, 0:128], sm[hk][0:nq, 24 + g:25 + g], acc[hk][0:nq, g, :], ALU.mult, ALU.add,
                                  r=[bk[PVB[g]], sm_t[hk], acc_t[hk]], w=[acc_t[hk]])

                def win_qk(kc):
                    sb = ectr[0] % 3
                    ei = ectr[0] % NE
                    ectr[0] += 1
                    wc = kc - 16
                    P.mm(banks[sb][:, 0:4 * nq].rearrange("p (g q) -> p g q", g=4), kwT[:, hk, wc * 128:(wc + 1) * 128], qv,
                         True, True, r=[kw_t, qT_t], w=[bk[sb]])
                    P.act(Pt[ei][:, :, 0:nq], banks[sb][:, 0:4 * nq].rearrange("p (g q) -> p g q", g=4), AF.Exp,
                          r=[bk[sb]], w=[Pt_t[ei]])
                    if kc == Dq:
                        P.op("pool", lambda e, ei=ei: e.affine_select(
                            out=Pt[ei][:, :, 0:nq], in_=Pt[ei][:, :, 0:nq], pattern=[[0, 4], [1, nq]],
                            compare_op=ALU.is_ge, fill=0.0, base=q0, channel_multiplier=-1), r=[Pt_t[ei]], w=[Pt_t[ei]])
                    if kc == Dq - 4:
                        P.op("pool", lambda e, ei=ei: e.affine_select(
                            out=Pt[ei][:, :, 0:nq], in_=Pt[ei][:, :, 0:nq], pattern=[[0, 4], [-1, nq]],
                            compare_op=ALU.is_ge, fill=0.0, base=-q0 - 1, channel_multiplier=1), r=[Pt_t[ei]], w=[Pt_t[ei]])
                    return ei

                def win_pv(kc, ei):
                    wc = kc - 16
                    for g in range(4):
                        P.mm(banks[PVB[g]][0:nq, 0:129], Pt[ei][:, g, 0:nq], vwin[:, wc, hk, :], kc == Dq - 4, kc == Dq,
                             r=[Pt_t[ei], vwin_t], w=[bk[PVB[g]]])

                steps = [(sel_qk, sel_pv, kc) for kc in range(Dq + 1)] + [(win_qk, win_pv, kc) for kc in range(Dq - 4, Dq + 1)]
                LA = 2
                eis = [steps[j][0](steps[j][2]) for j in range(min(LA, len(steps)))]
                for si_, (fq, fp, kc) in enumerate(steps):
                    if si_ + LA < len(steps):
                        eis.append(steps[si_ + LA][0](steps[si_ + LA][2]))
                    fp(kc, eis[si_])
                for g in range(4):
                    P.ts("dve", sm[hk][0:nq, 32 + g:33 + g], banks[PVB[g]][0:nq, 128:129], 1e-30, None, ALU.max, r=[bk[PVB[g]]], w=[sm_t[hk]])
                P.op("dve", lambda e, hk=hk: e.reciprocal(out=sm[hk][0:nq, 36:40], in_=sm[hk][0:nq, 32:36]), r=[sm_t[hk]], w=[sm_t[hk]])
                P.tt("dve", sm[hk][0:nq, 40:44], sm[hk][0:nq, 36:40], gsig[0:nq, gti, 12 * hk + 2:12 * hk + 12:3], ALU.mult, r=[sm_t[hk], gsig_t], w=[sm_t[hk]])
                for g in range(4):
                    P.stt(ob[0:nq, g, :], banks[PVB[g]][0:nq, 0:128], sm[hk][0:nq, 40 + g:41 + g], acc[hk][0:nq, g, :], ALU.mult, ALU.add,
                          r=[bk[PVB[g]], sm_t[hk], acc_t[hk]], w=[ob_t])
                for g in range(4):
                    P.tr(bf(banks[7][:, :])[:, g * 128:g * 128 + nq], ob[0:nq, g, :], ident[0:nq, 0:nq], r=[ob_t, ident_t], w=[bk[7]])
                P.cp("act", yattT[:, 4 * hk:4 * hk + 4, qc:qc + nq],
                     bf(banks[7][:, :])[:, 0:512].rearrange("p (g q) -> p g q", g=4)[:, :, 0:nq], r=[bk[7]], w=[yatt_t])

        qblock(23, 126, 2, 0, 0)
        for i in range(8):
            qblock(24 + i, 0, 128, 2 + 128 * i, 1 + i)
        P.barrier()
        A.release(m0)

    wo_top = (A.limit - 2 * 16384) // 64 * 64
    wo = [nc.alloc_sbuf_tensor_at("wo%d" % i, [128, 16, 512], BF16, offset=wo_top + i * 16384) for i in range(2)]
    wo_t = [Tk("wo0"), Tk("wo1")]

    def load_wo(obk):
        wb = obk % 2
        for c in range(8):
            P.dma("pool", wo[wb][:, c, :], wol_d[:, c, obk * 512:(obk + 1) * 512], w=[wo_t[wb]])
            P.dma("pool", wo[wb][:, 8 + c, :], woa_d[:, c, obk * 512:(obk + 1) * 512], w=[wo_t[wb]])

    load_wo(0)
    load_wo(1)
    run_attention()
    P.barrier()
    A.release(m_mixer)

    def run_ffn():
        gout = A.alloc("gout", [128, 2, 8], F32)
        gout_t = Tk("gout")
        P.dma("sp", gout[:, :, :], gout_d[:, :, :], w=[gout_t])
        gffn = A.alloc("gffn", [128, D], F32)
        gffn_t = Tk("gffn")
        P.dma("sp", gffn[:, :], gffn_d[:, :], w=[gffn_t])
        fcw = A.alloc("fcw", [128, 48, 3], F32)
        fcb = A.alloc("fcb", [128, 48], F32)
        fc_t = Tk("fc")
        P.dma("sp", fcw[:, :, :], fcw_d[:, :, :], w=[fc_t])
        P.dma("sp", fcb[:, :], fcb_d[:, :], w=[fc_t])
        h = A.alloc("h", [128, 8, D], F32)
        h_t = [Tk("h%d" % i) for i in range(8)]
        hh = A.alloc("hh", [2, D], F32)
        hh_t = Tk("hh")
        xnT = A.alloc("xnTf", [128, KC, NOWN], BF16)
        xnT_t = Tk("xnTf")
        m1 = A.mark()
        rs = A.alloc("rs", [128, 9, 8], F32)
        rs_t = Tk("rs")
        xnb = A.alloc("xnb", [128, D], BF16)
        xnb_t = Tk("xnb")
        ysq = [A.alloc("ysq", [128, 16, 128], BF16) for _ in range(2)]
        ysq_t = [Tk("ysq0"), Tk("ysq1")]
        assert A.off <= wo_top
        tiles = [(0, 2, hh[:, :], hh_t, 3070)] + [(2 + 128 * i, 128, h[:, i, :], h_t[i], 3072 + 128 * i) for i in range(8)]
        for ti, (c0, nt, hap, hapt, u0) in enumerate(tiles):
            P.dma("sp", hap, x_loc[u0:u0 + nt, :], w=[hapt])
            yb = ti % 2
            P.tt("pool", ysq[yb][:, 0:8, 0:nt], ylruT[:, :, c0:c0 + nt], ylruT[:, :, c0:c0 + nt], ALU.mult, r=ylru_t, w=[ysq_t[yb]])
            P.tt("dve", ysq[yb][:, 8:16, 0:nt], yattT[:, :, c0:c0 + nt], yattT[:, :, c0:c0 + nt], ALU.mult, r=[yatt_t], w=[ysq_t[yb]])
            for br in range(2):
                for c in range(8):
                    P.mm(banks[0][0:nt, br:br + 1], ysq[yb][:, br * 8 + c, 0:nt], ones_bf[:, 0:1], c == 0, c == 7,
                         r=[ysq_t[yb], ones_t], w=[bk[0]])
            P.act(rs[0:nt, ti, 0:2], banks[0][0:nt, 0:2], AF.Ln, r=[bk[0]], w=[rs_t], scale=1.0 / 1024, bias=epsb[0:nt, 0:1])
            P.act(rs[0:nt, ti, 2:4], rs[0:nt, ti, 0:2], AF.Exp, r=[rs_t], w=[rs_t], scale=-0.5)
        for c in range(8):
            P.ts("pool", ylruT[:, c, :], ylruT[:, c, :], gout[:, 0, c:c + 1], None, ALU.mult, r=[gout_t] + ysq_t, w=[ylru_t[c]])
            P.ts("dve", yattT[:, c, :], yattT[:, c, :], gout[:, 1, c:c + 1], None, ALU.mult, r=[gout_t] + ysq_t, w=[yatt_t])
        for obk in range(4):
            wb = obk % 2
            if obk >= 2:
                load_wo(obk)
            for ti, (c0, nt, hap, hapt, u0) in enumerate(tiles):
                for br in range(2):
                    pb = 2 + (ti % 2) * 2 + br
                    ysrc = ylruT if br == 0 else yattT
                    for c in range(8):
                        P.mm(banks[pb][0:nt, :], ysrc[:, c, c0:c0 + nt], wo[wb][:, br * 8 + c, :], c == 0, c == 7,
                             r=[ylru_t[c] if br == 0 else yatt_t, wo_t[wb]], w=[bk[pb]])
                    P.stt(hap[:, obk * 512:(obk + 1) * 512], banks[pb][0:nt, :], rs[0:nt, ti, 2 + br:3 + br],
                          hap[:, obk * 512:(obk + 1) * 512], ALU.mult, ALU.add, r=[bk[pb], rs_t, hapt], w=[hapt])
        for ti, (c0, nt, hap, hapt, u0) in enumerate(tiles):
            P.act(xnb[0:nt, :], hap, AF.Square, r=[hapt], w=[xnb_t, rs_t], accum_out=rs[0:nt, ti, 4:5])
            P.act(rs[0:nt, ti, 5:6], rs[0:nt, ti, 4:5], AF.Ln, r=[rs_t], w=[rs_t], scale=1.0 / D, bias=epsb[0:nt, 0:1])
            P.act(rs[0:nt, ti, 6:7], rs[0:nt, ti, 5:6], AF.Exp, r=[rs_t], w=[rs_t], scale=-0.5)
            P.stt(xnb[0:nt, :], hap, rs[0:nt, ti, 6:7], gffn[0:nt, :], ALU.mult, ALU.mult, r=[hapt, rs_t, gffn_t], w=[xnb_t])
            for half in range(2):
                for j in range(8):
                    jj = half * 8 + j
                    P.tr(bf(banks[6 + half][:, :])[:, j * 128:j * 128 + nt],
                         xnb[0:nt, jj * 128:(jj + 1) * 128], ident[0:nt, 0:nt], r=[xnb_t, ident_t], w=[bk[6 + half]])
                P.cp("dve" if half == 0 else "act", xnT[:, half * 8:(half + 1) * 8, c0:c0 + nt],
                     bf(banks[6 + half][:, :]).rearrange("p (j t) -> p j t", j=8)[:, :, 0:nt], r=[bk[6 + half]], w=[xnT_t])
        P.barrier()
        A.release(m1)
        wup = [nc.alloc_sbuf_tensor_at("wup%d" % i, [128, KC, 512], BF16, offset=y_base + i * 16384) for i in range(2)]
        assert y_base + 2 * 16384 <= y_end
        wup_t = [Tk("wup0"), Tk("wup1")]
        m2 = A.mark()
        wdn = [A.alloc("wdn", [128, 2, D], BF16) for _ in range(3)]
        wdn_t = [Tk("wdn0"), Tk("wdn1"), Tk("wdn2")]
        gT = [A.alloc("gT", [128, 2, 1024], BF16) for _ in range(2)]
        gT_t = [Tk("gT0"), Tk("gT1")]
        usb = [A.alloc("usb", [128, NOWN], F32) for _ in range(2)]
        usb_t = [Tk("usb0"), Tk("usb1")]
        vsb = [A.alloc("vsb", [128, NOWN], F32) for _ in range(2)]
        vsb_t = [Tk("vsb0"), Tk("vsb1")]
        uc = A.alloc("uc", [128, 1024], F32)
        uc_t = Tk("uc")
        gg = A.alloc("gg", [128, 1024], F32)
        gg_t = Tk("gg")
        CT = 342
        rotu = [0]
        def prefetch(G):
            P.dma("pool", wup[G % 2][:, :, :], wup_d[G, :, :, :], w=[wup_t[G % 2]])
            P.dma("pool", wdn[G % 3][:, :, :], wdn_d[G, :, :, :], w=[wdn_t[G % 3]])

        def up(G):
            gb = G % 2
            for fc in range(2):
                fa = 2 * G + fc
                ub = fa % 2
                for ct in range(3):
                    pu = rotu[0] % 2
                    rotu[0] += 1
                    bu, bv = pu * 2, pu * 2 + 1
                    for k in range(KC):
                        P.mm(banks[bu][:, 0:CT], wup[gb][:, k, fc * 128:(fc + 1) * 128], xnT[:, k, ct * CT:(ct + 1) * CT], k == 0, k == KC - 1,
                             r=[wup_t[gb], xnT_t], w=[bk[bu]])
                    for k in range(KC):
                        P.mm(banks[bv][:, 0:CT], wup[gb][:, k, 256 + fc * 128:256 + (fc + 1) * 128], xnT[:, k, ct * CT:(ct + 1) * CT], k == 0, k == KC - 1,
                             r=[wup_t[gb], xnT_t], w=[bk[bv]])
                    P.cp("act", usb[ub][:, ct * CT:(ct + 1) * CT], banks[bu][:, 0:CT], r=[bk[bu]], w=[usb_t[ub]])
                    P.cp("act", vsb[ub][:, ct * CT:(ct + 1) * CT], banks[bv][:, 0:CT], r=[bk[bv]], w=[vsb_t[ub]])
                P.ts("dve", uc[:, :], usb[ub][:, 2:1026], fcw[:, fa, 2:3], fcb[:, fa:fa + 1], ALU.mult, ALU.add, r=[usb_t[ub], fc_t], w=[uc_t])
                P.stt(uc[:, :], usb[ub][:, 1:1025], fcw[:, fa, 1:2], uc[:, :], ALU.mult, ALU.add, r=[usb_t[ub], fc_t, uc_t], w=[uc_t])
                P.stt(uc[:, :], usb[ub][:, 0:1024], fcw[:, fa, 0:1], uc[:, :], ALU.mult, ALU.add, r=[usb_t[ub], fc_t, uc_t], w=[uc_t])
                gelu(gg[:, :], uc[:, :], gg[:, :], [uc_t], gg_t, gg_t)
                P.tt("pool", gT[gb][:, fc, :], gg[:, :], vsb[ub][:, 2:1026], ALU.mult, r=[gg_t, vsb_t[ub]], w=[gT_t[gb]])

        def down(G):
            gb = G % 2
            wd = G % 3
            for tt in range(8):
                for obk in range(4):
                    pb = 4 + (tt * 4 + obk) % 4
                    for fc in range(2):
                        P.mm(banks[pb][:, :], gT[gb][:, fc, tt * 128:(tt + 1) * 128], wdn[wd][:, fc, obk * 512:(obk + 1) * 512], fc == 0, fc == 1,
                             r=[gT_t[gb], wdn_t[wd]], w=[bk[pb]])
                    P.tt("dve", h[:, tt, obk * 512:(obk + 1) * 512], banks[pb][:, :], h[:, tt, obk * 512:(obk + 1) * 512], ALU.add,
                         r=[bk[pb], h_t[tt]], w=[h_t[tt]])

        prefetch(0)
        for G in range(24):
            if G + 1 < 24:
                prefetch(G + 1)
            up(G)
            if G > 0:
                down(G - 1)
        down(23)
        P.barrier()
        A.release(m2)
        gfin = A.alloc("gfin", [128, D], F32)
        gfin_t = Tk("gfin")
        P.dma("sp", gfin[:, :], gfin_d[:, :], w=[gfin_t])
        fs = A.alloc("fs", [128, 8, 4], F32)
        fs_t = Tk("fs")
        xs2 = A.alloc("xs2", [128, D], BF16)
        xs2_t = Tk("xs2")
        for tt in range(8):
            P.act(xs2[:, :], h[:, tt, :], AF.Square, r=[h_t[tt]], w=[xs2_t, fs_t], accum_out=fs[:, tt, 0:1])
            P.act(fs[:, tt, 1:2], fs[:, tt, 0:1], AF.Ln, r=[fs_t], w=[fs_t], scale=1.0 / D, bias=epsb[:, 0:1])
            P.act(fs[:, tt, 2:3], fs[:, tt, 1:2], AF.Exp, r=[fs_t], w=[fs_t], scale=-0.5)
            P.stt(h[:, tt, :], h[:, tt, :], fs[:, tt, 2:3], gfin[:, :], ALU.mult, ALU.mult, r=[h_t[tt], fs_t, gfin_t], w=[h_t[tt]])
            P.dma("sp", y_out[tt * 128:(tt + 1) * 128, :], h[:, tt, :], r=[h_t[tt]], is_output=True)

    run_ffn()
    P.finish()
    return nc


def _tile_k(w):
    n = w.shape[1]
    return np.ascontiguousarray(w.reshape(KC, 128, n).transpose(1, 0, 2))


def _host_consts():
    c = {}
    c["ident"] = np.eye(128, dtype=np.float32).astype(ml_dtypes.bfloat16)
    eb = np.zeros((128, S), np.float32)
    for s in range(64):
        eb[s, s * 64:(s + 1) * 64] = 1.0
    c["ebig"] = eb.astype(ml_dtypes.bfloat16)
    n = np.arange(256)
    s = np.arange(64)
    ov = np.clip(np.minimum(n[:, None] * 16 + 32, s[None, :] * 64 + 64) - np.maximum(n[:, None] * 16, s[None, :] * 64), 0, None)
    ov = (ov / 32.0).astype(np.float32)
    ov[255] = 0.0
    c["ovl"] = np.ascontiguousarray(ov.reshape(2, 128, 64).transpose(1, 0, 2))

    def wide(hi):
        rel = np.arange(128)[None, :] - 64
        causal = (rel <= hi[:, None]).astype(np.float32)
        forced = ((rel <= hi[:, None]) & (rel > hi[:, None] - 2)).astype(np.float32)
        return np.ascontiguousarray(np.stack([causal, causal - 1.0, forced * 1e4], axis=1).astype(np.float32))

    c["cwide"] = wide((np.arange(128) >= 64).astype(np.int64))
    c["cwide_h"] = wide(np.array([1, 1], np.int64))
    return c


_NC_CACHE = {}


def kernel(**inp):
    f32 = np.float32
    g = {k: np.asarray(v, dtype=f32) for k, v in inp.items()}
    x = g["x"]
    w_in = g["w_in"][0]
    rep = {}
    rep["wA1"] = _tile_k(w_in[:, 0:1024])
    rep["wB3"] = _tile_k(w_in[:, 1024:2048])
    rep["wB2"] = _tile_k(w_in[:, 2048:3072])
    rep["wA2"] = _tile_k(np.concatenate([w_in[:, 3072:3584], w_in[:, 3584:3840], w_in[:, 3840:4096]], axis=1))
    rep["wB1"] = _tile_k(np.concatenate([w_in[:, 4096:4352], w_in[:, 4352:4608], w_in[:, 4608:4632]], axis=1))
    rep["gmixT"] = np.ascontiguousarray(g["g_mix"][0].reshape(KC, 128).T)
    rep["lcw"] = np.ascontiguousarray(g["lru_conv_w"][0].reshape(4, 8, 128).transpose(2, 1, 0))
    rep["lvec"] = np.ascontiguousarray(np.stack([g["lru_conv_b"][0], g["lru_ba"][0], g["lru_bx"][0], g["lru_lambda"][0]], 0)
                                       .reshape(4, 8, 128).transpose(2, 0, 1))
    rep["wa"] = np.ascontiguousarray(g["lru_wa"][0].transpose(1, 0, 2))
    rep["wx"] = np.ascontiguousarray(g["lru_wx"][0].transpose(1, 0, 2))
    rep["w1k"] = np.ascontiguousarray(g["cmp_w1_k"][0].reshape(32, 128, 128).transpose(1, 0, 2))
    rep["w1v"] = np.ascontiguousarray(g["cmp_w1_v"][0].reshape(32, 128, 128).transpose(1, 0, 2))
    rep["peT"] = np.ascontiguousarray(np.stack([g["cmp_pe_k"][0].T, g["cmp_pe_v"][0].T], axis=1))
    rep["cmpb"] = np.ascontiguousarray(np.stack([g["cmp_b1_k"][0], g["cmp_b1_v"][0]], axis=1))
    rep["w2k"] = np.ascontiguousarray(g["cmp_w2_k"][0])
    rep["w2v"] = np.ascontiguousarray(g["cmp_w2_v"][0])
    w_out = g["w_out"][0]
    rep["wol"] = np.ascontiguousarray(w_out[0:1024].reshape(8, 128, D).transpose(1, 0, 2))
    rep["woa"] = np.ascontiguousarray(w_out[1024:2048].reshape(8, 128, D).transpose(1, 0, 2))
    rep["gout"] = np.ascontiguousarray(np.stack([g["g_lru_out"][0].reshape(8, 128).T, g["g_attn_out"][0].reshape(8, 128).T], axis=1))
    rep["gffn_bc"] = np.ascontiguousarray(np.broadcast_to(g["g_ffn"][0][None, :], (128, D)))
    rep["gfin_bc"] = np.ascontiguousarray(np.broadcast_to(g["g_final"][None, :], (128, D)))
    w_up = g["w_up"][0]
    wu = w_up[:, 0:6144].reshape(KC, 128, 24, 256)
    wv = w_up[:, 6144:12288].reshape(KC, 128, 24, 256)
    rep["wup"] = np.ascontiguousarray(np.concatenate([wu, wv], axis=3).transpose(2, 1, 0, 3))
    rep["wdn"] = np.ascontiguousarray(g["w_down"][0].reshape(24, 2, 128, D).transpose(0, 2, 1, 3))
    rep["fcw"] = np.ascontiguousarray(g["ffn_conv_w"][0].reshape(3, 48, 128).transpose(2, 1, 0))
    rep["fcb"] = np.ascontiguousarray(g["ffn_conv_b"][0].reshape(48, 128).T)
    rep.update(_host_consts())

    in_maps = []
    for c in range(8):
        b, r = c // 4, c % 4
        pad = 1024 * (3 - r)
        xl = np.zeros((S, D), f32)
        xl[pad:] = x[b, 0:S - pad]
        valid = np.zeros((S,), f32)
        valid[pad:] = 1.0
        m = dict(rep)
        m["x_loc"] = xl
        m["stflag"] = np.ascontiguousarray(np.broadcast_to(valid[0:S:512][None, :], (128, 8)))
        m["validT"] = np.ascontiguousarray(valid.reshape(32, 128).T)
        vn = np.zeros((256,), f32)
        vn[pad // 16:255] = 1.0
        m["validn"] = np.ascontiguousarray(vn.reshape(2, 128).T)
        f0 = np.zeros((128, 64), f32)
        f0[:, pad // 64] = 1e4
        m["f0"] = f0
        in_maps.append(m)

    if "nc" not in _NC_CACHE:
        _NC_CACHE["nc"] = build_program()
    res = run_bass_kernel_spmd(_NC_CACHE["nc"], in_maps, core_ids=list(range(8)))
    out = np.zeros((2, S, D), f32)
    for c in range(8):
        b, r = c // 4, c % 4
        out[b, r * 1024:(r + 1) * 1024] = np.asarray(res.results[c]["y"], dtype=f32)
    return out
```

```python
import numpy as np
import ml_dtypes
import concourse.bass as bass
import concourse.mybir as mybir
from concourse.bass_utils import run_bass_kernel_spmd

F32 = mybir.dt.float32
BF16 = mybir.dt.bfloat16
AF = mybir.ActivationFunctionType
ALU = mybir.AluOpType

D = 2048
KC = 16
S = 4096
NOWN = 1026
EPS = 1e-6
NDMA = 12
USE_GELU_TANH_LUT = True


class Tk:
    __slots__ = ("name", "w", "r")

    def __init__(self, name):
        self.name = name
        self.w = None
        self.r = []


class Prog:
    def __init__(self, nc):
        self.nc = nc
        self.engs = ("pe", "act", "dve", "pool", "sp")
        self.sem = {k: nc.alloc_semaphore("sem_" + k) for k in self.engs}
        self.cnt = {k: 0 for k in self.engs}
        self.streams = {k: [] for k in self.engs}
        self.waited = {k: {} for k in self.engs}
        self.dq = {q: [[nc.alloc_semaphore("dq_%s_%d" % (q, i)), 0] for i in range(NDMA)] for q in ("sp", "pool")}
        self.dq_rr = {"sp": 0, "pool": 0}
        self.out_events = []

    def _wait(self, eng, evs):
        best = {}
        for ev in evs:
            if ev is None:
                continue
            key, sem, val = ev
            if key == eng and eng == "pe":
                continue
            if self.waited[eng].get(key, 0) >= val:
                continue
            if key not in best or best[key][1] < val:
                best[key] = (sem, val)
        for key, (sem, val) in best.items():
            self.waited[eng][key] = val
            self.streams[eng].append(lambda e, sem=sem, val=val: e.wait_ge(sem, val))

    def _deps(self, r, w):
        evs = []
        for t in r:
            evs.append(t.w)
        for t in w:
            evs.append(t.w)
            evs.extend(t.r)
        return evs

    def op(self, eng, fn, r=(), w=()):
        self._wait(eng, self._deps(r, w))
        self.cnt[eng] += 1
        sem = self.sem[eng]
        self.streams[eng].append(lambda e, fn=fn, sem=sem: fn(e).then_inc(sem, 1))
        ev = (eng, sem, self.cnt[eng])
        for t in r:
            t.r.append(ev)
        for t in w:
            t.w = ev
            t.r = []
        return ev

    def dma(self, q, out, in_, r=(), w=(), is_output=False):
        slot = self.dq[q][self.dq_rr[q] % NDMA]
        self.dq_rr[q] += 1
        sem = slot[0]
        key = "dma_" + str(id(slot))
        evs = self._deps(r, w)
        if slot[1] > 0:
            evs.append((key, sem, slot[1]))
        self._wait(q, evs)
        slot[1] += 16
        val = slot[1]
        self.streams[q].append(lambda e, out=out, in_=in_, sem=sem: e.dma_start(out=out, in_=in_).then_inc(sem, 16))
        ev = (key, sem, val)
        for t in r:
            t.r.append(ev)
        for t in w:
            t.w = ev
            t.r = []
        if is_output:
            self.out_events.append(ev)
        return ev

    def barrier(self):
        evs = [(k, self.sem[k], self.cnt[k]) for k in self.engs if self.cnt[k] > 0]
        for q in ("sp", "pool"):
            for slot in self.dq[q]:
                if slot[1] > 0:
                    evs.append(("dma_" + str(id(slot)), slot[0], slot[1]))
        for e in self.engs:
            self._wait(e, [ev for ev in evs if ev[0] != e])

    def finish(self):
        self.barrier()
        self._wait("sp", self.out_events)
        nc = self.nc
        st = self.streams
        with nc.Block() as block:
            @block.tensor
            def _(e):
                for f in st["pe"]:
                    f(e)

            @block.scalar
            def _(e):
                for f in st["act"]:
                    f(e)

            @block.vector
            def _(e):
                for f in st["dve"]:
                    f(e)

            @block.gpsimd
            def _(e):
                for f in st["pool"]:
                    f(e)

            @block.sync
            def _(e):
                for f in st["sp"]:
                    f(e)

    def act(self, out, in_, func, r=(), w=(), **kw):
        return self.op("act", lambda e: e.activation(out=out, in_=in_, func=func, **kw), r, w)

    def mm(self, out, lhsT, rhs, start, stop, r=(), w=()):
        return self.op("pe", lambda e: e.matmul(out, lhsT, rhs, start=start, stop=stop), r, w)

    def tr(self, out, in_, ident, r=(), w=()):
        return self.op("pe", lambda e: e.transpose(out, in_, ident), r, w)

    def tt(self, eng, out, in0, in1, op, r=(), w=()):
        return self.op(eng, lambda e: e.tensor_tensor(out=out, in0=in0, in1=in1, op=op), r, w)

    def ts(self, eng, out, in0, s1, s2, op0, op1=None, r=(), w=()):
        if op1 is None:
            return self.op(eng, lambda e: e.tensor_scalar(out=out, in0=in0, scalar1=s1, scalar2=None, op0=op0), r, w)
        return self.op(eng, lambda e: e.tensor_scalar(out=out, in0=in0, scalar1=s1, scalar2=s2, op0=op0, op1=op1), r, w)

    def stt(self, out, in0, scalar, in1, op0, op1, r=(), w=()):
        return self.op("dve", lambda e: e.scalar_tensor_tensor(out=out, in0=in0, scalar=scalar, in1=in1, op0=op0, op1=op1), r, w)

    def cp(self, eng, out, in_, r=(), w=()):
        if eng == "act":
            return self.act(out, in_, AF.Copy, r, w)
        return self.op(eng, lambda e: e.tensor_copy(out=out, in_=in_), r, w)


class Arena:
    def __init__(self, nc, base, limit):
        self.nc = nc
        self.off = base
        self.limit = limit
        self.n = 0

    def alloc(self, name, shape, dt):
        esz = 2 if dt == BF16 else 4
        nb = esz
        for s in shape[1:]:
            nb *= s
        nb = (nb + 31) // 32 * 32
        t = self.nc.alloc_sbuf_tensor_at("%s_%d" % (name, self.n), list(shape), dt, offset=self.off)
        self.n += 1
        self.off += nb
        assert self.off <= self.limit, ("SBUF overflow", name, self.off, self.limit)
        return t

    def mark(self):
        return self.off

    def release(self, m):
        self.off = m


def build_program():
    nc = bass.Bass("TRN2", target_bir_lowering=False)
    P = Prog(nc)

    def din(name, shape, dt=F32):
        return nc.dram_tensor(name, list(shape), dt, kind="ExternalInput").ap()

    x_loc = din("x_loc", [S, D])
    stflag_d = din("stflag", [128, 8])
    validT_d = din("validT", [128, 32])
    validn_d = din("validn", [128, 2])
    f0_d = din("f0", [128, 64])
    wA1_d = din("wA1", [128, KC, 1024])
    wB3_d = din("wB3", [128, KC, 1024])
    wA2_d = din("wA2", [128, KC, 1024])
    wB1_d = din("wB1", [128, KC, 536])
    wB2_d = din("wB2", [128, KC, 1024])
    gmixT_d = din("gmixT", [128, KC])
    lcw_d = din("lcw", [128, 8, 4])
    lvec_d = din("lvec", [128, 4, 8])
    wa_d = din("wa", [128, 8, 128])
    wx_d = din("wx", [128, 8, 128])
    w1k_d = din("w1k", [128, 32, 128])
    w1v_d = din("w1v", [128, 32, 128])
    pe_d = din("peT", [128, 2, 32])
    cmpb_d = din("cmpb", [128, 2])
    w2k_d = din("w2k", [128, 128])
    w2v_d = din("w2v", [128, 128])
    ident_d = din("ident", [128, 128], BF16)
    ebig_d = din("ebig", [128, S], BF16)
    ovl_d = din("ovl", [128, 2, 64])
    cwide_d = din("cwide", [128, 3, 128])
    cwide_h_d = din("cwide_h", [2, 3, 128])
    wol_d = din("wol", [128, 8, D])
    woa_d = din("woa", [128, 8, D])
    gout_d = din("gout", [128, 2, 8])
    gffn_d = din("gffn_bc", [128, D])
    gfin_d = din("gfin_bc", [128, D])
    wup_d = din("wup", [24, 128, KC, 512])
    wdn_d = din("wdn", [24, 128, 2, D])
    fcw_d = din("fcw", [128, 48, 3])
    fcb_d = din("fcb", [128, 48])
    y_out = nc.dram_tensor("y", [1024, D], F32, kind="ExternalOutput").ap()

    wsc = {}
    wsc_t = {}
    for nm, ncl in (("B3", 1024), ("A2", 1024), ("B1", 536), ("B2", 1024)):
        wsc[nm] = nc.dram_tensor("wsc_" + nm, [128, KC, ncl], BF16, kind="Internal").ap()
        wsc_t[nm] = [Tk("wsc_%s%d" % (nm, i)) for i in range(4)]
    wraw = {"B3": wB3_d, "A2": wA2_d, "B1": wB1_d, "B2": wB2_d}
    prep_chunks = []

    base = (int(nc.sbuf_base) + 63) // 64 * 64
    A = Arena(nc, base, int(nc.sbuf_base) + int(nc.sbuf_bytes_remaining) - 64)
    banks = [nc.alloc_psum_tensor("bank%d" % i, [128, 512], F32) for i in range(8)]
    bk = [Tk("bank%d" % i) for i in range(8)]

    def bf(bank_ap):
        return bank_ap.bitcast(BF16)

    ident = A.alloc("ident", [128, 128], BF16)
    ident_t = Tk("ident")
    P.dma("sp", ident[:, :], ident_d[:, :], w=[ident_t])
    gmixT = A.alloc("gmixT", [128, KC], F32)
    gmixT_t = Tk("gmixT")
    P.dma("sp", gmixT[:, :], gmixT_d[:, :], w=[gmixT_t])
    ones_bf = A.alloc("ones", [128, 2], BF16)
    ones_t = Tk("ones")
    P.op("dve", lambda e: e.memset(ones_bf[:, :], 1.0), w=[ones_t])

    epsb = A.alloc("epsb", [128, 1], F32)
    epsb_t = Tk("epsb")
    P.op("dve", lambda e: e.memset(epsb[:, :], EPS), w=[epsb_t])
    oneb = A.alloc("oneb", [128, 1], F32)
    oneb_t = Tk("oneb")
    P.op("dve", lambda e: e.memset(oneb[:, :], 1.0), w=[oneb_t])
    y_base = A.mark()
    ylruT = A.alloc("ylruT", [128, 8, NOWN], BF16)
    ylru_t = [Tk("ylru%d" % c) for c in range(8)]
    yattT = A.alloc("yattT", [128, 8, NOWN], BF16)
    yatt_t = Tk("yatt")
    y_end = A.mark()

    m_mixer = A.mark()

    def proj_pass(w_dram, ncols, sts, groups, nrot=6, scratch=None):
        m0 = A.mark()
        W = A.alloc("W", [128, KC, ncols], BF16)
        W_q = [Tk("Wq%d" % i) for i in range(4)]
        W_t = [W_q[k // 4] for k in range(KC)] if scratch is not None else [Tk("W%d" % k) for k in range(KC)]
        xt = [A.alloc("xt", [128, D], F32) for _ in range(2)]
        xt_t = [Tk("xt0"), Tk("xt1")]
        if scratch is not None:
            for q4 in range(4):
                P.dma("sp", W[:, 4 * q4:4 * q4 + 4, :], wsc[scratch][:, 4 * q4:4 * q4 + 4, :], r=[wsc_t[scratch][q4]], w=[W_q[q4]])
        else:
            slots = [xt[0][:, 0:1024], xt[0][:, 1024:2048], xt[1][:, 0:1024], xt[1][:, 1024:2048]]
            slot_t = [Tk("wslot%d" % i) for i in range(4)]
            for k in range(KC):
                i4 = k % 4
                P.dma("sp", slots[i4][:, 0:ncols], w_dram[:, k, :], w=[slot_t[i4]])
                P.ts("dve" if k % 2 == 0 else "pool", W[:, k, :], slots[i4][:, 0:ncols], gmixT[:, k:k + 1], None, ALU.mult,
                     r=[slot_t[i4], gmixT_t], w=[W_t[k]])
            for i4 in range(4):
                xt_t[i4 // 2].r.extend(slot_t[i4].r)
        xn = [A.alloc("xn", [128, D], BF16) for _ in range(2)]
        xn_t = [Tk("xn0"), Tk("xn1")]
        ss = [A.alloc("ss", [128, 4], F32) for _ in range(2)]
        ss_t = [Tk("ss0"), Tk("ss1")]
        xnT = [A.alloc("xnT", [128, KC, 512], BF16) for _ in range(2)]
        xnT_t = [Tk("xnT0"), Tk("xnT1")]
        ctx = dict(W=W, W_t=W_t)
        for g in groups:
            if "setup" in g:
                g["setup"](ctx)
        rot = [2]

        def prep_tile(si, tt):
            tk = sts[si] * 4 + tt
            b = tk % 2
            P.dma("sp", xt[b][:, :], x_loc[tk * 128:(tk + 1) * 128, :], w=[xt_t[b]])
            P.act(xn[b][:, :], xt[b][:, :], AF.Square, r=[xt_t[b]], w=[xn_t[b], ss_t[b]], accum_out=ss[b][:, 0:1])
            P.act(ss[b][:, 1:2], ss[b][:, 0:1], AF.Ln, r=[ss_t[b]], w=[ss_t[b]], scale=1.0 / D, bias=epsb[:, 0:1])
            P.act(ss[b][:, 2:3], ss[b][:, 1:2], AF.Exp, r=[ss_t[b]], w=[ss_t[b]], scale=-0.5)
            P.act(xn[b][:, :], xt[b][:, :], AF.Copy, r=[xt_t[b], ss_t[b]], w=[xn_t[b]], scale=ss[b][:, 2:3])

        def prep_tr(si, tt):
            tk = sts[si] * 4 + tt
            b = tk % 2
            xb = si % 2
            for half in range(2):
                pb = banks[half]
                for j in range(8):
                    jj = half * 8 + j
                    P.tr(bf(pb[:, :])[:, j * 128:(j + 1) * 128], xn[b][:, jj * 128:(jj + 1) * 128], ident[:, :],
                         r=[xn_t[b], ident_t], w=[bk[half]])
                P.cp("dve" if half == 0 else "act",
                     xnT[xb][:, half * 8:(half + 1) * 8, tt * 128:(tt + 1) * 128],
                     bf(pb[:, :]).rearrange("p (j t) -> p j t", j=8),
                     r=[bk[half]], w=[xnT_t[xb]])

        def make_units(si):
            st = sts[si]
            xb = si % 2
            units = []
            for g in groups:
                if st not in g.get("sts", sts):
                    continue
                if g["kind"] == "fm":
                    for j in range(g["ncols"] // 128):
                        def mmf(g=g, j=j):
                            bi = rot[0]
                            rot[0] = 2 + (rot[0] - 1) % nrot
                            for k in range(KC):
                                P.mm(banks[bi][:, :], W[:, k, g["col0"] + j * 128: g["col0"] + (j + 1) * 128], xnT[xb][:, k, :],
                                     k == 0, k == KC - 1, r=[W_t[k], xnT_t[xb]], w=[bk[bi]])
                            return bi
                        units.append((mmf, lambda bi, g=g, j=j: g["evac"](st, j, banks[bi], bk[bi])))
                else:
                    nco = g["ncols"]
                    for tt in range(4):
                        def mmf(g=g, tt=tt, nco=nco):
                            bi = rot[0]
                            rot[0] = 2 + (rot[0] - 1) % nrot
                            for k in range(KC):
                                P.mm(banks[bi][:, 0:nco], xnT[xb][:, k, tt * 128:(tt + 1) * 128], W[:, k, g["col0"]: g["col0"] + nco],
                                     k == 0, k == KC - 1, r=[W_t[k], xnT_t[xb]], w=[bk[bi]])
                            return bi
                        units.append((mmf, lambda bi, g=g, tt=tt: g["evac"](st, tt, banks[bi], bk[bi])))
            return units

        for tt in range(4):
            prep_tile(0, tt)
            prep_tr(0, tt)
        for si in range(len(sts)):
            units = make_units(si)
            n = len(units)
            sched = {}
            if si + 1 < len(sts):
                for k in range(4):
                    u_tile = (k * n) // 4
                    sched.setdefault(u_tile, []).append(("tile", k))
                    u_tr = min(n - 1, u_tile + max(1, n // 8))
                    sched.setdefault(u_tr, []).append(("tr", k))
            pending = None
            for ui, (mmf, evf) in enumerate(units):
                for kind, k in sched.get(ui, []):
                    if kind == "tile":
                        prep_tile(si + 1, k)
                    else:
                        prep_tr(si + 1, k)
                bi = mmf()
                if pending is not None:
                    pending[0](pending[1])
                pending = (evf, bi)
            pending[0](pending[1])
        for g in groups:
            if "flush" in g:
                g["flush"]()
        P.barrier()
        A.release(m0)


    def run_lru():
        m0 = A.mark()
        h_own = A.alloc("h_own", [128, 8, NOWN], F32)
        h_own_t = [Tk("h_own%d" % c) for c in range(8)]
        m_l = A.mark()
        lcw = A.alloc("lcw", [128, 8, 4], F32)
        lvec = A.alloc("lvec", [128, 4, 8], F32)
        c12 = A.alloc("c12", [128, 2, 8], F32)
        lsm_t = Tk("lsm")
        P.dma("sp", lcw[:, :, :], lcw_d[:, :, :], w=[lsm_t])
        P.dma("sp", lvec[:, :, :], lvec_d[:, :, :], w=[lsm_t])
        P.act(c12[:, 0, :], lvec[:, 3, :], AF.Exp, r=[lsm_t], w=[lsm_t], scale=-1.0)
        P.act(c12[:, 0, :], c12[:, 0, :], AF.Ln, r=[lsm_t], w=[lsm_t], bias=oneb[:, 0:1])
        P.ts("dve", c12[:, 1, :], c12[:, 0, :], -16.0, None, ALU.mult, r=[lsm_t], w=[lsm_t])
        P.ts("dve", c12[:, 0, :], c12[:, 0, :], -8.0, None, ALU.mult, r=[lsm_t], w=[lsm_t])
        wa = A.alloc("wa", [128, 8, 128], F32)
        wx = A.alloc("wx", [128, 8, 128], F32)
        wax_t = Tk("wax")
        P.dma("sp", wa[:, :, :], wa_d[:, :, :], w=[wax_t])
        P.dma("sp", wx[:, :, :], wx_d[:, :, :], w=[wax_t])
        stf = A.alloc("stf", [128, 8], F32)
        stf_t = Tk("stf")
        P.dma("sp", stf[:, :], stflag_d[:, :], w=[stf_t])
        hin = A.alloc("hin", [128, 8], F32)
        hin_t = [Tk("hin%d" % c) for c in range(8)]
        xbuf = A.alloc("xcarry", [128, 8, 6], F32)
        xbuf_t = [Tk("xcarry%d" % c) for c in range(8)]
        hst = A.alloc("hst", [128, 8], F32)
        hst_t = [Tk("hst%d" % c) for c in range(8)]
        for c in range(8):
            P.op("pool", lambda e, c=c: e.memset(xbuf[:, c, 0:3], 0.0), w=[xbuf_t[c]])
        NB = 3
        tmp = {}
        tmp_t = {}
        for nm in ("u", "r", "i", "a", "h"):
            tmp[nm] = [A.alloc("l" + nm, [128, 512], F32) for _ in range(NB)]
            tmp_t[nm] = [Tk("l%s%d" % (nm, b)) for b in range(NB)]
        tmp["s"], tmp_t["s"] = tmp["r"], tmp_t["r"]
        tmp["v"], tmp_t["v"] = tmp["i"], tmp_t["i"]
        NPF = 3
        pf = [A.alloc("pf", [128, 512], F32) for _ in range(NPF)]
        pf_t = [Tk("pf%d" % i) for i in range(NPF)]
        pb_ = [A.alloc("pb", [128, 512], BF16) for _ in range(NPF)]
        pb_t = [Tk("pb%d" % i) for i in range(NPF)]
        chunk_list = []
        for nm_ in ("B3", "A2", "B1", "B2"):
            ncl = wraw[nm_].shape[2]
            for k in range(KC):
                for c0 in range(0, ncl, 512):
                    chunk_list.append((nm_, k, c0, min(ncl, c0 + 512)))
        pstate = [0]

        def prep_step():
            j = pstate[0]
            pstate[0] += 1
            if 0 <= j - 2 < len(chunk_list):
                nm_, k, c0, c1 = chunk_list[j - 2]
                i3 = (j - 2) % NPF
                P.dma("pool", wsc[nm_][:, k, c0:c1], pb_[i3][:, 0:c1 - c0], r=[pb_t[i3]], w=[wsc_t[nm_][k // 4]])
            if 0 <= j - 1 < len(chunk_list):
                nm_, k, c0, c1 = chunk_list[j - 1]
                i3 = (j - 1) % NPF
                P.ts("dve", pb_[i3][:, 0:c1 - c0], pf[i3][:, 0:c1 - c0], gmixT[:, k:k + 1], None, ALU.mult, r=[pf_t[i3], gmixT_t], w=[pb_t[i3]])
            if j < len(chunk_list):
                nm_, k, c0, c1 = chunk_list[j]
                i3 = j % NPF
                P.dma("sp", pf[i3][:, 0:c1 - c0], wraw[nm_][:, k, c0:c1], w=[pf_t[i3]])
            return j - 2 < len(chunk_list)

        prep_chunks.append(prep_step)

        pendB = [None]
        uctr = [0]

        def stageB(st, c, b, gp):
            u = tmp["u"][b]
            ut = tmp_t["u"][b]
            rr, ii, aa, sq, vv = tmp["r"][b], tmp["i"][b], tmp["a"][b], tmp["s"][b], tmp["v"][b]
            P.act(rr[:, :], banks[gp[0]][:, :], AF.Sigmoid, r=[bk[gp[0]], lsm_t], w=[tmp_t["r"][b]], bias=lvec[:, 1, c:c + 1])
            P.act(ii[:, :], banks[gp[1]][:, :], AF.Sigmoid, r=[bk[gp[1]], lsm_t], w=[tmp_t["i"][b]], bias=lvec[:, 2, c:c + 1])
            P.act(aa[:, :], rr[:, :], AF.Exp, r=[tmp_t["r"][b], lsm_t], w=[tmp_t["a"][b]], scale=c12[:, 0, c:c + 1])
            P.act(sq[:, :], rr[:, :], AF.Exp, r=[tmp_t["r"][b], lsm_t], w=[tmp_t["s"][b]], scale=c12[:, 1, c:c + 1])
            P.act(sq[:, :], sq[:, :], AF.Ln, r=[tmp_t["s"][b], oneb_t], w=[tmp_t["s"][b]], scale=-1.0, bias=oneb[:, 0:1])
            P.act(sq[:, :], sq[:, :], AF.Exp, r=[tmp_t["s"][b]], w=[tmp_t["s"][b]], scale=0.5)
            P.tt("dve", vv[:, :], ii[:, :], u[:, :], ALU.mult, r=[tmp_t["i"][b], ut], w=[tmp_t["v"][b]])
            P.tt("dve", vv[:, :], vv[:, :], sq[:, :], ALU.mult, r=[tmp_t["s"][b], tmp_t["v"][b]], w=[tmp_t["v"][b]])
            if st >= 6:
                hout = h_own[:, c, 2 + (st - 6) * 512: 2 + (st - 5) * 512]
                ht = h_own_t[c]
            else:
                hout = tmp["h"][b][:, :]
                ht = tmp_t["h"][b]
            if st == 0:
                init = 0.0
            else:
                P.tt("dve", hin[:, c:c + 1], hst[:, c:c + 1], stf[:, st - 1:st], ALU.mult, r=[hst_t[c], stf_t], w=[hin_t[c]])
                init = hin[:, c:c + 1]
            P.op("dve", lambda e, hout=hout, aa=aa, vv=vv, init=init: e.tensor_tensor_scan(
                out=hout, data0=aa[:, :], data1=vv[:, :], initial=init, op0=ALU.mult, op1=ALU.add),
                r=[tmp_t["a"][b], tmp_t["v"][b], hin_t[c]], w=[ht])
            P.cp("dve", hst[:, c:c + 1], hout[:, 511:512], r=[ht], w=[hst_t[c]])
            if st == 5:
                P.cp("dve", h_own[:, c, 0:2], hout[:, 510:512], r=[ht], w=[h_own_t[c]])

        def evac(st, c, ps, ps_t):
            b = uctr[0] % NB
            gp = [4, 5] if uctr[0] % 2 == 0 else [6, 7]
            uctr[0] += 1
            for _ in range(3):
                if prep_chunks and not prep_chunks[0]():
                    prep_chunks.pop(0)
            u = tmp["u"][b]
            ut = tmp_t["u"][b]
            P.cp("dve", xbuf[:, c, 3:6], ps[:, 0:3], r=[ps_t], w=[xbuf_t[c]])
            P.ts("dve", u[:, 3:512], ps[:, 3:512], lcw[:, c, 3:4], lvec[:, 0, c:c + 1], ALU.mult, ALU.add,
                 r=[ps_t, lsm_t], w=[ut])
            for j in range(3):
                P.stt(u[:, 3:512], ps[:, j:j + 509], lcw[:, c, j:j + 1], u[:, 3:512], ALU.mult, ALU.add,
                      r=[ps_t, lsm_t, ut], w=[ut])
            P.ts("dve", u[:, 0:3], xbuf[:, c, 3:6], lcw[:, c, 3:4], lvec[:, 0, c:c + 1], ALU.mult, ALU.add,
                 r=[xbuf_t[c], lsm_t], w=[ut])
            for j in range(3):
                P.stt(u[:, 0:3], xbuf[:, c, j:j + 3], lcw[:, c, j:j + 1], u[:, 0:3], ALU.mult, ALU.add,
                      r=[xbuf_t[c], lsm_t, ut], w=[ut])
            P.cp("dve", xbuf[:, c, 0:3], ps[:, 509:512], r=[ps_t], w=[xbuf_t[c]])
            P.mm(banks[gp[0]][:, :], wa[:, c, :], u[:, :], True, True, r=[wax_t, ut], w=[bk[gp[0]]])
            P.mm(banks[gp[1]][:, :], wx[:, c, :], u[:, :], True, True, r=[wax_t, ut], w=[bk[gp[1]]])
            prev = pendB[0]
            pendB[0] = (st, c, b, gp)
            if prev is not None:
                stageB(*prev)

        def flush_lru():
            if pendB[0] is not None:
                stageB(*pendB[0])
                pendB[0] = None

        proj_pass(wA1_d, 1024, list(range(8)), [dict(kind="fm", col0=0, ncols=1024, evac=evac, flush=flush_lru)], nrot=2)
        assert not prep_chunks
        A.release(m_l)

        gt = [A.alloc("gt", [128, 512], F32) for _ in range(2)]
        gt_t = [Tk("gt0"), Tk("gt1")]
        gt2 = [A.alloc("gt2", [128, 512], F32) for _ in range(2)]
        gt2_t = [Tk("gt20"), Tk("gt21")]

        def evac_gate(st, c, ps, ps_t):
            b = c % 2
            if st == 5:
                lo, hi, o0 = 510, 512, 0
            else:
                lo, hi, o0 = 0, 512, 2 + (st - 6) * 512
            n = hi - lo
            gelu(gt[b][:, 0:n], ps[:, lo:hi], gt2[b][:, 0:n], [ps_t], gt_t[b], gt2_t[b])
            P.tt("dve", ylruT[:, c, o0:o0 + n], gt[b][:, 0:n], h_own[:, c, o0:o0 + n], ALU.mult,
                 r=[gt_t[b], h_own_t[c]], w=[ylru_t[c]])

        proj_pass(wB3_d, 1024, [5, 6, 7], [dict(kind="fm", col0=0, ncols=1024, evac=evac_gate)], scratch="B3")
        A.release(m0)

    def gelu(out, in_, scratch, in_t, out_t, scratch_t):
        if USE_GELU_TANH_LUT:
            P.act(out, in_, AF.Gelu_apprx_tanh, r=in_t, w=[out_t])
            return
        P.act(scratch, in_, AF.Square, r=in_t, w=[scratch_t])
        P.ts("dve", scratch, scratch, 0.044715, 1.0, ALU.mult, ALU.add, r=[scratch_t], w=[scratch_t])
        P.tt("dve", scratch, scratch, in_, ALU.mult, r=[scratch_t] + list(in_t), w=[scratch_t])
        P.act(scratch, scratch, AF.Sigmoid, r=[scratch_t], w=[scratch_t], scale=1.5957691216057308)
        P.tt("dve", out, scratch, in_, ALU.mult, r=[scratch_t] + list(in_t), w=[out_t])


    run_lru()

    kselT = A.alloc("kselT", [128, 2, S], BF16)
    ksel_t = Tk("kselT")
    vsel = A.alloc("vsel", [128, 32, 2, 129], BF16)
    vsel_t = Tk("vsel")
    kcmpT = A.alloc("kcmpT", [128, 2, 256], BF16)
    kcmp_t = Tk("kcmpT")
    Rc = A.alloc("Rc", [128, 2, 2, 193], BF16)
    Rc_t = Tk("Rc")
    validT = A.alloc("validT", [128, 32], F32)
    validT_t = Tk("validT")
    P.dma("sp", validT[:, :], validT_d[:, :], w=[validT_t])
    P.op("dve", lambda e: e.tensor_copy(out=vsel[:, :, 0, 128:129], in_=validT[:, :].unsqueeze(2)), r=[validT_t], w=[vsel_t])
    P.op("dve", lambda e: e.tensor_copy(out=vsel[:, :, 1, 128:129], in_=validT[:, :].unsqueeze(2)), r=[validT_t], w=[vsel_t])
    m_a2 = A.mark()
    kvcT = A.alloc("kvcT", [128, 4, S], BF16)
    kvc_t = Tk("kvcT")

    def evac_a2_fm(st, j, ps, ps_t):
        eng = "act" if j % 2 == 0 else "dve"
        if j < 4:
            P.cp(eng, kvcT[:, j, st * 512:(st + 1) * 512], ps[:, :], r=[ps_t], w=[kvc_t])
        else:
            P.cp(eng, kselT[:, j - 4, st * 512:(st + 1) * 512], ps[:, :], r=[ps_t], w=[ksel_t])

    def evac_a2_v(st, tt, ps, ps_t):
        ch = st * 4 + tt
        P.cp("act" if tt % 2 else "dve", vsel[:, ch, :, 0:128], ps[:, 0:256].rearrange("p (h d) -> p h d", h=2),
             r=[ps_t], w=[vsel_t])

    proj_pass(wA2_d, 1024, list(range(8)),
              [dict(kind="fm", col0=0, ncols=768, evac=evac_a2_fm),
               dict(kind="tm", col0=768, ncols=256, evac=evac_a2_v)], scratch="A2")

    def run_compress():
        m0 = A.mark()
        w1 = [A.alloc("w1", [128, 32, 128], BF16) for _ in range(2)]
        w1_t = [Tk("w1k"), Tk("w1v")]
        P.dma("pool", w1[0][:, :, :], w1k_d[:, :, :], w=[w1_t[0]])
        P.dma("pool", w1[1][:, :, :], w1v_d[:, :, :], w=[w1_t[1]])
        w2 = A.alloc("w2", [128, 2, 128], BF16)
        w2_t = Tk("w2")
        P.dma("pool", w2[:, 0, :], w2k_d[:, :], w=[w2_t])
        P.dma("pool", w2[:, 1, :], w2v_d[:, :], w=[w2_t])
        peT = A.alloc("peT", [128, 2, 32], BF16)
        peT_t = Tk("peT")
        P.dma("pool", peT[:, :, :], pe_d[:, :, :], w=[peT_t])
        cb = A.alloc("cb", [128, 4], F32)
        cb_t = Tk("cb")
        P.dma("sp", cb[:, 0:2], cmpb_d[:, :], w=[cb_t])
        vn = A.alloc("vn", [128, 2], F32)
        ovl = A.alloc("ovl", [128, 2, 64], F32)
        vn_t = Tk("vn")
        P.dma("sp", vn[:, :], validn_d[:, :], w=[vn_t])
        P.dma("sp", ovl[:, :, :], ovl_d[:, :, :], w=[vn_t])
        for ty in range(2):
            for l in range(32):
                P.mm(banks[2][:, ty:ty + 1], w1[ty][:, l, :], peT[:, ty, l:l + 1], l == 0, l == 31,
                     r=[w1_t[ty], peT_t], w=[bk[2]])
            P.tt("dve", cb[:, 2 + ty:3 + ty], banks[2][:, ty:ty + 1], cb[:, ty:ty + 1], ALU.add, r=[bk[2], cb_t], w=[cb_t])
        hid = [A.alloc("hid", [128, 256], F32) for _ in range(2)]
        hid_t = [Tk("hid0"), Tk("hid1")]
        hs = [A.alloc("hs", [128, 256], F32) for _ in range(2)]
        hs_t = [Tk("hs0"), Tk("hs1")]
        hp = [A.alloc("hp", [128, 256], F32) for _ in range(2)]
        hp_t = [Tk("hp0"), Tk("hp1")]
        hb = [A.alloc("hb", [128, 256], BF16) for _ in range(2)]
        hb_t = [Tk("hb0"), Tk("hb1")]
        for c2 in range(2):
            for hk in range(2):
                P.cp("dve", Rc[:, c2, hk, 0:1], vn[:, c2:c2 + 1], r=[vn_t], w=[Rc_t])
                P.ts("dve", Rc[:, c2, hk, 1:65], ovl[:, c2, :], vn[:, c2:c2 + 1], None, ALU.mult, r=[vn_t], w=[Rc_t])
        it = 0
        for hk in range(2):
            for ty in range(2):
                b = it % 2
                it += 1
                pb = 3 + b
                for l in range(32):
                    P.mm(banks[pb][:, 0:255], w1[ty][:, l, :], kvcT[:, ty * 2 + hk, l: l + 16 * 254 + 1: 16], l == 0, l == 31,
                         r=[w1_t[ty], kvc_t], w=[bk[pb]])
                P.ts("dve", hp[b][:, 0:255], banks[pb][:, 0:255], cb[:, 2 + ty:3 + ty], None, ALU.add, r=[bk[pb], cb_t], w=[hp_t[b]])
                gelu(hid[b][:, 0:255], hp[b][:, 0:255], hs[b][:, 0:255], [hp_t[b]], hid_t[b], hs_t[b])
                P.cp("dve", hb[b][:, 0:255], hid[b][:, 0:255], r=[hid_t[b]], w=[hb_t[b]])
                if ty == 0:
                    P.mm(banks[5][:, 0:255], w2[:, 0, :], hb[b][:, 0:255], True, True, r=[w2_t, hb_t[b]], w=[bk[5]])
                    P.cp("act", kcmpT[:, hk, 0:255], banks[5][:, 0:255], r=[bk[5]], w=[kcmp_t])
                else:
                    for c2 in range(2):
                        rows = 128 if c2 == 0 else 127
                        P.mm(banks[6 + c2][0:rows, 0:128], hb[b][:, c2 * 128: c2 * 128 + rows], w2[:, 1, :], True, True,
                             r=[w2_t, hb_t[b]], w=[bk[6 + c2]])
                        P.ts("dve", Rc[0:rows, c2, hk, 65:193], banks[6 + c2][0:rows, 0:128], vn[0:rows, c2:c2 + 1], None, ALU.mult,
                             r=[bk[6 + c2], vn_t], w=[Rc_t])
        P.barrier()
        A.release(m0)

    run_compress()
    A.release(m_a2)

    kwT = A.alloc("kwT", [128, 2, 2048], BF16)
    kw_t = Tk("kwT")
    vwin = A.alloc("vwin", [128, 16, 2, 129], BF16)
    vwin_t = Tk("vwin")
    gsig = A.alloc("gsig", [128, 9, 24], F32)
    gsig_t = Tk("gsig")
    qT = A.alloc("qT", [128, 8, NOWN], BF16)
    qT_t = Tk("qT")
    P.op("dve", lambda e: e.tensor_copy(out=vwin[:, :, 0, 128:129], in_=validT[:, 16:32].unsqueeze(2)), r=[validT_t], w=[vwin_t])
    P.op("dve", lambda e: e.tensor_copy(out=vwin[:, :, 1, 128:129], in_=validT[:, 16:32].unsqueeze(2)), r=[validT_t], w=[vwin_t])
    def evac_b1_k(st, j, ps, ps_t):
        P.cp("act" if j else "dve", kwT[:, j, (st - 4) * 512:(st - 3) * 512], ps[:, :], r=[ps_t], w=[kw_t])

    def evac_b1_v(st, tt, ps, ps_t):
        ch = (st - 4) * 4 + tt
        P.cp("dve", vwin[:, ch, :, 0:128], ps[:, 0:256].rearrange("p (h d) -> p h d", h=2), r=[ps_t], w=[vwin_t])
        if st >= 6:
            ti = 1 + (st - 6) * 4 + tt
            P.act(gsig[:, ti, :], ps[:, 256:280], AF.Sigmoid, r=[ps_t], w=[gsig_t])

    halo_ctx = {}

    proj_pass(wB1_d, 536, [4, 5, 6, 7],
              [dict(kind="fm", col0=0, ncols=256, evac=evac_b1_k),
               dict(kind="tm", col0=256, ncols=280, evac=evac_b1_v)], scratch="B1")

    def setup_b2(ctx):
        halo_ctx.update(ctx)

    def evac_b2(st, j, ps, ps_t):
        if st == 5:
            lo, hi, o0 = 510, 512, 0
        else:
            lo, hi, o0 = 0, 512, 2 + (st - 6) * 512
        P.act(qT[:, j, o0:o0 + hi - lo], ps[:, lo:hi], AF.Copy, r=[ps_t], w=[qT_t], scale=float(128 ** -0.5))

    proj_pass(wB2_d, 1024, [5, 6, 7], [dict(kind="fm", col0=0, ncols=1024, evac=evac_b2)], scratch="B2")

    def halo_gates():
        m0 = A.mark()
        Wg = A.alloc("Wg", [128, KC, 24], BF16)
        Wg_t = Tk("Wg")
        sg = A.alloc("sg", [128, KC, 24], F32)
        sg_t = Tk("sg")
        P.dma("sp", sg[:, :, :], wB1_d[:, :, 512:536], w=[sg_t])
        for k in range(KC):
            P.ts("dve", Wg[:, k, :], sg[:, k, :], gmixT[:, k:k + 1], None, ALU.mult, r=[sg_t, gmixT_t], w=[Wg_t])
        xh = A.alloc("xh", [2, D], F32)
        xh_t = Tk("xh")
        P.dma("sp", xh[:, :], x_loc[3070:3072, :], w=[xh_t])
        xq = A.alloc("xq", [2, D], BF16)
        xq_t = Tk("xq")
        sh = A.alloc("sh", [2, 4], F32)
        sh_t = Tk("sh")
        P.act(xq[:, :], xh[:, :], AF.Square, r=[xh_t], w=[xq_t, sh_t], accum_out=sh[:, 0:1])
        P.act(sh[:, 1:2], sh[:, 0:1], AF.Ln, r=[sh_t], w=[sh_t], scale=1.0 / D, bias=epsb[0:2, 0:1])
        P.act(sh[:, 2:3], sh[:, 1:2], AF.Exp, r=[sh_t], w=[sh_t], scale=-0.5)
        P.act(xq[:, :], xh[:, :], AF.Copy, r=[xh_t, sh_t], w=[xq_t], scale=sh[:, 2:3])
        xqT = A.alloc("xqT", [128, KC, 2], BF16)
        xqT_t = Tk("xqT")
        for j in range(KC):
            P.tr(bf(banks[0][:, :])[:, j * 2:(j + 1) * 2], xq[:, j * 128:(j + 1) * 128], ident[0:2, 0:2], r=[xq_t, ident_t], w=[bk[0]])
        P.cp("dve", xqT[:, :, :], bf(banks[0][:, :])[:, 0:32].rearrange("p (j t) -> p j t", j=KC), r=[bk[0]], w=[xqT_t])
        for k in range(KC):
            P.mm(banks[2][0:2, 0:24], xqT[:, k, :], Wg[:, k, :], k == 0, k == KC - 1, r=[xqT_t, Wg_t], w=[bk[2]])
        P.act(gsig[0:2, 0, :], banks[2][0:2, 0:24], AF.Sigmoid, r=[bk[2]], w=[gsig_t])
        P.barrier()
        A.release(m0)

    halo_gates()

    def run_attention():
        m0 = A.mark()
        ebig = A.alloc("ebig", [128, S], BF16)
        cw = A.alloc("cw", [128, 3, 128], F32)
        cwh = A.alloc("cwh", [2, 3, 128], F32)
        f0 = A.alloc("f0", [128, 64], F32)
        cst_t = Tk("attn_consts")
        P.dma("sp", ebig[:, :], ebig_d[:, :], w=[cst_t])
        P.dma("sp", cw[:, :, :], cwide_d[:, :, :], w=[cst_t])
        P.dma("sp", cwh[:, :, :], cwide_h_d[:, :, :], w=[cst_t])
        P.dma("sp", f0[:, :], f0_d[:, :], w=[cst_t])
        NE = 4
        Pt = [A.alloc("Pm", [128, 4, 128], BF16) for _ in range(NE)]
        Pt_t = [Tk("Pm%d" % i) for i in range(NE)]
        Ec = [[A.alloc("Ec", [128, 4, 128], BF16) for _ in range(2)] for _ in range(2)]
        Ec_t = [[Tk("Ec%d%d" % (a, b)) for b in range(2)] for a in range(2)]
        sm = [A.alloc("sm", [128, 64], F32) for _ in range(2)]
        sm_t = [Tk("sm0"), Tk("sm1")]
        imp = [A.alloc("imp", [128, 64], F32) for _ in range(2)]
        sc2 = [A.alloc("sc2", [128, 64], F32) for _ in range(2)]
        top = [A.alloc("top", [128, 16], F32) for _ in range(2)]
        selb = [A.alloc("selb", [128, 64], BF16) for _ in range(2)]
        negm = [A.alloc("negm", [128, 4, 128], BF16) for _ in range(2)]
        tk_t = [Tk("topk0"), Tk("topk1")]
        selT_t = [Tk("selT0"), Tk("selT1")]
        acc = [A.alloc("acc", [128, 4, 128], F32) for _ in range(2)]
        acc_t = [Tk("acc0"), Tk("acc1")]
        ob = A.alloc("ob", [128, 4, 128], BF16)
        ob_t = Tk("ob")
        for hk_ in range(2):
            P.op("pool", lambda e, hk_=hk_: e.memset(negm[hk_][:, :, :], 0.0), w=[selT_t[hk_]])
        ectr = [0]
        PVB = [3, 4, 5, 6]

        def qblock(Dq, q0, nq, qc, gti):
            cwm = cwh if nq == 2 else cw
            lo = 64 - 2 * Dq

            def cmp_gen(hk):
                qv = qT[:, 4 * hk:4 * hk + 4, qc:qc + nq]
                cb0 = 3 + 2 * hk
                for c2 in range(2):
                    rows = 128 if c2 == 0 else 127
                    sb = c2
                    P.mm(banks[sb][0:rows, 0:4 * nq].rearrange("p (g q) -> p g q", g=4), kcmpT[:, hk, c2 * 128:c2 * 128 + rows], qv,
                         True, True, r=[kcmp_t, qT_t], w=[bk[sb]])
                    P.act(Ec[hk][c2][0:rows, :, 0:nq], banks[sb][0:rows, 0:4 * nq].rearrange("p (g q) -> p g q", g=4), AF.Exp,
                          r=[bk[sb]], w=[Ec_t[hk][c2]])
                    basev = 128 * Dq + q0 - 31 - 2048 * c2
                    P.op("pool", lambda e, c2=c2, rows=rows, basev=basev: e.affine_select(
                        out=Ec[hk][c2][0:rows, :, 0:nq], in_=Ec[hk][c2][0:rows, :, 0:nq], pattern=[[0, 4], [1, nq]],
                        compare_op=ALU.is_ge, fill=0.0, base=basev, channel_multiplier=-16), r=[Ec_t[hk][c2]], w=[Ec_t[hk][c2]])
                yield
                for g in range(4):
                    pb = cb0 + g // 2
                    co = (g % 2) * 193
                    for c2 in range(2):
                        rows = 128 if c2 == 0 else 127
                        P.mm(banks[pb][0:nq, co:co + 193], Ec[hk][c2][0:rows, g, 0:nq], Rc[0:rows, c2, hk, :], c2 == 0, c2 == 1,
                             r=[Ec_t[hk][c2], Rc_t], w=[bk[pb]])
                yield
                for g in range(4):
                    pb = cb0 + g // 2
                    co = (g % 2) * 193
                    P.ts("dve", sm[hk][0:nq, g:g + 1], banks[pb][0:nq, co:co + 1], 1e-30, None, ALU.max, r=[bk[pb]], w=[sm_t[hk]])
                P.op("dve", lambda e: e.reciprocal(out=sm[hk][0:nq, 4:8], in_=sm[hk][0:nq, 0:4]), r=[sm_t[hk]], w=[sm_t[hk]])
                P.tt("dve", sm[hk][0:nq, 8:12], sm[hk][0:nq, 4:8], gsig[0:nq, gti, 12 * hk:12 * hk + 10:3], ALU.mult, r=[sm_t[hk], gsig_t], w=[sm_t[hk]])
                yield
                for g in range(4):
                    pb = cb0 + g // 2
                    co = (g % 2) * 193
                    if g == 0:
                        P.ts("dve", imp[hk][0:nq, :], banks[pb][0:nq, co + 1:co + 65], sm[hk][0:nq, 4:5], None, ALU.mult, r=[bk[pb], sm_t[hk]], w=[tk_t[hk]])
                    else:
                        P.stt(imp[hk][0:nq, :], banks[pb][0:nq, co + 1:co + 65], sm[hk][0:nq, 4 + g:5 + g], imp[hk][0:nq, :], ALU.mult, ALU.add,
                              r=[bk[pb], sm_t[hk], tk_t[hk]], w=[tk_t[hk]])
                    P.ts("dve", acc[hk][0:nq, g, :], banks[pb][0:nq, co + 65:co + 193], sm[hk][0:nq, 8 + g:9 + g], None, ALU.mult,
                         r=[bk[pb], sm_t[hk]], w=[acc_t[hk]])
                    if g % 2 == 1:
                        yield
                P.tt("dve", imp[hk][0:nq, :], imp[hk][0:nq, :], cwm[0:nq, 0, lo:lo + 64], ALU.mult, r=[tk_t[hk], cst_t], w=[tk_t[hk]])
                P.tt("dve", imp[hk][0:nq, :], imp[hk][0:nq, :], cwm[0:nq, 1, lo:lo + 64], ALU.add, r=[tk_t[hk], cst_t], w=[tk_t[hk]])
                yield
                P.tt("dve", imp[hk][0:nq, :], imp[hk][0:nq, :], cwm[0:nq, 2, lo:lo + 64], ALU.max, r=[tk_t[hk], cst_t], w=[tk_t[hk]])
                P.tt("dve", imp[hk][0:nq, :], imp[hk][0:nq, :], f0[0:nq, :], ALU.max, r=[tk_t[hk], cst_t], w=[tk_t[hk]])
                yield
                P.op("dve", lambda e: e.max(out=top[hk][0:nq, 0:8], in_=imp[hk][0:nq, :]), r=[tk_t[hk]], w=[tk_t[hk]])
                yield
                P.op("dve", lambda e: e.match_replace(out=sc2[hk][0:nq, :], in_to_replace=top[hk][0:nq, 0:8], in_values=imp[hk][0:nq, :],
                                                      imm_value=-1e9), r=[tk_t[hk]], w=[tk_t[hk]])
                yield
                P.op("dve", lambda e: e.max(out=top[hk][0:nq, 8:16], in_=sc2[hk][0:nq, :]), r=[tk_t[hk]], w=[tk_t[hk]])
                yield
                P.ts("dve", sc2[hk][0:nq, :], imp[hk][0:nq, :], top[hk][0:nq, 15:16], None, ALU.is_ge, r=[tk_t[hk]], w=[tk_t[hk]])
                yield
                P.tt("dve", selb[hk][0:nq, :], sc2[hk][0:nq, :], cwm[0:nq, 0, lo:lo + 64], ALU.mult, r=[tk_t[hk], cst_t], w=[tk_t[hk]])
                yield
                to = 512 + hk * 128
                P.tr(bf(banks[7][:, :])[0:64, to:to + nq], selb[hk][0:nq, :], ident[0:nq, 0:nq], r=[tk_t[hk], ident_t], w=[bk[7]])
                yield
                P.ts("dve", negm[hk][0:64, :, 0:nq], bf(banks[7][:, :])[0:64, to:to + nq].unsqueeze(1).broadcast_to([64, 4, nq]),
                     -1.0, 30000.0, ALU.add, ALU.mult, r=[bk[7]], w=[selT_t[hk]])

            gens = [cmp_gen(0), cmp_gen(1)]
            while gens:
                for gen in list(gens):
                    try:
                        next(gen)
                    except StopIteration:
                        gens.remove(gen)

            for hk in range(2):
                qv = qT[:, 4 * hk:4 * hk + 4, qc:qc + nq]
                def sel_qk(kc):
                    sb = ectr[0] % 3
                    ei = ectr[0] % NE
                    ectr[0] += 1
                    so = banks[sb][:, 0:4 * nq].rearrange("p (g q) -> p g q", g=4)
                    P.mm(so, kselT[:, hk, kc * 128:(kc + 1) * 128], qv, True, False, r=[ksel_t, qT_t], w=[bk[sb]])
                    P.mm(so, ebig[:, kc * 128:(kc + 1) * 128], negm[hk][:, :, 0:nq], False, True, r=[cst_t, selT_t[hk]], w=[bk[sb]])
                    P.act(Pt[ei][:, :, 0:nq], so, AF.Exp, r=[bk[sb]], w=[Pt_t[ei]])
                    if kc == Dq:
                        P.op("pool", lambda e, ei=ei: e.affine_select(
                            out=Pt[ei][:, :, 0:nq], in_=Pt[ei][:, :, 0:nq], pattern=[[0, 4], [1, nq]],
                            compare_op=ALU.is_ge, fill=0.0, base=q0, channel_multiplier=-1), r=[Pt_t[ei]], w=[Pt_t[ei]])
                    return ei

                def sel_pv(kc, ei):
                    for g in range(4):
                        P.mm(banks[PVB[g]][0:nq, 0:129], Pt[ei][:, g, 0:nq], vsel[:, kc, hk, :], kc == 0, kc == Dq,
                             r=[Pt_t[ei], vsel_t], w=[bk[PVB[g]]])
                    if kc == Dq:
                        for g in range(4):
                            P.ts("dve", sm[hk][0:nq, 16 + g:17 + g], banks[PVB[g]][0:nq, 128:129], 1e-30, None, ALU.max, r=[bk[PVB[g]]], w=[sm_t[hk]])
                        P.op("dve", lambda e, hk=hk: e.reciprocal(out=sm[hk][0:nq, 20:24], in_=sm[hk][0:nq, 16:20]), r=[sm_t[hk]], w=[sm_t[hk]])
                        P.tt("dve", sm[hk][0:nq, 24:28], sm[hk][0:nq, 20:24], gsig[0:nq, gti, 12 * hk + 1:12 * hk + 11:3], ALU.mult, r=[sm_t[hk], gsig_t], w=[sm_t[hk]])
                        for g in range(4):
                            P.stt(acc[hk][0:nq, g, :], banks[PVB[g]][0:nq, 0:128], sm[hk][0:nq, 24 + g:25 + g], acc[hk][0:nq, g, :], ALU.mult, ALU.add,
                                  r=[bk[PVB[g]], sm_t[hk], acc_t[hk]], w=[acc_t[hk]])

                def win_qk(kc):
                    sb = ectr[0] % 3
                    ei = ectr[0] % NE
                    ectr[0] += 1
                    wc = kc - 16
                    P.mm(banks[sb][:, 0:4 * nq].rearrange("p (g q) -> p g q", g=4), kwT[:, hk, wc * 128:(wc + 1) * 128], qv,
                         True, True, r=[kw_t, qT_t], w=[bk[sb]])
                    P.act(Pt[ei][:, :, 0:nq], banks[sb][:, 0:4 * nq].rearrange("p (g q) -> p g q", g=4), AF.Exp,
                          r=[bk[sb]], w=[Pt_t[ei]])
                    if kc == Dq:
                        P.op("pool", lambda e, ei=ei: e.affine_select(
                            out=Pt[ei][:, :, 0:nq], in_=Pt[ei][:, :, 0:nq], pattern=[[0, 4], [1, nq]],
                            compare_op=ALU.is_ge, fill=0.0, base=q0, channel_multiplier=-1), r=[Pt_t[ei]], w=[Pt_t[ei]])
                    if kc == Dq - 4:
                        P.op("pool", lambda e, ei=ei: e.affine_select(
                            out=Pt[ei][:, :, 0:nq], in_=Pt[ei][:, :, 0:nq], pattern=[[0, 4], [-1, nq]],
                            compare_op=ALU.is_ge, fill=0.0, base=-q0 - 1, channel_multiplier=1), r=[Pt_t[ei]], w=[Pt_t[ei]])
                    return ei

                def win_pv(kc, ei):
                    wc = kc - 16
                    for g in range(4):
                        P.mm(banks[PVB[g]][0:nq, 0:129], Pt[ei][:, g, 0:nq], vwin[:, wc, hk, :], kc == Dq - 4, kc == Dq,
                             r=[Pt_t[ei], vwin_t], w=[bk[PVB[g]]])

                steps = [(sel_qk, sel_pv, kc) for kc in range(Dq + 1)] + [(win_qk, win_pv, kc) for kc in range(Dq - 4, Dq + 1)]
                LA = 2
                eis = [steps[j][0](steps[j][2]) for j in range(min(LA, len(steps)))]
                for si_, (fq, fp, kc) in enumerate(steps):
                    if si_ + LA < len(steps):
                        eis.append(steps[si_ + LA][0](steps[si_ + LA][2]))
                    fp(kc, eis[si_])
                for g in range(4):
                    P.ts("dve", sm[hk][0:nq, 32 + g:33 + g], banks[PVB[g]][0:nq, 128:129], 1e-30, None, ALU.max, r=[bk[PVB[g]]], w=[sm_t[hk]])
                P.op("dve", lambda e, hk=hk: e.reciprocal(out=sm[hk][0:nq, 36:40], in_=sm[hk][0:nq, 32:36]), r=[sm_t[hk]], w=[sm_t[hk]])
                P.tt("dve", sm[hk][0:nq, 40:44], sm[hk][0:nq, 36:40], gsig[0:nq, gti, 12 * hk + 2:12 * hk + 12:3], ALU.mult, r=[sm_t[hk], gsig_t], w=[sm_t[hk]])
                for g in range(4):
                    P.stt(ob[0:nq, g, :], banks[PVB[g]][0:nq, 0:128], sm[hk][0:nq, 40 + g:41 + g], acc[hk][0:nq, g, :], ALU.mult, ALU.add,
                          r=[bk[PVB[g]], sm_t[hk], acc_t[hk]], w=[ob_t])
                for g in range(4):
                    P.tr(bf(banks[7][:, :])[:, g * 128:g * 128 + nq], ob[0:nq, g, :], ident[0:nq, 0:nq], r=[ob_t, ident_t], w=[bk[7]])
                P.cp("act", yattT[:, 4 * hk:4 * hk + 4, qc:qc + nq],
                     bf(banks[7][:, :])[:, 0:512].rearrange("p (g q) -> p g q", g=4)[:, :, 0:nq], r=[bk[7]], w=[yatt_t])

        qblock(23, 126, 2, 0, 0)
        for i in range(8):
            for _ in range(5):
                if wo_pre:
                    wo_pre.pop(0)()
            qblock(24 + i, 0, 128, 2 + 128 * i, 1 + i)
        P.barrier()
        A.release(m0)

    wo_top = (A.limit - 2 * 16384) // 64 * 64
    wo = [nc.alloc_sbuf_tensor_at("wo%d" % i, [128, 16, 512], BF16, offset=wo_top + i * 16384) for i in range(2)]
    wo_t = [Tk("wo0"), Tk("wo1")]

    def load_wo(obk):
        wb = obk % 2
        for c in range(8):
            P.dma("pool", wo[wb][:, c, :], wol_d[:, c, obk * 512:(obk + 1) * 512], w=[wo_t[wb]])
            P.dma("pool", wo[wb][:, 8 + c, :], woa_d[:, c, obk * 512:(obk + 1) * 512], w=[wo_t[wb]])

    wo_pre = []
    for obk_ in range(2):
        for c_ in range(8):
            wo_pre.append(lambda obk_=obk_, c_=c_: P.dma("pool", wo[obk_][:, c_, :], wol_d[:, c_, obk_ * 512:(obk_ + 1) * 512], w=[wo_t[obk_]]))
            wo_pre.append(lambda obk_=obk_, c_=c_: P.dma("pool", wo[obk_][:, 8 + c_, :], woa_d[:, c_, obk_ * 512:(obk_ + 1) * 512], w=[wo_t[obk_]]))
    run_attention()
    while wo_pre:
        wo_pre.pop(0)()
    P.barrier()
    A.release(m_mixer)

    def run_ffn():
        gout = A.alloc("gout", [128, 2, 8], F32)
        gout_t = Tk("gout")
        P.dma("sp", gout[:, :, :], gout_d[:, :, :], w=[gout_t])
        gffn = A.alloc("gffn", [128, D], F32)
        gffn_t = Tk("gffn")
        P.dma("sp", gffn[:, :], gffn_d[:, :], w=[gffn_t])
        fcw = A.alloc("fcw", [128, 48, 3], F32)
        fcb = A.alloc("fcb", [128, 48], F32)
        fc_t = Tk("fc")
        P.dma("sp", fcw[:, :, :], fcw_d[:, :, :], w=[fc_t])
        P.dma("sp", fcb[:, :], fcb_d[:, :], w=[fc_t])
        h = A.alloc("h", [128, 8, D], F32)
        h_t = [Tk("h%d" % i) for i in range(8)]
        hh = A.alloc("hh", [2, D], F32)
        hh_t = Tk("hh")
        xnT = A.alloc("xnTf", [128, KC, NOWN], BF16)
        xnT_t = Tk("xnTf")
        m1 = A.mark()
        rs = A.alloc("rs", [128, 9, 8], F32)
        rs_t = Tk("rs")
        xnb = A.alloc("xnb", [128, D], BF16)
        xnb_t = Tk("xnb")
        ysq = [A.alloc("ysq", [128, 16, 128], BF16) for _ in range(2)]
        ysq_t = [Tk("ysq0"), Tk("ysq1")]
        assert A.off <= wo_top
        tiles = [(0, 2, hh[:, :], hh_t, 3070)] + [(2 + 128 * i, 128, h[:, i, :], h_t[i], 3072 + 128 * i) for i in range(8)]
        for ti, (c0, nt, hap, hapt, u0) in enumerate(tiles):
            P.dma("sp", hap, x_loc[u0:u0 + nt, :], w=[hapt])
            yb = ti % 2
            P.tt("dve", ysq[yb][:, 0:8, 0:nt], ylruT[:, :, c0:c0 + nt], ylruT[:, :, c0:c0 + nt], ALU.mult, r=ylru_t, w=[ysq_t[yb]])
            P.tt("dve", ysq[yb][:, 8:16, 0:nt], yattT[:, :, c0:c0 + nt], yattT[:, :, c0:c0 + nt], ALU.mult, r=[yatt_t], w=[ysq_t[yb]])
            for br in range(2):
                for c in range(8):
                    P.mm(banks[0][0:nt, br:br + 1], ysq[yb][:, br * 8 + c, 0:nt], ones_bf[:, 0:1], c == 0, c == 7,
                         r=[ysq_t[yb], ones_t], w=[bk[0]])
            P.act(rs[0:nt, ti, 0:2], banks[0][0:nt, 0:2], AF.Ln, r=[bk[0]], w=[rs_t], scale=1.0 / 1024, bias=epsb[0:nt, 0:1])
            P.act(rs[0:nt, ti, 2:4], rs[0:nt, ti, 0:2], AF.Exp, r=[rs_t], w=[rs_t], scale=-0.5)
        for c in range(8):
            P.ts("dve", ylruT[:, c, :], ylruT[:, c, :], gout[:, 0, c:c + 1], None, ALU.mult, r=[gout_t] + ysq_t, w=[ylru_t[c]])
            P.ts("dve", yattT[:, c, :], yattT[:, c, :], gout[:, 1, c:c + 1], None, ALU.mult, r=[gout_t] + ysq_t, w=[yatt_t])
        for obk in range(4):
            wb = obk % 2
            if obk >= 2:
                load_wo(obk)
            for ti, (c0, nt, hap, hapt, u0) in enumerate(tiles):
                for br in range(2):
                    pb = 2 + (ti % 2) * 2 + br
                    ysrc = ylruT if br == 0 else yattT
                    for c in range(8):
                        P.mm(banks[pb][0:nt, :], ysrc[:, c, c0:c0 + nt], wo[wb][:, br * 8 + c, :], c == 0, c == 7,
                             r=[ylru_t[c] if br == 0 else yatt_t, wo_t[wb]], w=[bk[pb]])
                    P.stt(hap[:, obk * 512:(obk + 1) * 512], banks[pb][0:nt, :], rs[0:nt, ti, 2 + br:3 + br],
                          hap[:, obk * 512:(obk + 1) * 512], ALU.mult, ALU.add, r=[bk[pb], rs_t, hapt], w=[hapt])
        for ti, (c0, nt, hap, hapt, u0) in enumerate(tiles):
            P.act(xnb[0:nt, :], hap, AF.Square, r=[hapt], w=[xnb_t, rs_t], accum_out=rs[0:nt, ti, 4:5])
            P.act(rs[0:nt, ti, 5:6], rs[0:nt, ti, 4:5], AF.Ln, r=[rs_t], w=[rs_t], scale=1.0 / D, bias=epsb[0:nt, 0:1])
            P.act(rs[0:nt, ti, 6:7], rs[0:nt, ti, 5:6], AF.Exp, r=[rs_t], w=[rs_t], scale=-0.5)
            P.stt(xnb[0:nt, :], hap, rs[0:nt, ti, 6:7], gffn[0:nt, :], ALU.mult, ALU.mult, r=[hapt, rs_t, gffn_t], w=[xnb_t])
            for half in range(2):
                for j in range(8):
                    jj = half * 8 + j
                    P.tr(bf(banks[6 + half][:, :])[:, j * 128:j * 128 + nt],
                         xnb[0:nt, jj * 128:(jj + 1) * 128], ident[0:nt, 0:nt], r=[xnb_t, ident_t], w=[bk[6 + half]])
                P.cp("dve" if half == 0 else "act", xnT[:, half * 8:(half + 1) * 8, c0:c0 + nt],
                     bf(banks[6 + half][:, :]).rearrange("p (j t) -> p j t", j=8)[:, :, 0:nt], r=[bk[6 + half]], w=[xnT_t])
        P.barrier()
        A.release(m1)
        wup = [nc.alloc_sbuf_tensor_at("wup%d" % i, [128, KC, 512], BF16, offset=y_base + i * 16384) for i in range(2)]
        assert y_base + 2 * 16384 <= y_end
        wup_t = [Tk("wup0"), Tk("wup1")]
        m2 = A.mark()
        wdn = [A.alloc("wdn", [128, 2, D], BF16) for _ in range(3)]
        wdn_t = [Tk("wdn0"), Tk("wdn1"), Tk("wdn2")]
        gT = [A.alloc("gT", [128, 2, 1024], BF16) for _ in range(2)]
        gT_t = [Tk("gT0"), Tk("gT1")]
        usb = [A.alloc("usb", [128, NOWN], F32) for _ in range(2)]
        usb_t = [Tk("usb0"), Tk("usb1")]
        vsb = [A.alloc("vsb", [128, NOWN], F32) for _ in range(2)]
        vsb_t = [Tk("vsb0"), Tk("vsb1")]
        uc = A.alloc("uc", [128, 1024], F32)
        uc_t = Tk("uc")
        gg = A.alloc("gg", [128, 1024], F32)
        gg_t = Tk("gg")
        CT = 342
        rotu = [0]
        def prefetch(G):
            P.dma("pool", wup[G % 2][:, :, :], wup_d[G, :, :, :], w=[wup_t[G % 2]])
            P.dma("pool", wdn[G % 3][:, :, :], wdn_d[G, :, :, :], w=[wdn_t[G % 3]])

        def up(G):
            gb = G % 2
            for fc in range(2):
                fa = 2 * G + fc
                ub = fa % 2
                for ct in range(3):
                    pu = rotu[0] % 2
                    rotu[0] += 1
                    bu, bv = pu * 2, pu * 2 + 1
                    for k in range(KC):
                        P.mm(banks[bu][:, 0:CT], wup[gb][:, k, fc * 128:(fc + 1) * 128], xnT[:, k, ct * CT:(ct + 1) * CT], k == 0, k == KC - 1,
                             r=[wup_t[gb], xnT_t], w=[bk[bu]])
                    for k in range(KC):
                        P.mm(banks[bv][:, 0:CT], wup[gb][:, k, 256 + fc * 128:256 + (fc + 1) * 128], xnT[:, k, ct * CT:(ct + 1) * CT], k == 0, k == KC - 1,
                             r=[wup_t[gb], xnT_t], w=[bk[bv]])
                    P.cp("act", usb[ub][:, ct * CT:(ct + 1) * CT], banks[bu][:, 0:CT], r=[bk[bu]], w=[usb_t[ub]])
                    P.cp("act", vsb[ub][:, ct * CT:(ct + 1) * CT], banks[bv][:, 0:CT], r=[bk[bv]], w=[vsb_t[ub]])
                P.ts("dve", uc[:, :], usb[ub][:, 2:1026], fcw[:, fa, 2:3], fcb[:, fa:fa + 1], ALU.mult, ALU.add, r=[usb_t[ub], fc_t], w=[uc_t])
                P.stt(uc[:, :], usb[ub][:, 1:1025], fcw[:, fa, 1:2], uc[:, :], ALU.mult, ALU.add, r=[usb_t[ub], fc_t, uc_t], w=[uc_t])
                P.stt(uc[:, :], usb[ub][:, 0:1024], fcw[:, fa, 0:1], uc[:, :], ALU.mult, ALU.add, r=[usb_t[ub], fc_t, uc_t], w=[uc_t])
                gelu(gg[:, :], uc[:, :], gg[:, :], [uc_t], gg_t, gg_t)
                P.tt("pool", gT[gb][:, fc, :], gg[:, :], vsb[ub][:, 2:1026], ALU.mult, r=[gg_t, vsb_t[ub]], w=[gT_t[gb]])

        def down(G):
            gb = G % 2
            wd = G % 3
            for tt in range(8):
                for obk in range(4):
                    pb = 4 + (tt * 4 + obk) % 4
                    for fc in range(2):
                        P.mm(banks[pb][:, :], gT[gb][:, fc, tt * 128:(tt + 1) * 128], wdn[wd][:, fc, obk * 512:(obk + 1) * 512], fc == 0, fc == 1,
                             r=[gT_t[gb], wdn_t[wd]], w=[bk[pb]])
                    P.tt("dve", h[:, tt, obk * 512:(obk + 1) * 512], banks[pb][:, :], h[:, tt, obk * 512:(obk + 1) * 512], ALU.add,
                         r=[bk[pb], h_t[tt]], w=[h_t[tt]])

        prefetch(0)
        for G in range(24):
            if G + 1 < 24:
                prefetch(G + 1)
            up(G)
            if G > 0:
                down(G - 1)
        down(23)
        P.barrier()
        A.release(m2)
        gfin = A.alloc("gfin", [128, D], F32)
        gfin_t = Tk("gfin")
        P.dma("sp", gfin[:, :], gfin_d[:, :], w=[gfin_t])
        fs = A.alloc("fs", [128, 8, 4], F32)
        fs_t = Tk("fs")
        xs2 = A.alloc("xs2", [128, D], BF16)
        xs2_t = Tk("xs2")
        for tt in range(8):
            P.act(xs2[:, :], h[:, tt, :], AF.Square, r=[h_t[tt]], w=[xs2_t, fs_t], accum_out=fs[:, tt, 0:1])
            P.act(fs[:, tt, 1:2], fs[:, tt, 0:1], AF.Ln, r=[fs_t], w=[fs_t], scale=1.0 / D, bias=epsb[:, 0:1])
            P.act(fs[:, tt, 2:3], fs[:, tt, 1:2], AF.Exp, r=[fs_t], w=[fs_t], scale=-0.5)
            P.stt(h[:, tt, :], h[:, tt, :], fs[:, tt, 2:3], gfin[:, :], ALU.mult, ALU.mult, r=[h_t[tt], fs_t, gfin_t], w=[h_t[tt]])
            P.dma("sp", y_out[tt * 128:(tt + 1) * 128, :], h[:, tt, :], r=[h_t[tt]], is_output=True)

    run_ffn()
    P.finish()
    return nc


def _tile_k(w):
    n = w.shape[1]
    return np.ascontiguousarray(w.reshape(KC, 128, n).transpose(1, 0, 2))


def _host_consts():
    c = {}
    c["ident"] = np.eye(128, dtype=np.float32).astype(ml_dtypes.bfloat16)
    eb = np.zeros((128, S), np.float32)
    for s in range(64):
        eb[s, s * 64:(s + 1) * 64] = 1.0
    c["ebig"] = eb.astype(ml_dtypes.bfloat16)
    n = np.arange(256)
    s = np.arange(64)
    ov = np.clip(np.minimum(n[:, None] * 16 + 32, s[None, :] * 64 + 64) - np.maximum(n[:, None] * 16, s[None, :] * 64), 0, None)
    ov = (ov / 32.0).astype(np.float32)
    ov[255] = 0.0
    c["ovl"] = np.ascontiguousarray(ov.reshape(2, 128, 64).transpose(1, 0, 2))

    def wide(hi):
        rel = np.arange(128)[None, :] - 64
        causal = (rel <= hi[:, None]).astype(np.float32)
        forced = ((rel <= hi[:, None]) & (rel > hi[:, None] - 2)).astype(np.float32)
        return np.ascontiguousarray(np.stack([causal, causal - 1.0, forced * 1e4], axis=1).astype(np.float32))

    c["cwide"] = wide((np.arange(128) >= 64).astype(np.int64))
    c["cwide_h"] = wide(np.array([1, 1], np.int64))
    return c


_NC_CACHE = {}


def kernel(**inp):
    f32 = np.float32
    g = {k: np.asarray(v, dtype=f32) for k, v in inp.items()}
    x = g["x"]
    w_in = g["w_in"][0]
    rep = {}
    rep["wA1"] = _tile_k(w_in[:, 0:1024])
    rep["wB3"] = _tile_k(w_in[:, 1024:2048])
    rep["wB2"] = _tile_k(w_in[:, 2048:3072])
    rep["wA2"] = _tile_k(np.concatenate([w_in[:, 3072:3584], w_in[:, 3584:3840], w_in[:, 3840:4096]], axis=1))
    rep["wB1"] = _tile_k(np.concatenate([w_in[:, 4096:4352], w_in[:, 4352:4608], w_in[:, 4608:4632]], axis=1))
    rep["gmixT"] = np.ascontiguousarray(g["g_mix"][0].reshape(KC, 128).T)
    rep["lcw"] = np.ascontiguousarray(g["lru_conv_w"][0].reshape(4, 8, 128).transpose(2, 1, 0))
    rep["lvec"] = np.ascontiguousarray(np.stack([g["lru_conv_b"][0], g["lru_ba"][0], g["lru_bx"][0], g["lru_lambda"][0]], 0)
                                       .reshape(4, 8, 128).transpose(2, 0, 1))
    rep["wa"] = np.ascontiguousarray(g["lru_wa"][0].transpose(1, 0, 2))
    rep["wx"] = np.ascontiguousarray(g["lru_wx"][0].transpose(1, 0, 2))
    rep["w1k"] = np.ascontiguousarray(g["cmp_w1_k"][0].reshape(32, 128, 128).transpose(1, 0, 2))
    rep["w1v"] = np.ascontiguousarray(g["cmp_w1_v"][0].reshape(32, 128, 128).transpose(1, 0, 2))
    rep["peT"] = np.ascontiguousarray(np.stack([g["cmp_pe_k"][0].T, g["cmp_pe_v"][0].T], axis=1))
    rep["cmpb"] = np.ascontiguousarray(np.stack([g["cmp_b1_k"][0], g["cmp_b1_v"][0]], axis=1))
    rep["w2k"] = np.ascontiguousarray(g["cmp_w2_k"][0])
    rep["w2v"] = np.ascontiguousarray(g["cmp_w2_v"][0])
    w_out = g["w_out"][0]
    rep["wol"] = np.ascontiguousarray(w_out[0:1024].reshape(8, 128, D).transpose(1, 0, 2))
    rep["woa"] = np.ascontiguousarray(w_out[1024:2048].reshape(8, 128, D).transpose(1, 0, 2))
    rep["gout"] = np.ascontiguousarray(np.stack([g["g_lru_out"][0].reshape(8, 128).T, g["g_attn_out"][0].reshape(8, 128).T], axis=1))
    rep["gffn_bc"] = np.ascontiguousarray(np.broadcast_to(g["g_ffn"][0][None, :], (128, D)))
    rep["gfin_bc"] = np.ascontiguousarray(np.broadcast_to(g["g_final"][None, :], (128, D)))
    w_up = g["w_up"][0]
    wu = w_up[:, 0:6144].reshape(KC, 128, 24, 256)
    wv = w_up[:, 6144:12288].reshape(KC, 128, 24, 256)
    rep["wup"] = np.ascontiguousarray(np.concatenate([wu, wv], axis=3).transpose(2, 1, 0, 3))
    rep["wdn"] = np.ascontiguousarray(g["w_down"][0].reshape(24, 2, 128, D).transpose(0, 2, 1, 3))
    rep["fcw"] = np.ascontiguousarray(g["ffn_conv_w"][0].reshape(3, 48, 128).transpose(2, 1, 0))
    rep["fcb"] = np.ascontiguousarray(g["ffn_conv_b"][0].reshape(48, 128).T)
    rep.update(_host_consts())

    in_maps = []
    for c in range(8):
        b, r = c // 4, c % 4
        pad = 1024 * (3 - r)
        xl = np.zeros((S, D), f32)
        xl[pad:] = x[b, 0:S - pad]
        valid = np.zeros((S,), f32)
        valid[pad:] = 1.0
        m = dict(rep)
        m["x_loc"] = xl
        m["stflag"] = np.ascontiguousarray(np.broadcast_to(valid[0:S:512][None, :], (128, 8)))
        m["validT"] = np.ascontiguousarray(valid.reshape(32, 128).T)
        vn = np.zeros((256,), f32)
        vn[pad // 16:255] = 1.0
        m["validn"] = np.ascontiguousarray(vn.reshape(2, 128).T)
        f0 = np.zeros((128, 64), f32)
        f0[:, pad // 64] = 1e4
        m["f0"] = f0
        in_maps.append(m)

    if "nc" not in _NC_CACHE:
        _NC_CACHE["nc"] = build_program()
    res = run_bass_kernel_spmd(_NC_CACHE["nc"], in_maps, core_ids=list(range(8)))
    out = np.zeros((2, S, D), f32)
    for c in range(8):
        b, r = c // 4, c % 4
        out[b, r * 1024:(r + 1) * 1024] = np.asarray(res.results[c]["y"], dtype=f32)
    return out
```
